# Optimizing a Trainium2 kernel written in Bass

```python
import jax, jax.numpy as jnp
from jax import lax
import numpy as np

D_MODEL = 1024
BATCH = 2
SEQ = 16384
DEPTH = 2

MIX_WIDTH = D_MODEL
CONV_WIDTH = MIX_WIDTH // 2
CONV_GROUPS = 4
CONV_WIN = 31
DN_WIDTH = MIX_WIDTH - CONV_WIDTH
DN_HEADS = 4
DN_HEAD_DIM = DN_WIDTH // DN_HEADS
SHORT_CONV = 4
CHUNK = 64
D_FF_DENSE = 2816
N_EXPERTS = 8
TOP_K = 2
D_FF_EXPERT = 3584
N_DENSE = (DEPTH + 1) // 2
N_MOE = DEPTH // 2
DEEPNORM_ALPHA = (2 * DEPTH) ** 0.25
DEEPNORM_BETA = (8 * DEPTH) ** -0.25
LN_EPS = 1e-5
RMS_EPS = 1e-6
L2_EPS = 1e-6
IN_SIZES = (CONV_WIDTH, CONV_WIDTH, DN_WIDTH, DN_WIDTH, DN_WIDTH, DN_WIDTH, DN_HEADS, DN_HEADS)
IN_COLS = sum(IN_SIZES)
IN_SPLITS = tuple(int(s) for s in np.cumsum(IN_SIZES)[:-1])

kernel_name = "hybrid_conformer_deltanet_deepnorm_moe"


def layer_norm(x, g, b):
    xf = x.astype(jnp.float32)
    mu = xf.mean(-1, keepdims=True)
    var = jnp.square(xf - mu).mean(-1, keepdims=True)
    return ((xf - mu) * lax.rsqrt(var + LN_EPS) * g + b).astype(x.dtype)


def causal_depthwise_conv(x, w):
    width = w.shape[0]
    return lax.conv_general_dilated(
        x, w[:, None, :].astype(x.dtype), (1,), ((width - 1, 0),),
        dimension_numbers=("NWC", "WIO", "NWC"), feature_group_count=x.shape[-1])


def conformer_conv(h_val, h_gate, dw_w, dw_b, ln_g, ln_b):
    h = h_val * jax.nn.sigmoid(h_gate)
    h = causal_depthwise_conv(h, dw_w) + dw_b
    h = layer_norm(h, ln_g, ln_b)
    return jax.nn.silu(h)


def l2norm(t):
    return t * lax.rsqrt(jnp.sum(t * t, -1, keepdims=True) + L2_EPS)


def chunk_gated_delta_rule(q, k, v, g, beta):
    B, S, H, Dk = q.shape
    Dv = v.shape[-1]
    N = S // CHUNK
    q = q * (Dk ** -0.5)
    to_chunks = lambda t: t.reshape(B, N, CHUNK, H, t.shape[-1]).transpose(0, 3, 1, 2, 4)
    q, k, v = to_chunks(q), to_chunks(k), to_chunks(v)
    g = g.reshape(B, N, CHUNK, H).transpose(0, 3, 1, 2)
    beta = beta.reshape(B, N, CHUNK, H).transpose(0, 3, 1, 2)
    g = jnp.cumsum(g, axis=-1)
    kb = k * beta[..., None]
    vb = v * beta[..., None]
    causal = jnp.tril(jnp.ones((CHUNK, CHUNK), bool))
    strict = jnp.tril(jnp.ones((CHUNK, CHUNK), bool), -1)
    decay = jnp.exp(jnp.where(causal, g[..., :, None] - g[..., None, :], -jnp.inf))
    lower = jnp.where(strict, jnp.einsum('bhncd,bhnjd->bhncj', kb, k) * decay, 0.0)
    eye = jnp.eye(CHUNK, dtype=jnp.float32)
    rhs = jnp.concatenate([vb, kb * jnp.exp(g)[..., None]], axis=-1)
    sol = lax.linalg.triangular_solve(eye + lower, rhs, left_side=True, lower=True, unit_diagonal=True)
    u, w = sol[..., :Dv], sol[..., Dv:]
    attn = jnp.einsum('bhncd,bhnjd->bhncj', q, k) * decay

    def step(state, inp):
        qc, kc, uc, wc, gc, ac = inp
        v_new = uc - jnp.einsum('bhcd,bhde->bhce', wc, state)
        o = jnp.einsum('bhcd,bhde->bhce', qc * jnp.exp(gc)[..., None], state) \
            + jnp.einsum('bhcj,bhje->bhce', ac, v_new)
        g_last = gc[..., -1]
        k_dec = kc * jnp.exp(g_last[..., None] - gc)[..., None]
        state = state * jnp.exp(g_last)[..., None, None] + jnp.einsum('bhcd,bhce->bhde', k_dec, v_new)
        return state, o

    lead = lambda t: jnp.moveaxis(t, 2, 0)
    s0 = jnp.zeros((B, H, Dk, Dv), jnp.float32)
    _, o = lax.scan(step, s0, (lead(q), lead(k), lead(u), lead(w), lead(g), lead(attn)))
    o = jnp.moveaxis(o, 0, 2)
    return o.transpose(0, 2, 3, 1, 4).reshape(B, S, H, Dv)


def gated_deltanet(q, k, v, z, b_logit, a_logit, sc_w, a_log, dt_bias, on_g):
    B, S, _ = q.shape
    qkv = jax.nn.silu(causal_depthwise_conv(jnp.concatenate([q, k, v], -1), sc_w))
    q, k, v = jnp.split(qkv, 3, axis=-1)
    heads = lambda t: t.reshape(B, S, DN_HEADS, DN_HEAD_DIM).astype(jnp.float32)
    q, k, v = l2norm(heads(q)), l2norm(heads(k)), heads(v)
    beta = jax.nn.sigmoid(b_logit.astype(jnp.float32))
    g = -jnp.exp(a_log.astype(jnp.float32)) * jax.nn.softplus(
        a_logit.astype(jnp.float32) + dt_bias.astype(jnp.float32))
    o = chunk_gated_delta_rule(q, k, v, g, beta)
    o = o * lax.rsqrt(jnp.mean(o * o, -1, keepdims=True) + RMS_EPS) * on_g.astype(jnp.float32)
    o = o * jax.nn.silu(heads(z))
    return o.reshape(B, S, DN_WIDTH).astype(z.dtype)


def hybrid_mixer(x, w_in, dw_w, dw_b, cln_g, cln_b, sc_w, a_log, dt_bias, on_g, w_out):
    h = x @ w_in
    c_val, c_gate, q, k, v, z, b_logit, a_logit = jnp.split(h, IN_SPLITS, axis=-1)
    y_conv = conformer_conv(c_val, c_gate, dw_w, dw_b, cln_g, cln_b)
    y_dn = gated_deltanet(q, k, v, z, b_logit, a_logit, sc_w, a_log, dt_bias, on_g)
    return jnp.concatenate([y_conv, y_dn], axis=-1) @ w_out


def swiglu(x, w_gate, w_up, w_down):
    return (jax.nn.silu(x @ w_gate) * (x @ w_up)) @ w_down


def moe_swiglu(x, router_w, w_gate, w_up, w_down):
    logits = (x @ router_w).astype(jnp.float32)
    top_v, top_i = lax.top_k(logits, TOP_K)
    gates = jax.nn.softmax(top_v, axis=-1)
    combine = jnp.sum(jax.nn.one_hot(top_i, N_EXPERTS, dtype=jnp.float32) * gates[..., None], axis=-2)
    y = jnp.zeros_like(x)
    for e in range(N_EXPERTS):
        y = y + combine[..., e:e + 1].astype(x.dtype) * swiglu(x, w_gate[e], w_up[e], w_down[e])
    return y


def setup_inputs(seed: int = 0) -> dict:
    key = jax.random.key(seed)
    ks = jax.random.split(key, 24)
    f32 = jnp.float32
    nrm = lambda k, shape, scale: jax.random.normal(k, shape, f32) * scale
    dt = jnp.exp(jax.random.uniform(ks[8], (DEPTH, DN_HEADS), f32, np.log(1e-3), np.log(1e-1)))
    return {
        "x": nrm(ks[0], (BATCH, SEQ, D_MODEL), 1.0),
        "w_in": nrm(ks[1], (DEPTH, D_MODEL, IN_COLS), D_MODEL ** -0.5),
        "conv_dw_w": nrm(ks[2], (DEPTH, CONV_WIN, CONV_WIDTH), CONV_WIN ** -0.5),
        "conv_dw_b": nrm(ks[3], (DEPTH, CONV_WIDTH), 0.02),
        "conv_ln_g": 1.0 + nrm(ks[4], (DEPTH, CONV_WIDTH), 0.02),
        "conv_ln_b": nrm(ks[5], (DEPTH, CONV_WIDTH), 0.02),
        "short_conv_w": nrm(ks[6], (DEPTH, SHORT_CONV, 3 * DN_WIDTH), SHORT_CONV ** -0.5),
        "a_log": jnp.log(jax.random.uniform(ks[7], (DEPTH, DN_HEADS), f32, 1.0, 16.0)),
        "dt_bias": dt + jnp.log(-jnp.expm1(-dt)),
        "out_norm_g": 1.0 + nrm(ks[9], (DEPTH, DN_HEAD_DIM), 0.02),
        "w_out": nrm(ks[10], (DEPTH, MIX_WIDTH, D_MODEL), MIX_WIDTH ** -0.5 * DEEPNORM_BETA),
        "ln_mix_g": 1.0 + nrm(ks[11], (DEPTH, D_MODEL), 0.02),
        "ln_mix_b": nrm(ks[12], (DEPTH, D_MODEL), 0.02),
        "ffn_w_gate": nrm(ks[13], (N_DENSE, D_MODEL, D_FF_DENSE), D_MODEL ** -0.5),
        "ffn_w_up": nrm(ks[14], (N_DENSE, D_MODEL, D_FF_DENSE), D_MODEL ** -0.5),
        "ffn_w_down": nrm(ks[15], (N_DENSE, D_FF_DENSE, D_MODEL), D_FF_DENSE ** -0.5 * DEEPNORM_BETA),
        "router_w": nrm(ks[16], (N_MOE, D_MODEL, N_EXPERTS), D_MODEL ** -0.5),
        "moe_w_gate": nrm(ks[17], (N_MOE, N_EXPERTS, D_MODEL, D_FF_EXPERT), D_MODEL ** -0.5),
        "moe_w_up": nrm(ks[18], (N_MOE, N_EXPERTS, D_MODEL, D_FF_EXPERT), D_MODEL ** -0.5),
        "moe_w_down": nrm(ks[19], (N_MOE, N_EXPERTS, D_FF_EXPERT, D_MODEL), D_FF_EXPERT ** -0.5 * DEEPNORM_BETA),
        "ln_ffn_g": 1.0 + nrm(ks[20], (DEPTH, D_MODEL), 0.02),
        "ln_ffn_b": nrm(ks[21], (DEPTH, D_MODEL), 0.02),
    }


def reference(x, w_in, conv_dw_w, conv_dw_b, conv_ln_g, conv_ln_b, short_conv_w, a_log, dt_bias,
              out_norm_g, w_out, ln_mix_g, ln_mix_b, ffn_w_gate, ffn_w_up, ffn_w_down, router_w,
              moe_w_gate, moe_w_up, moe_w_down, ln_ffn_g, ln_ffn_b):
    for layer in range(DEPTH):
        m = hybrid_mixer(x, w_in[layer], conv_dw_w[layer], conv_dw_b[layer], conv_ln_g[layer],
                         conv_ln_b[layer], short_conv_w[layer], a_log[layer], dt_bias[layer],
                         out_norm_g[layer], w_out[layer])
        x = layer_norm(DEEPNORM_ALPHA * x + m, ln_mix_g[layer], ln_mix_b[layer])
        j = layer // 2
        if layer % 2 == 0:
            f = swiglu(x, ffn_w_gate[j], ffn_w_up[j], ffn_w_down[j])
        else:
            f = moe_swiglu(x, router_w[j], moe_w_gate[j], moe_w_up[j], moe_w_down[j])
        x = layer_norm(DEEPNORM_ALPHA * x + f, ln_ffn_g[layer], ln_ffn_b[layer])
    return x
```

```python
import re
import numpy as np
import concourse.bass as bass
import concourse.mybir as mybir
from concourse.bass_utils import run_bass_kernel_spmd

F32 = mybir.dt.float32
BF16 = mybir.dt.bfloat16
I32 = mybir.dt.int32
AF = mybir.ActivationFunctionType
ALU = mybir.AluOpType
AX = mybir.AxisListType

D = 1024
NCORES = 8
NSEG = 4
HALO = 128
CW = 31
ALPHA = 4.0 ** 0.25
LN_EPS = 1e-5
RMS_EPS = 1e-6
L2_EPS = 1e-6
IN_COLS = 3080
DFF_DENSE = 2816
DFF_EXP = 3584
NEXP = 8
PV_DWW = 0
PV_DWB = 124
PV_CLG = 128
PV_CLB = 132
PV_SCW = 136
PV_ONG = 184
PV_ALOG128 = 185
PV_ALOG64 = 186
PV_DTB128 = 188
PV_DTB64 = 189
PV_MSEG = 192
PV_MPRED = 196
NPV = 200


class Sched:
    SEM_MAX = 30000

    def __init__(self, nc, dma_ring=8):
        self.nc = nc
        self.engs = {"pe": nc.tensor, "dve": nc.vector, "act": nc.scalar, "pool": nc.gpsimd, "sp": nc.sync}
        self.cnt = {e: 0 for e in self.engs}
        self.csem = {e: [] for e in self.engs}
        self.ring = {}
        self.ring_n = dma_ring
        self.dcnt = {e: 0 for e in self.engs}
        self.last_w = {}
        self.readers = {}
        self.seen = {e: {} for e in self.engs}
        self.nsem = 0
        self.ctoks = []

    def _newsem(self, name):
        self.nsem += 1
        return self.nc.alloc_semaphore(name=f"{name}_{self.nsem}")

    def _wait(self, eng, tok):
        sem, val, semid, src = tok
        if self.seen[eng].get(semid, 0) >= val:
            return
        if src == eng and eng == "pe":
            return
        self.engs[eng].wait_ge(sem, val)
        self.seen[eng][semid] = val

    def _deps(self, eng, reads, writes):
        toks = []
        for k in reads:
            if k in self.last_w:
                toks.append(self.last_w[k])
        for k in writes:
            if k in self.last_w:
                toks.append(self.last_w[k])
            toks.extend(self.readers.get(k, {}).values())
        for t in toks:
            self._wait(eng, t)

    def _record(self, tok, reads, writes):
        for k in reads:
            d = self.readers.setdefault(k, {})
            old = d.get(tok[2])
            if old is None or old[1] < tok[1]:
                d[tok[2]] = tok
        for k in writes:
            self.last_w[k] = tok
            self.readers[k] = {}

    def op(self, eng, fn, reads=(), writes=()):
        writes = list(writes) + [k for k in reads if k.startswith("ps")]
        reads = [k for k in reads if not k.startswith("ps")]
        self._deps(eng, reads, writes)
        inst = fn(self.engs[eng])
        n = self.cnt[eng]
        si, v = divmod(n, self.SEM_MAX)
        if si >= len(self.csem[eng]):
            self.csem[eng].append(self._newsem(f"c_{eng}"))
        sem = self.csem[eng][si]
        inst.then_inc(sem, 1)
        self.cnt[eng] = n + 1
        tok = (sem, v + 1, f"c_{eng}_{si}", eng)
        self._record(tok, reads, writes)
        return tok

    def dma(self, eng, out, in_, reads=(), writes=(), **kw):
        if eng not in self.ring:
            self.ring[eng] = [[self._newsem(f"d_{eng}"), 0] for _ in range(self.ring_n)]
        i = self.dcnt[eng]
        slot = self.ring[eng][i % self.ring_n]
        semid = f"d_{eng}_{i % self.ring_n}"
        if slot[1] > 0:
            self._wait(eng, (slot[0], 16 * slot[1], semid, "dma"))
        self._deps(eng, reads, writes)
        inst = self.engs[eng].dma_start(out=out, in_=in_, **kw)
        self.last_inst = inst
        slot[1] += 1
        inst.then_inc(slot[0], 16)
        self.dcnt[eng] = i + 1
        tok = (slot[0], 16 * slot[1], semid, "dma")
        self._record(tok, reads, writes)
        return tok

    def ind_dma(self, out, in_, idx_ap, gather, reads=(), writes=()):
        eng = "pool"
        if eng not in self.ring:
            self.ring[eng] = [[self._newsem(f"d_{eng}"), 0] for _ in range(self.ring_n)]
        i = self.dcnt[eng]
        slot = self.ring[eng][i % self.ring_n]
        semid = f"d_{eng}_{i % self.ring_n}"
        if slot[1] > 0:
            self._wait(eng, (slot[0], 16 * slot[1], semid, "dma"))
        self._deps(eng, reads, writes)
        off = bass.IndirectOffsetOnAxis(ap=idx_ap, axis=0)
        if gather:
            inst = self.nc.gpsimd.indirect_dma_start(out=out, out_offset=None, in_=in_, in_offset=off)
        else:
            inst = self.nc.gpsimd.indirect_dma_start(out=out, out_offset=off, in_=in_, in_offset=None)
        slot[1] += 1
        inst.then_inc(slot[0], 16)
        self.dcnt[eng] = i + 1
        tok = (slot[0], 16 * slot[1], semid, "dma")
        self._record(tok, reads, writes)
        return tok

    def coll(self, kind, in_ap, out_ap, groups, reads=(), writes=()):
        self._deps("pool", reads, writes)
        sem = self._newsem("cc")
        inst = self.nc.gpsimd.collective_compute(kind, ALU.bypass, replica_groups=groups, ins=[in_ap], outs=[out_ap])
        inst.then_inc(sem)
        tok = (sem, 1, f"cc_{self.nsem}", "dma")
        self.ctoks.append(tok)
        self._record(tok, reads, writes)
        return tok

    def barrier(self):
        toks = list(self.ctoks)
        for e in self.engs:
            n = self.cnt[e]
            if n:
                si, v = divmod(n - 1, self.SEM_MAX)
                toks.append((self.csem[e][si], v + 1, f"c_{e}_{si}", e))
        for q, slots in self.ring.items():
            for j, (sem, c) in enumerate(slots):
                if c:
                    toks.append((sem, 16 * c, f"d_{q}_{j}", "dma"))
        for e in self.engs:
            for t in toks:
                if t[3] == e:
                    continue
                self._wait(e, t)
        self.last_w = {}
        self.readers = {}


class Ctx:
    def __init__(self, nc):
        self.nc = nc
        self.S = Sched(nc)
        self.scopes = []
        self.ps = [nc.alloc_psum_tensor(f"psb{i}", [128, 512], F32) for i in range(8)]
        self.psk = [f"ps{i}" for i in range(8)]
        self.uid = 0

    def sb(self, name, shape, dt=F32):
        self.uid += 1
        g = self.nc.sbuf_tensor(f"{name}_{self.uid}", list(shape), dt)
        t = g.__enter__()
        self.scopes[-1].append(g)
        return t

    def push(self):
        self.scopes.append([])

    def pop(self):
        self.S.barrier()
        for g in reversed(self.scopes.pop()):
            g.__exit__(None, None, None)

    def consts(self):
        S = self.S
        c = {}
        c["ident"] = self.sb("ident", [128, 128])
        c["ones"] = self.sb("ones", [128, 128])
        S.op("pool", lambda e: e.memset(c["ident"][:], 1.0), writes=["c_ident"])
        S.op("pool", lambda e: e.affine_select(out=c["ident"][:], in_=c["ident"][:], pattern=[[-1, 128]],
                                               compare_op=ALU.is_equal, fill=0.0, base=0, channel_multiplier=1),
             reads=["c_ident"], writes=["c_ident"])
        S.op("pool", lambda e: e.memset(c["ones"][:], 1.0), writes=["c_ones"])
        self.c = c
        return c


def phase_a(cx, S_core, xin, win, pv_d, yc_d, zg_d, qkv_d, gb_d):
    nc, S, c = cx.nc, cx.S, cx.c
    T = min(256, S_core)
    cx.push()
    wbf = cx.sb("wbf", [128, 8, IN_COLS], BF16)
    winv = win.rearrange("(kc p) n -> p kc n", p=128)
    for kc in range(8):
        S.dma("pool", wbf[:, kc, :], winv[:, kc, :], writes=[f"wbf{kc}"])
    wkeys = [f"wbf{kc}" for kc in range(8)]
    pv = cx.sb("pv", [128, NPV])
    S.dma("sp", pv[:], pv_d[:, :], writes=["pv"])
    o512 = cx.sb("o512", [128, 128])
    S.op("pool", lambda e: e.memset(o512[:], 1.0 / 512.0), writes=["o512"])
    xt = [cx.sb(f"xt{i}", [128, 2, D]) for i in range(2)]
    xT = cx.sb("xT", [128, 8, T], BF16)
    U = cx.sb("U", [128, 4, 30 + T], BF16)
    dgw = cx.sb("dgw", [128, 4, CW, 128], BF16)
    for cc in range(4):
        for j in range(CW):
            eng = "pool" if (cc * CW + j) % 2 == 0 else "dve"
            S.op(eng, lambda e, cc=cc, j=j: e.tensor_scalar(out=dgw[:, cc, j, :], in0=c["ident"][:],
                                                            scalar1=pv[:, PV_DWW + cc * 31 + j:PV_DWW + cc * 31 + j + 1], scalar2=None, op0=ALU.mult),
                 reads=["c_ident", "pv"], writes=[f"dgw{cc}_{eng}"])
    PRE = cx.sb("PRE", [128, 12, 3 + T])
    sg = cx.sb("sg", [128, 4, T])
    acc = cx.sb("acc", [128, 4, T])
    sq = [cx.sb(f"sq{i}", [128, T]) for i in range(2)]
    mean = cx.sb("mean", [128, T])
    rstd = cx.sb("rstd", [128, T])
    ycs = cx.sb("ycs", [128, 4, T])
    zgs = cx.sb("zgs", [128, 4, T])
    qa = [cx.sb(f"qa{i}", [128, T]) for i in range(12)]
    lg = cx.sb("lg", [8, T])
    ps, pk = cx.ps, cx.psk
    pctr = [0]

    def bank():
        i = pctr[0] % 8
        pctr[0] += 1
        return ps[i], pk[i]

    ntiles = S_core // T
    tiles = [(-1, 128)] + [(i, T) for i in range(ntiles)]

    def xload(it):
        ti, Tt = tiles[it]
        r0 = 0 if ti < 0 else HALO + ti * T
        S.dma("sp", xt[it % 2][:, 0:Tt // 128, :], xin[r0:r0 + Tt, :].rearrange("(s p) d -> p s d", p=128), writes=[f"xt{it % 2}"])

    def stage1(it):
        ti, Tt = tiles[it]
        nsub = Tt // 128
        xb = xt[it % 2]
        xk = f"xt{it % 2}"
        if it < 2:
            xload(it)
        for kc in range(8):
            pb, pkk = bank()
            if kc == 7 and it + 2 < len(tiles):
                pass
            for s_ in range(nsub):
                S.op("pe", lambda e, s_=s_, kc=kc, pb=pb: e.transpose(out=pb[:, s_ * 128:(s_ + 1) * 128],
                                                                       in_=xb[:, s_, kc * 128:(kc + 1) * 128],
                                                                       identity=c["ident"][:]),
                     reads=[xk, "c_ident"], writes=[pkk])
            if kc % 2 == 0:
                S.op("act", lambda e, kc=kc, pb=pb: e.activation(out=xT[:, kc, 0:Tt], in_=pb[:, 0:Tt], func=AF.Copy),
                     reads=[pkk], writes=[f"xT{kc}"])
            else:
                S.op("dve", lambda e, kc=kc, pb=pb: e.tensor_copy(out=xT[:, kc, 0:Tt], in_=pb[:, 0:Tt]),
                     reads=[pkk], writes=[f"xT{kc}"])
        xTk = [f"xT{kc}" for kc in range(8)]
        if it + 2 < len(tiles):
            xload(it + 2)

        def hchunk(oc):
            pb, pkk = bank()
            for kc in range(8):
                S.op("pe", lambda e, kc=kc, pb=pb: e.matmul(pb[:, 0:Tt], lhsT=wbf[:, kc, oc * 128:(oc + 1) * 128],
                                                            rhs=xT[:, kc, 0:Tt], start=(kc == 0), stop=(kc == 7)),
                     reads=[wkeys[kc], xTk[kc]], writes=[pkk])
            return pb, pkk

        for cc in range(4):
            pb, pkk = hchunk(4 + cc)
            S.op("act", lambda e, cc=cc, pb=pb: e.activation(out=sg[:, cc, 0:Tt], in_=pb[:, 0:Tt], func=AF.Sigmoid),
                 reads=[pkk], writes=[f"sg{cc}"])
        for cc in range(4):
            pb, pkk = hchunk(cc)
            S.op("dve", lambda e, cc=cc, pb=pb: e.tensor_tensor(out=U[:, cc, 30:30 + Tt], in0=pb[:, 0:Tt],
                                                                in1=sg[:, cc, 0:Tt], op=ALU.mult),
                 reads=[pkk, f"sg{cc}"], writes=[f"U{cc}"])
        for j in range(12):
            pb, pkk = hchunk(8 + j)
            S.op("act", lambda e, j=j, pb=pb: e.activation(out=PRE[:, j, 3:3 + Tt], in_=pb[:, 0:Tt], func=AF.Copy),
                 reads=[pkk], writes=[f"PRE{j}"])
        if ti < 0:
            for cc in range(4):
                S.op("pool", lambda e, cc=cc: e.tensor_copy(out=U[:, cc, 0:30], in_=U[:, cc, Tt:Tt + 30]),
                     reads=[f"U{cc}"], writes=[f"U{cc}"])
            for j in range(12):
                S.op("pool", lambda e, j=j: e.tensor_copy(out=PRE[:, j, 0:3], in_=PRE[:, j, Tt:Tt + 3]),
                     reads=[f"PRE{j}"], writes=[f"PRE{j}"])
            return
        t0 = ti * T
        for cc in range(4):
            pb, pkk = hchunk(20 + cc)
            S.op("act", lambda e, cc=cc, pb=pb: e.activation(out=zgs[:, cc, 0:Tt], in_=pb[:, 0:Tt], func=AF.Silu),
                 reads=[pkk], writes=[f"zgs{cc}"])
            S.dma("pool", zg_d[cc * 128:(cc + 1) * 128, t0:t0 + Tt], zgs[:, cc, 0:Tt], reads=[f"zgs{cc}"], writes=[f"zg_d{cc}"])
        pb, pkk = bank()
        for kc in range(8):
            S.op("pe", lambda e, kc=kc, pb=pb: e.matmul(pb[0:8, 0:Tt], lhsT=wbf[:, kc, 3072:3080], rhs=xT[:, kc, 0:Tt],
                                                        start=(kc == 0), stop=(kc == 7)),
                 reads=[wkeys[kc], xTk[kc]], writes=[pkk])
        S.op("act", lambda e, pb=pb: e.activation(out=lg[0:8, 0:Tt], in_=pb[0:8, 0:Tt], func=AF.Copy), reads=[pkk], writes=["lg"])
        for w in range(2):
            for h in range(4):
                S.dma("pool", gb_d[h, w:w + 1, t0:t0 + Tt], lg[w * 4 + h:w * 4 + h + 1, 0:Tt], reads=["lg"], writes=[f"gb_d{w}{h}"])

    def conv(it):
        ti, Tt = tiles[it]
        for cc in range(4):
            pb, pkk = bank()
            for j in range(CW):
                S.op("pe", lambda e, cc=cc, j=j, pb=pb: e.matmul(pb[:, 0:Tt], lhsT=dgw[:, cc, j, :], rhs=U[:, cc, j:j + Tt],
                                                                 start=(j == 0), stop=(j == CW - 1)),
                     reads=[f"dgw{cc}_pool", f"dgw{cc}_dve", f"U{cc}"], writes=[pkk])
            S.op("act", lambda e, cc=cc, pb=pb: e.activation(out=acc[:, cc, 0:Tt], in_=pb[:, 0:Tt], func=AF.Identity,
                                                             bias=pv[:, PV_DWB + cc:PV_DWB + cc + 1]),
                 reads=[pkk, "pv"], writes=[f"acc{cc}"])
            S.op("pool", lambda e, cc=cc: e.tensor_copy(out=U[:, cc, 0:30], in_=U[:, cc, Tt:Tt + 30]),
                 reads=[f"U{cc}"], writes=[f"U{cc}"])

    def shortconv(it):
        ti, Tt = tiles[it]
        t0 = ti * T
        for j in range(12):
            which, h = j // 4, j % 4
            qb, qk = qa[j], f"qa{j}"
            S.op("dve", lambda e, j=j, qb=qb: e.tensor_scalar(out=qb[:, 0:Tt], in0=PRE[:, j, 0:Tt],
                                                              scalar1=pv[:, PV_SCW + j * 4:PV_SCW + j * 4 + 1], scalar2=None,
                                                              op0=ALU.mult),
                 reads=[f"PRE{j}", "pv"], writes=[qk])
            for tp in range(1, 4):
                S.op("dve", lambda e, j=j, tp=tp, qb=qb: e.scalar_tensor_tensor(
                    out=qb[:, 0:Tt], in0=PRE[:, j, tp:tp + Tt], scalar=pv[:, PV_SCW + j * 4 + tp:PV_SCW + j * 4 + tp + 1],
                    in1=qb[:, 0:Tt], op0=ALU.mult, op1=ALU.add),
                     reads=[f"PRE{j}", qk], writes=[qk])
            S.op("pool", lambda e, j=j: e.tensor_copy(out=PRE[:, j, 0:3], in_=PRE[:, j, Tt:Tt + 3]),
                 reads=[f"PRE{j}"], writes=[f"PRE{j}"])
            S.op("act", lambda e, qb=qb: e.activation(out=qb[:, 0:Tt], in_=qb[:, 0:Tt], func=AF.Silu), reads=[qk], writes=[qk])
            if which == 2:
                S.dma("pool", qkv_d[h, which, :, t0:t0 + Tt], qb[:, 0:Tt], reads=[qk], writes=[f"qkv_d{j}"])

    def finish(it):
        ti, Tt = tiles[it]
        t0 = ti * T
        pm, pmk = bank()
        pq, pqk = bank()
        for cc in range(4):
            S.op("pe", lambda e, cc=cc: e.matmul(pm[:, 0:Tt], lhsT=o512[:], rhs=acc[:, cc, 0:Tt], start=(cc == 0), stop=(cc == 3)),
                 reads=["o512", f"acc{cc}"], writes=[pmk])
        for cc in range(4):
            sb_, sk = sq[cc % 2], f"sq{cc % 2}"
            S.op("act", lambda e, cc=cc, sb_=sb_: e.activation(out=sb_[:, 0:Tt], in_=acc[:, cc, 0:Tt], func=AF.Square),
                 reads=[f"acc{cc}"], writes=[sk])
            S.op("pe", lambda e, cc=cc, sb_=sb_: e.matmul(pq[:, 0:Tt], lhsT=o512[:], rhs=sb_[:, 0:Tt], start=(cc == 0), stop=(cc == 3)),
                 reads=["o512", sk], writes=[pqk])
        S.op("act", lambda e: e.activation(out=mean[:, 0:Tt], in_=pm[:, 0:Tt], func=AF.Copy), reads=[pmk], writes=["mean"])
        S.op("dve", lambda e: e.tensor_tensor(out=rstd[:, 0:Tt], in0=mean[:, 0:Tt], in1=mean[:, 0:Tt], op=ALU.mult),
             reads=["mean"], writes=["rstd"])
        S.op("dve", lambda e: e.tensor_tensor(out=rstd[:, 0:Tt], in0=pq[:, 0:Tt], in1=rstd[:, 0:Tt], op=ALU.subtract),
             reads=[pqk, "rstd"], writes=["rstd"])
        S.op("dve", lambda e: e.tensor_scalar(out=rstd[:, 0:Tt], in0=rstd[:, 0:Tt], scalar1=0.0, scalar2=LN_EPS,
                                              op0=ALU.max, op1=ALU.add), reads=["rstd"], writes=["rstd"])
        S.op("act", lambda e: e.activation(out=rstd[:, 0:Tt], in_=rstd[:, 0:Tt], func=AF.Sqrt), reads=["rstd"], writes=["rstd"])
        S.op("dve", lambda e: e.reciprocal(out=rstd[:, 0:Tt], in_=rstd[:, 0:Tt]), reads=["rstd"], writes=["rstd"])
        for cc in range(4):
            S.op("dve", lambda e, cc=cc: e.tensor_tensor(out=acc[:, cc, 0:Tt], in0=acc[:, cc, 0:Tt], in1=mean[:, 0:Tt], op=ALU.subtract),
                 reads=[f"acc{cc}", "mean"], writes=[f"acc{cc}"])
            S.op("dve", lambda e, cc=cc: e.tensor_tensor(out=acc[:, cc, 0:Tt], in0=acc[:, cc, 0:Tt], in1=rstd[:, 0:Tt], op=ALU.mult),
                 reads=[f"acc{cc}", "rstd"], writes=[f"acc{cc}"])
            S.op("act", lambda e, cc=cc: e.activation(out=ycs[:, cc, 0:Tt], in_=acc[:, cc, 0:Tt], func=AF.Silu,
                                                      scale=pv[:, PV_CLG + cc:PV_CLG + cc + 1],
                                                      bias=pv[:, PV_CLB + cc:PV_CLB + cc + 1]),
                 reads=[f"acc{cc}", "pv"], writes=[f"ycs{cc}"])
            S.dma("pool", yc_d[cc * 128:(cc + 1) * 128, t0:t0 + Tt], ycs[:, cc, 0:Tt], reads=[f"ycs{cc}"], writes=[f"yc_d{cc}"])
        for j in range(8):
            which, h = j // 4, j % 4
            qb, qk = qa[j], f"qa{j}"
            sb_, sk = sq[j % 2], f"sq{j % 2}"
            pb, pkk = bank()
            S.op("act", lambda e, qb=qb, sb_=sb_: e.activation(out=sb_[:, 0:Tt], in_=qb[:, 0:Tt], func=AF.Square), reads=[qk], writes=[sk])
            S.op("pe", lambda e, pb=pb, sb_=sb_: e.matmul(pb[:, 0:Tt], lhsT=c["ones"][:], rhs=sb_[:, 0:Tt], start=True, stop=True),
                 reads=["c_ones", sk], writes=[pkk])
            sc = 128.0 if which == 0 else 1.0
            S.op("dve", lambda e, pb=pb, sc=sc, sb_=sb_: e.tensor_scalar(out=sb_[:, 0:Tt], in0=pb[:, 0:Tt], scalar1=L2_EPS, scalar2=sc,
                                                                         op0=ALU.add, op1=ALU.mult), reads=[pkk], writes=[sk])
            S.op("act", lambda e, sb_=sb_: e.activation(out=sb_[:, 0:Tt], in_=sb_[:, 0:Tt], func=AF.Sqrt), reads=[sk], writes=[sk])
            S.op("dve", lambda e, sb_=sb_: e.reciprocal(out=sb_[:, 0:Tt], in_=sb_[:, 0:Tt]), reads=[sk], writes=[sk])
            S.op("dve", lambda e, qb=qb, sb_=sb_: e.tensor_tensor(out=qb[:, 0:Tt], in0=qb[:, 0:Tt], in1=sb_[:, 0:Tt], op=ALU.mult),
                 reads=[qk, sk], writes=[qk])
            S.dma("pool", qkv_d[h, which, :, t0:t0 + Tt], qb[:, 0:Tt], reads=[qk], writes=[f"qkv_d{j}"])

    stage1(0)
    stage1(1)
    for it in range(1, len(tiles)):
        conv(it)
        shortconv(it)
        if it + 1 < len(tiles):
            stage1(it + 1)
        finish(it)
    cx.pop()


def phase_b(cx, S_core, qkv_r, gb_r, pv_d, oc_d, st_d):
    nc, S, c = cx.nc, cx.S, cx.c
    ident, ones = c["ident"], c["ones"]
    cx.push()
    NB = 4 * S_core // 128
    NCH = 2 * NB
    rps128 = S_core // 128
    rps64 = S_core // 64
    pv = cx.sb("pvb", [128, NPV])
    S.dma("sp", pv[:], pv_d[:, :], writes=["pv"])
    triBD = cx.sb("triBD", [128, 128])
    maskpos = cx.sb("maskpos", [128, 128])
    strict = cx.sb("strict", [128, 128])
    selL = cx.sb("selL", [128, 128])
    sel63 = cx.sb("sel63", [128, 128])
    negexpa = cx.sb("negexpa", [128, 3])
    S.op("pool", lambda e: e.memset(triBD[:], 1.0), writes=["triBD"])
    S.op("pool", lambda e: e.affine_select(out=triBD[:], in_=triBD[:], pattern=[[1, 128]], compare_op=ALU.is_ge,
                                           fill=0.0, base=0, channel_multiplier=-1), reads=["triBD"], writes=["triBD"])
    S.op("pool", lambda e: e.memset(triBD[0:64, 64:128], 0.0), reads=["triBD"], writes=["triBD"])
    S.op("pool", lambda e: e.memset(maskpos[:], 0.0), writes=["maskpos"])
    S.op("pool", lambda e: e.affine_select(out=maskpos[:], in_=maskpos[:], pattern=[[-1, 128]], compare_op=ALU.is_ge,
                                           fill=1e9, base=0, channel_multiplier=1), reads=["maskpos"], writes=["maskpos"])
    S.op("pool", lambda e: e.memset(maskpos[64:128, 0:64], 1e9), reads=["maskpos"], writes=["maskpos"])
    S.op("pool", lambda e: e.memset(strict[:], 1.0), writes=["strict"])
    S.op("pool", lambda e: e.affine_select(out=strict[:], in_=strict[:], pattern=[[-1, 128]], compare_op=ALU.is_ge,
                                           fill=0.0, base=-1, channel_multiplier=1), reads=["strict"], writes=["strict"])
    S.op("pool", lambda e: e.memset(strict[64:128, 0:64], 0.0), reads=["strict"], writes=["strict"])
    S.op("pool", lambda e: e.memset(selL[:], 1.0), writes=["selL"])
    S.op("pool", lambda e: e.affine_select(out=selL[:], in_=selL[:], pattern=[[-64, 2], [0, 64]], compare_op=ALU.is_equal,
                                           fill=0.0, base=-63, channel_multiplier=1), reads=["selL"], writes=["selL"])
    S.op("pool", lambda e: e.memset(sel63[:], 1.0), writes=["sel63"])
    S.op("pool", lambda e: e.affine_select(out=sel63[:], in_=sel63[:], pattern=[[0, 128]], compare_op=ALU.is_equal,
                                           fill=0.0, base=-63, channel_multiplier=1), reads=["sel63"], writes=["sel63"])
    S.op("act", lambda e: e.activation(out=negexpa[:], in_=pv[:, PV_ALOG128:PV_ALOG128 + 3], func=AF.Exp), reads=["pv"], writes=["nea"])
    S.op("dve", lambda e: e.tensor_scalar(out=negexpa[:], in0=negexpa[:], scalar1=-1.0, scalar2=None, op0=ALU.mult),
         reads=["nea"], writes=["nea"])
    ps, pk = cx.ps, cx.psk

    def load_rows(w, width, rps, nrows, name):
        ntile = (nrows + 127) // 128
        tl = [cx.sb(f"{name}{i}", [128, width]) for i in range(ntile)]
        for seg in range(NSEG):
            r = seg * rps
            S.dma("sp", tl[r // 128][r % 128:r % 128 + rps, :], gb_r[seg, w, :].rearrange("(r t) -> r t", t=width),
                  writes=[f"{name}{r // 128}"])
        return tl, ntile

    def gbeta_rows(tl_b, tl_a, ntile, nrows, width, name, c0):
        tmp = cx.sb(f"{name}_tmp", [128, width])
        tmp2 = cx.sb(f"{name}_tmp2", [128, width])
        for i in range(ntile):
            n_p = min(128, nrows - i * 128)
            kb, ka = f"{name}b{i}", f"{name}a{i}"
            S.op("act", lambda e, i=i: e.activation(out=tl_b[i][0:n_p, :], in_=tl_b[i][0:n_p, :], func=AF.Sigmoid), reads=[kb], writes=[kb])
            S.op("dve", lambda e, i=i: e.tensor_scalar(out=tl_a[i][0:n_p, :], in0=tl_a[i][0:n_p, :], scalar1=pv[0:n_p, PV_DTB128 + c0 + i:PV_DTB128 + c0 + i + 1],
                                                       scalar2=None, op0=ALU.add), reads=[ka, "pv"], writes=[ka])
            S.op("act", lambda e, i=i: e.activation(out=tmp[0:n_p, :], in_=tl_a[i][0:n_p, :], func=AF.Abs),
                 reads=[ka], writes=[f"{name}tmp"])
            S.op("act", lambda e: e.activation(out=tmp[0:n_p, :], in_=tmp[0:n_p, :], func=AF.Exp, scale=-1.0), reads=[f"{name}tmp"], writes=[f"{name}tmp"])
            S.op("act", lambda e: e.activation(out=tmp[0:n_p, :], in_=tmp[0:n_p, :], func=AF.Ln, bias=1.0), reads=[f"{name}tmp"], writes=[f"{name}tmp"])
            S.op("dve", lambda e, i=i: e.tensor_scalar(out=tmp2[0:n_p, :], in0=tl_a[i][0:n_p, :], scalar1=0.0, scalar2=None, op0=ALU.max),
                 reads=[ka], writes=[f"{name}tmp2"])
            S.op("dve", lambda e: e.tensor_tensor(out=tmp[0:n_p, :], in0=tmp[0:n_p, :], in1=tmp2[0:n_p, :], op=ALU.add),
                 reads=[f"{name}tmp", f"{name}tmp2"], writes=[f"{name}tmp"])
            S.op("dve", lambda e, i=i: e.tensor_scalar(out=tl_a[i][0:n_p, :], in0=tmp[0:n_p, :], scalar1=negexpa[0:n_p, c0 + i:c0 + i + 1], scalar2=None, op0=ALU.mult),
                 reads=[f"{name}tmp", "nea"], writes=[ka])

    def transpose_rows(tl, ntile, nrows, width, dst, dkey, key):
        for i in range(ntile):
            n_p = min(128, nrows - i * 128)
            S.op("pe", lambda e, i=i: e.transpose(out=ps[6][0:width, 0:n_p], in_=tl[i][0:n_p, :], identity=ident[0:n_p, 0:n_p]),
                 reads=[f"{key}{i}", "c_ident"], writes=[pk[6]])
            S.op("dve", lambda e, i=i: e.tensor_copy(out=dst[0:width, i * 128:i * 128 + n_p], in_=ps[6][0:width, 0:n_p]),
                 reads=[pk[6]], writes=[dkey])

    b128r, nt128 = load_rows(0, 128, rps128, NB, "r128b")
    a128r, _ = load_rows(1, 128, rps128, NB, "r128a")
    b64r, nt64 = load_rows(0, 64, rps64, NCH, "r64b")
    a64r, _ = load_rows(1, 64, rps64, NCH, "r64a")
    gbeta_rows(b128r, a128r, nt128, NB, 128, "r128", 0)
    gbeta_rows(b64r, a64r, nt64, NCH, 64, "r64", 1)
    beta128 = cx.sb("beta128", [128, NB])
    g128 = cx.sb("g128", [128, NB])
    gc128 = cx.sb("gc128", [128, NB])
    egc128 = cx.sb("egc128", [128, NB])
    nbeta128 = cx.sb("nbeta128", [128, NB])
    beg128 = cx.sb("beg128", [128, NB])
    g64 = cx.sb("g64", [64, NCH])
    gc64 = cx.sb("gc64", [64, NCH])
    kdsc64 = cx.sb("kdsc64", [64, NCH])
    egl = cx.sb("egl", [128, NCH])
    transpose_rows(b128r, nt128, NB, 128, beta128, "beta128", "r128b")
    transpose_rows(a128r, nt128, NB, 128, g128, "g128", "r128a")
    transpose_rows(a64r, nt64, NCH, 64, g64, "g64", "r64a")
    S.op("pe", lambda e: e.matmul(ps[6][:, 0:NB], lhsT=triBD[:], rhs=g128[:, :], start=True, stop=True), reads=["triBD", "g128"], writes=[pk[6]])
    S.op("dve", lambda e: e.tensor_copy(out=gc128[:, :], in_=ps[6][:, 0:NB]), reads=[pk[6]], writes=["gc128"])
    S.op("act", lambda e: e.activation(out=egc128[:, :], in_=gc128[:, :], func=AF.Exp), reads=["gc128"], writes=["egc128"])
    S.op("dve", lambda e: e.tensor_scalar(out=nbeta128[:, :], in0=beta128[:, :], scalar1=-1.0, scalar2=None, op0=ALU.mult),
         reads=["beta128"], writes=["nbeta128"])
    S.op("dve", lambda e: e.tensor_tensor(out=beg128[:, :], in0=beta128[:, :], in1=egc128[:, :], op=ALU.mult),
         reads=["beta128", "egc128"], writes=["beg128"])
    S.op("pe", lambda e: e.matmul(ps[7][0:64, 0:NCH], lhsT=triBD[0:64, 0:64], rhs=g64[:, :], start=True, stop=True),
         reads=["triBD", "g64"], writes=[pk[7]])
    S.op("dve", lambda e: e.tensor_copy(out=gc64[:, :], in_=ps[7][0:64, 0:NCH]), reads=[pk[7]], writes=["gc64"])
    S.op("pe", lambda e: e.matmul(ps[6][0:64, 0:NCH], lhsT=sel63[0:64, 0:64], rhs=gc64[:, :], start=True, stop=True),
         reads=["sel63", "gc64"], writes=[pk[6]])
    S.op("dve", lambda e: e.tensor_tensor(out=kdsc64[:, :], in0=ps[6][0:64, 0:NCH], in1=gc64[:, :], op=ALU.subtract),
         reads=[pk[6], "gc64"], writes=["kdsc64"])
    S.op("act", lambda e: e.activation(out=kdsc64[:, :], in_=kdsc64[:, :], func=AF.Exp), reads=["kdsc64"], writes=["kdsc64"])
    S.op("pe", lambda e: e.matmul(ps[7][:, 0:NCH], lhsT=sel63[0:64, :], rhs=gc64[:, :], start=True, stop=True),
         reads=["sel63", "gc64"], writes=[pk[7]])
    S.op("act", lambda e: e.activation(out=egl[:, :], in_=ps[7][:, 0:NCH], func=AF.Exp), reads=[pk[7]], writes=["egl"])

    NBh = S_core // 128
    GRP = min(4, NBh)
    qkv = [cx.sb(f"qkv{i}", [128, 3, GRP * 128]) for i in range(2)]
    oc = [cx.sb(f"oc{i}", [128, 2, GRP * 128]) for i in range(2)]
    St = [[cx.sb(f"St{h}_{i}", [128, 256]) for i in range(2)] for h in range(4)]
    sidx = [0, 0, 0, 0]
    for h in range(4):
        S.op("pool", lambda e, h=h: e.memset(St[h][0][:, 0:128], 0.0), writes=[f"St{h}_0"])
        S.op("pool", lambda e, h=h: e.tensor_copy(out=St[h][0][:, 128:256], in_=ident[:]), reads=["c_ident", f"St{h}_0"], writes=[f"St{h}_0"])
    W = {}
    for par in range(2):
        for b in range(GRP):
            for nm, shp in [("kbg", [128, 128]), ("vb", [128, 128]), ("kdec", [64, 256]), ("dg", [128, 256]), ("tmp", [128, 128]),
                            ("Dm", [128, 128]), ("Ds", [128, 128]), ("TT", [128, 128]), ("Pf", [128, 128]),
                            ("attn", [128, 128]), ("attnT", [64, 256]), ("qg", [128, 128]), ("u", [64, 2, 256]), ("wT", [128, 128])]:
                W[(nm, b, par)] = cx.sb(f"{nm}{b}_{par}", shp)
            for nm in ("P", "PT", "TTb"):
                W[(nm, b, par)] = cx.sb(f"{nm}{b}_{par}", [128, 128], BF16)
            S.op("pool", lambda e, b=b, par=par: e.memset(W[("u", b, par)][:, :, :], 0.0), writes=[f"u{b}_{par}"])
    vnew = [cx.sb(f"vnew{i}", [64, 256]) for i in range(2)]
    pctr = [0]

    def bank():
        i = pctr[0] % 5
        pctr[0] += 1
        return ps[i], pk[i]

    groups = [(g, h) for g in range(NBh // GRP) for h in range(4)]
    blocks = list(range(GRP))

    def prepass(gi):
        g, h = groups[gi]
        par = gi % 2
        st = g * GRP * 128
        qb = qkv[par]
        qk = f"qkv{par}"
        K = lambda nm, b: f"{nm}{b}_{par}"
        Wp = lambda nm, b: W[(nm, b, par)]
        bk = {}
        stages = []


        def s_p1():
            for b in blocks:
                n = h * NBh + g * GRP + b
                cs = slice(b * 128, (b + 1) * 128)
                pb, pkk = bank()
                S.op("pe", lambda e, pb=pb, cs=cs: e.transpose(out=pb[:, 0:128], in_=qb[:, 1, cs], identity=ident[:]), reads=[qk, "c_ident"], writes=[pkk])
                S.op("pe", lambda e, pb=pb, cs=cs: e.transpose(out=pb[:, 128:256], in_=qb[:, 2, cs], identity=ident[:]), reads=[qk, "c_ident"], writes=[pkk])
                S.op("pe", lambda e, pb=pb, b=b: e.transpose(out=pb[0:64, 256:384], in_=qb[:, 1, b * 128 + 64:b * 128 + 128], identity=ident[:]),
                     reads=[qk, "c_ident"], writes=[pkk])
                S.op("act", lambda e, pb=pb, b=b, n=n: e.activation(out=Wp("kbg", b)[:], in_=pb[:, 0:128], func=AF.Copy, scale=beg128[:, n:n + 1]),
                     reads=[pkk, "beg128"], writes=[K("kbg", b)])
                S.op("dve", lambda e, pb=pb, b=b, n=n: e.tensor_scalar(out=Wp("vb", b)[:], in0=pb[:, 128:256], scalar1=beta128[:, n:n + 1], scalar2=None, op0=ALU.mult),
                     reads=[pkk, "beta128"], writes=[K("vb", b)])
                S.op("dve", lambda e, pb=pb, b=b, n=n: e.tensor_scalar(out=Wp("kdec", b)[:, 0:128], in0=pb[0:64, 0:128], scalar1=kdsc64[:, 2 * n:2 * n + 1], scalar2=None, op0=ALU.mult),
                     reads=[pkk, "kdsc64"], writes=[K("kdec", b)])
                S.op("act", lambda e, pb=pb, b=b, n=n: e.activation(out=Wp("kdec", b)[:, 128:256], in_=pb[0:64, 256:384], func=AF.Copy, scale=kdsc64[:, 2 * n + 1:2 * n + 2]),
                     reads=[pkk, "kdsc64"], writes=[K("kdec", b) + "b"])
                S.op("pool", lambda e, b=b, n=n: e.tensor_scalar(out=Wp("dg", b)[:, 0:128], in0=ident[:], scalar1=gc128[:, n:n + 1], scalar2=None, op0=ALU.mult),
                     reads=["c_ident", "gc128"], writes=[K("dg", b)])
                S.op("pool", lambda e, b=b, n=n: e.tensor_scalar(out=Wp("dg", b)[:, 128:256], in0=ident[:], scalar1=egc128[:, n:n + 1], scalar2=None, op0=ALU.mult),
                     reads=["c_ident", "egc128"], writes=[K("dg", b)])
        stages.append(s_p1)

        def s_p2():
            for b in blocks:
                cs = slice(b * 128, (b + 1) * 128)
                pb, pkk = bank()
                bk[b] = (pb, pkk)
                S.op("pe", lambda e, pb=pb, cs=cs: e.matmul(pb[:, 0:128], lhsT=qb[:, 1, cs], rhs=qb[:, 1, cs], start=True, stop=True), reads=[qk], writes=[pkk])
                S.op("pe", lambda e, pb=pb, cs=cs: e.matmul(pb[:, 128:256], lhsT=qb[:, 0, cs], rhs=qb[:, 1, cs], start=True, stop=True), reads=[qk], writes=[pkk])
                S.op("pe", lambda e, pb=pb, b=b: e.matmul(pb[:, 256:512], lhsT=ones[:], rhs=Wp("dg", b)[:, :], start=True, stop=True),
                     reads=["c_ones", K("dg", b)], writes=[pkk])
        stages.append(s_p2)

        def s_p3():
            for b in blocks:
                n = h * NBh + g * GRP + b
                cs = slice(b * 128, (b + 1) * 128)
                pb, pkk = bk[b]
                S.op("dve", lambda e, pb=pb, b=b, n=n: e.scalar_tensor_tensor(out=Wp("tmp", b)[:], in0=pb[:, 256:384], scalar=gc128[:, n:n + 1],
                                                                              in1=maskpos[:], op0=ALU.subtract, op1=ALU.add),
                     reads=[pkk, "gc128", "maskpos"], writes=[K("tmp", b)])
                S.op("act", lambda e, b=b: e.activation(out=Wp("Dm", b)[:], in_=Wp("tmp", b)[:], func=AF.Exp, scale=-1.0),
                     reads=[K("tmp", b)], writes=[K("Dm", b)])
                S.op("pool", lambda e, b=b: e.tensor_tensor(out=Wp("Ds", b)[:], in0=Wp("Dm", b)[:], in1=strict[:], op=ALU.mult),
                     reads=[K("Dm", b), "strict"], writes=[K("Ds", b)])
                S.op("dve", lambda e, pb=pb, b=b, n=n: e.scalar_tensor_tensor(out=Wp("Pf", b)[:], in0=pb[:, 0:128], scalar=nbeta128[:, n:n + 1],
                                                                              in1=Wp("Ds", b)[:], op0=ALU.mult, op1=ALU.mult),
                     reads=[pkk, "nbeta128", K("Ds", b)], writes=[K("Pf", b)])
                S.op("pool", lambda e, b=b: e.tensor_copy(out=Wp("P", b)[:], in_=Wp("Pf", b)[:]), reads=[K("Pf", b)], writes=[K("P", b)])
                S.op("dve", lambda e, pb=pb, b=b: e.tensor_tensor(out=Wp("attn", b)[:], in0=pb[:, 128:256], in1=Wp("Dm", b)[:], op=ALU.mult),
                     reads=[pkk, K("Dm", b)], writes=[K("attn", b)])
                S.op("dve", lambda e, pb=pb, b=b, cs=cs: e.tensor_tensor(out=Wp("qg", b)[:], in0=qb[:, 0, cs], in1=pb[:, 384:512], op=ALU.mult),
                     reads=[pkk, qk], writes=[K("qg", b)])
        stages.append(s_p3)

        def s_p4():
            for b in blocks:
                pb, pkk = bank()
                S.op("pe", lambda e, pb=pb, b=b: e.transpose(out=pb[:, 0:128], in_=Wp("Pf", b)[:], identity=ident[:]), reads=[K("Pf", b), "c_ident"], writes=[pkk])
                S.op("pe", lambda e, pb=pb, b=b: e.transpose(out=pb[0:64, 128:256], in_=Wp("attn", b)[:, 0:64], identity=ident[:]), reads=[K("attn", b), "c_ident"], writes=[pkk])
                S.op("pe", lambda e, pb=pb, b=b: e.transpose(out=pb[0:64, 256:384], in_=Wp("attn", b)[:, 64:128], identity=ident[:]), reads=[K("attn", b), "c_ident"], writes=[pkk])
                S.op("act", lambda e, pb=pb, b=b: e.activation(out=Wp("PT", b)[:], in_=pb[:, 0:128], func=AF.Copy), reads=[pkk], writes=[K("PT", b)])
                S.op("dve", lambda e, pb=pb, b=b: e.tensor_tensor(out=Wp("TT", b)[:], in0=pb[:, 0:128], in1=ident[:], op=ALU.add),
                     reads=[pkk, "c_ident"], writes=[K("TT", b)])
                S.op("pool", lambda e, b=b: e.tensor_copy(out=Wp("TTb", b)[:], in_=Wp("TT", b)[:]), reads=[K("TT", b)], writes=[K("TTb", b)])
                S.op("act", lambda e, pb=pb, b=b: e.activation(out=Wp("attnT", b)[:, :], in_=pb[0:64, 128:384], func=AF.Copy), reads=[pkk], writes=[K("attnT", b)])
        stages.append(s_p4)

        for lvl in range(1, 6):
            def s_sq(lvl=lvl):
                for b in blocks:
                    pb, pkk = bank()
                    bk[b] = (pb, pkk)
                    S.op("pe", lambda e, pb=pb, b=b: e.matmul(pb[:, 0:128], lhsT=Wp("PT", b)[:], rhs=Wp("P", b)[:], start=True, stop=True),
                         reads=[K("PT", b), K("P", b)], writes=[pkk])
                    if lvl < 5:
                        S.op("pe", lambda e, pb=pb, b=b: e.matmul(pb[:, 128:256], lhsT=Wp("P", b)[:], rhs=Wp("PT", b)[:], start=True, stop=True),
                             reads=[K("PT", b), K("P", b)], writes=[pkk])
                for b in blocks:
                    pb, pkk = bk[b]
                    S.op("act", lambda e, pb=pb, b=b: e.activation(out=Wp("P", b)[:], in_=pb[:, 0:128], func=AF.Copy), reads=[pkk], writes=[K("P", b)])
                    if lvl < 5:
                        S.op("dve", lambda e, pb=pb, b=b: e.tensor_copy(out=Wp("PT", b)[:], in_=pb[:, 128:256]), reads=[pkk], writes=[K("PT", b)])
            stages.append(s_sq)

            def s_tt(lvl=lvl):
                for b in blocks:
                    pb, pkk = bank()
                    bk[b] = (pb, pkk)
                    S.op("pe", lambda e, pb=pb, b=b: e.matmul(pb[:, 0:128], lhsT=Wp("P", b)[:], rhs=Wp("TTb", b)[:], start=True, stop=True),
                         reads=[K("P", b), K("TTb", b)], writes=[pkk])
                for b in blocks:
                    pb, pkk = bk[b]
                    S.op("dve", lambda e, pb=pb, b=b: e.tensor_tensor(out=Wp("TT", b)[:], in0=Wp("TT", b)[:], in1=pb[:, 0:128], op=ALU.add),
                         reads=[pkk, K("TT", b)], writes=[K("TT", b)])
                    if lvl < 5:
                        S.op("pool", lambda e, b=b: e.tensor_copy(out=Wp("TTb", b)[:], in_=Wp("TT", b)[:]), reads=[K("TT", b)], writes=[K("TTb", b)])
            stages.append(s_tt)

        def s_p6():
            for b in blocks:
                pb, pkk = bank()
                S.op("pe", lambda e, pb=pb, b=b: e.matmul(pb[0:64, 0:128], lhsT=Wp("TT", b)[:, 0:64], rhs=Wp("vb", b)[:], start=True, stop=True),
                     reads=[K("TT", b), K("vb", b)], writes=[pkk])
                S.op("pe", lambda e, pb=pb, b=b: e.matmul(pb[0:64, 128:256], lhsT=Wp("TT", b)[:, 64:128], rhs=Wp("vb", b)[:], start=True, stop=True),
                     reads=[K("TT", b), K("vb", b)], writes=[pkk])
                S.op("pe", lambda e, pb=pb, b=b: e.matmul(pb[:, 256:384], lhsT=Wp("kbg", b)[:], rhs=Wp("TT", b)[:], start=True, stop=True),
                     reads=[K("TT", b), K("kbg", b)], writes=[pkk])
                S.op("act", lambda e, pb=pb, b=b: e.activation(out=Wp("u", b)[:, :, 0:128], in_=pb[0:64, 0:256].rearrange("p (c e) -> p c e", e=128), func=AF.Copy),
                     reads=[pkk], writes=[K("u", b)])
                S.op("dve", lambda e, pb=pb, b=b: e.tensor_copy(out=Wp("wT", b)[:], in_=pb[:, 256:384]), reads=[pkk], writes=[K("wT", b)])
        stages.append(s_p6)
        return stages

    def seqsteps(gi):
        g, h = groups[gi]
        par = gi % 2
        st = g * GRP * 128
        ob = oc[par]
        ok = f"oc{par}"
        K = lambda nm, b: f"{nm}{b}_{par}"
        Wp = lambda nm, b: W[(nm, b, par)]
        steps = []
        for b in blocks:
            for ch in range(2):
                def s_chunk(b=b, ch=ch):
                    n = h * NBh + g * GRP + b
                    ci = 2 * n + ch
                    si = sidx[h]
                    Sc, Sn = St[h][si], St[h][1 - si]
                    Sck, Snk = f"St{h}_{si}", f"St{h}_{1 - si}"
                    vb_, vk = vnew[ci % 2], f"vnew{ci % 2}"
                    S.op("pe", lambda e: e.matmul(ps[5][0:64, 0:256], lhsT=Wp("wT", b)[:, ch * 64:(ch + 1) * 64], rhs=Sc[:, :], start=True, stop=True),
                         reads=[K("wT", b), Sck], writes=[pk[5]])
                    S.op("dve", lambda e: e.tensor_tensor(out=vb_[:, :], in0=Wp("u", b)[:, ch, :], in1=ps[5][0:64, 0:256], op=ALU.subtract),
                         reads=[K("u", b), pk[5]], writes=[vk])
                    for part in range(2):
                        S.op("pe", lambda e, part=part: e.matmul(ps[6][:, part * 64:part * 64 + 64], lhsT=Sc[:, part * 128:(part + 1) * 128],
                                                                 rhs=Wp("qg", b)[:, ch * 64:(ch + 1) * 64], start=True, stop=False),
                             reads=[Sck, K("qg", b)], writes=[pk[6]])
                        S.op("pe", lambda e, part=part: e.matmul(ps[6][:, part * 64:part * 64 + 64], lhsT=vb_[:, part * 128:(part + 1) * 128],
                                                                 rhs=Wp("attnT", b)[:, ch * 128 + ch * 64:ch * 128 + ch * 64 + 64],
                                                                 start=False, stop=True),
                             reads=[vk, K("attnT", b)], writes=[pk[6]])
                    S.op("act", lambda e: e.activation(out=ob[:, :, b * 128 + ch * 64:b * 128 + ch * 64 + 64],
                                                       in_=ps[6][:, 0:128].rearrange("p (w c) -> p w c", c=64), func=AF.Copy),
                         reads=[pk[6]], writes=[ok])
                    S.op("pe", lambda e: e.matmul(ps[7][:, 0:256], lhsT=Wp("kdec", b)[:, ch * 128:(ch + 1) * 128], rhs=vb_[:, :], start=True, stop=True),
                         reads=[K("kdec", b), K("kdec", b) + "b", vk], writes=[pk[7]])
                    S.op("dve", lambda e: e.scalar_tensor_tensor(out=Sn[:, :], in0=Sc[:, :], scalar=egl[:, ci:ci + 1], in1=ps[7][:, 0:256],
                                                                 op0=ALU.mult, op1=ALU.add),
                         reads=[Sck, "egl", pk[7]], writes=[Snk])
                    sidx[h] = 1 - si
                steps.append(s_chunk)

        def s_out():
            S.dma("pool", oc_d[:, h * 128:(h + 1) * 128, st:st + GRP * 128].rearrange("w e t -> e w t"), ob[:, :, :], reads=[ok], writes=[f"oc_d{par}"])
        steps.append(s_out)
        return steps

    def qload(gi):
        g, h = groups[gi]
        st = g * GRP * 128
        S.dma("sp", qkv[gi % 2][:, :, :], qkv_r[h, :, :, st:st + GRP * 128].rearrange("w d t -> d w t"), writes=[f"qkv{gi % 2}"])

    qload(0)
    if len(groups) > 1:
        qload(1)
    for f in prepass(0):
        f()
    for gi in range(len(groups)):
        nxt = prepass(gi + 1) if gi + 1 < len(groups) else []
        if nxt:
            if gi + 2 < len(groups):
                nxt.insert(3, lambda gi=gi: qload(gi + 2))
        seq = seqsteps(gi)
        n_n, n_s = len(nxt), len(seq)
        si_ = 0
        for k in range(n_n):
            nxt[k]()
            while si_ < n_s and (si_ + 1) * n_n <= (k + 1) * n_s:
                seq[si_]()
                si_ += 1
        while si_ < n_s:
            seq[si_]()
            si_ += 1
    for h in range(4):
        S.dma("sp", st_d[h, :, :], St[h][sidx[h]][:, :], reads=[f"St{h}_{sidx[h]}"], writes=[f"st_d{h}"])
    cx.pop()


def phase_c(cx, S_core, oc_d, yc_d, zg_d, xin, wout, lnrows, pv_d, ffn, xout, sin, scr=None):
    nc, S, c = cx.nc, cx.S, cx.c
    ident, ones = c["ident"], c["ones"]
    ps, pk = cx.ps, cx.psk
    moe = ffn["moe"]
    G = S_core if moe else min(1024, S_core)
    T = min(512, G)
    nsubG = G // 128
    NT = 2 * S_core // 512 + NEXP - 1
    nexp = NEXP if moe else 1
    dff = DFF_EXP if moe else DFF_DENSE
    nfc = dff // 128
    FG = 4
    fgroups = [(f0, min(FG, nfc - f0)) for f0 in range(0, nfc, FG)]
    cx.push()
    pv = cx.sb("pvc", [128, NPV])
    S.dma("sp", pv[:], pv_d[:, :], writes=["pv"])
    o128 = cx.sb("o128", [128, 128])
    S.op("pool", lambda e: e.memset(o128[:], 1.0 / 128.0), writes=["o128"])
    lnp = cx.sb("lnp", [128, 4, D])
    for i in range(4):
        S.dma("sp", lnp[:, i, :], lnrows[i, :, :], writes=[f"lnp{i}"])
    wo = cx.sb("wo", [128, 8, D], BF16)
    wov = wout.rearrange("(kc p) n -> p kc n", p=128)
    for kc in range(8):
        S.dma("pool", wo[:, kc, :], wov[:, kc, :], writes=[f"wo{kc}"])
    if not moe:
        accb = cx.sb("accb", [128, nsubG, D])
        x1T = cx.sb("x1T", [128, 8, G], BF16)
    if moe:
        rw = cx.sb("rw", [128, 8, NEXP])
        S.dma("sp", rw[:, :, :], ffn["router"].rearrange("(kc p) n -> p kc n", p=128), writes=["rw"])
        selA = cx.sb("selA", [128, nsubG, NEXP])
        m1A = cx.sb("m1A", [128, nsubG, NEXP])
        combA = cx.sb("combA", [128, nsubG, NEXP])
        rankA = cx.sb("rankA", [128, nsubG, NEXP])
        base = cx.sb("base", [128, NEXP])
        UT = cx.sb("UT", [128, 128])
        S.op("pool", lambda e: e.memset(base[:], 0.0), writes=["base"])
        S.op("pool", lambda e: e.memset(UT[:], 1.0), writes=["UT"])
        S.op("pool", lambda e: e.affine_select(out=UT[:], in_=UT[:], pattern=[[1, 128]], compare_op=ALU.is_ge,
                                               fill=0.0, base=-1, channel_multiplier=-1), reads=["UT"], writes=["UT"])
    pctr = [0]

    def bank():
        i = pctr[0] % 8
        pctr[0] += 1
        return ps[i], pk[i]

    def layer_norm_rows(src, skey, dst, dkey, gi_, bi_, small):
        st, mv = small
        for hh in range(2):
            S.op("dve", lambda e, hh=hh: e.bn_stats(out=st[:, hh * 6:(hh + 1) * 6], in_=src[:, hh * 512:(hh + 1) * 512]), reads=[skey], writes=["bnst"])
        S.op("dve", lambda e: e.bn_aggr(out=mv[:, 0:2], in_=st[:, 0:12]), reads=["bnst"], writes=["bnmv"])
        S.op("dve", lambda e: e.tensor_scalar(out=mv[:, 2:3], in0=mv[:, 1:2], scalar1=LN_EPS, scalar2=None, op0=ALU.add), reads=["bnmv"], writes=["bnr"])
        S.op("act", lambda e: e.activation(out=mv[:, 2:3], in_=mv[:, 2:3], func=AF.Sqrt), reads=["bnr"], writes=["bnr"])
        S.op("dve", lambda e: e.reciprocal(out=mv[:, 2:3], in_=mv[:, 2:3]), reads=["bnr"], writes=["bnr"])
        S.op("dve", lambda e: e.tensor_scalar(out=mv[:, 3:4], in0=mv[:, 0:1], scalar1=mv[:, 2:3], scalar2=-1.0, op0=ALU.mult, op1=ALU.mult),
             reads=["bnmv", "bnr"], writes=["bnnb"])
        S.op("act", lambda e: e.activation(out=dst, in_=src, func=AF.Identity, scale=mv[:, 2:3], bias=mv[:, 3:4]),
             reads=[skey, "bnr", "bnnb"], writes=[dkey])
        S.op("pool", lambda e: e.tensor_tensor(out=dst, in0=dst, in1=lnp[:, gi_, :], op=ALU.mult), reads=[dkey, f"lnp{gi_}"], writes=[dkey])
        S.op("dve", lambda e: e.tensor_tensor(out=dst, in0=dst, in1=lnp[:, bi_, :], op=ALU.add), reads=[dkey, f"lnp{bi_}"], writes=[dkey])

    for g0 in range(0, S_core, G):
        cx.push()
        xt = [cx.sb(f"cxt{i}", [128, 4, D]) for i in range(2)]
        oTt = [cx.sb(f"coT{i}", [128, 4, 512]) for i in range(2)]
        zgt = [cx.sb(f"czg{i}", [128, 4, 512]) for i in range(2)]
        cTt = [cx.sb(f"ccT{i}", [128, 4, 512]) for i in range(2)]
        yct = [cx.sb(f"cyc{i}", [128, 4, 512], BF16) for i in range(2)]
        ydn = cx.sb("ydn", [128, 4, 512], BF16)
        sqc = cx.sb("sqc", [128, 512])
        rrs = [cx.sb(f"rr{i}", [128, D]) for i in range(2)]
        x1 = cx.sb("x1", [128, D])
        st = cx.sb("bnst", [128, 12])
        mv = cx.sb("bnmv", [128, 4])
        x1f = cx.sb("x1f", [128, 8, 128]) if moe else None
        lgt = cx.sb("lgt", [128, 8, 8]) if moe else None
        tile_starts = list(range(0, G, T))

        def load_tile(it):
            t0 = g0 + tile_starts[it]
            i2 = it % 2
            nsub = T // 128
            S.dma("sp", xt[i2][:, 0:nsub, :], xin[HALO + t0:HALO + t0 + T, :].rearrange("(s p) d -> p s d", p=128), writes=[f"cxt{i2}"])
            S.dma("sp", oTt[i2][:, :, 0:T], oc_d[0, :, t0:t0 + T].rearrange("(h e) t -> e h t", e=128), writes=[f"coT{i2}"])
            S.dma("sp", cTt[i2][:, :, 0:T], oc_d[1, :, t0:t0 + T].rearrange("(h e) t -> e h t", e=128), writes=[f"ccT{i2}"])
            S.dma("sp", zgt[i2][:, :, 0:T], zg_d[:, t0:t0 + T].rearrange("(h e) t -> e h t", e=128), writes=[f"czg{i2}"])
            S.dma("pool", yct[i2][:, :, 0:T], yc_d[:, t0:t0 + T].rearrange("(h e) t -> e h t", e=128), writes=[f"cyc{i2}"])

        load_tile(0)
        for it, tt0 in enumerate(tile_starts):
            t0 = g0 + tt0
            i2 = it % 2
            nsub = T // 128
            if it + 1 < len(tile_starts):
                load_tile(it + 1)
            for h in range(4):
                pb, pkk = bank()
                S.op("pe", lambda e, pb=pb, h=h: e.matmul(pb[:, 0:T], lhsT=sin[h][:, :], rhs=cTt[i2][:, h, 0:T], start=True, stop=True),
                     reads=[f"sin{h}", f"ccT{i2}"], writes=[pkk])
                S.op("dve", lambda e, pb=pb, h=h: e.tensor_tensor(out=oTt[i2][:, h, 0:T], in0=oTt[i2][:, h, 0:T], in1=pb[:, 0:T], op=ALU.add),
                     reads=[pkk, f"coT{i2}"], writes=[f"coT{i2}"])
                pb, pkk = bank()
                S.op("act", lambda e, h=h: e.activation(out=sqc[:, 0:T], in_=oTt[i2][:, h, 0:T], func=AF.Square), reads=[f"coT{i2}"], writes=["sqc"])
                S.op("pe", lambda e, pb=pb: e.matmul(pb[:, 0:T], lhsT=o128[:], rhs=sqc[:, 0:T], start=True, stop=True), reads=["o128", "sqc"], writes=[pkk])
                S.op("dve", lambda e, pb=pb: e.tensor_scalar(out=sqc[:, 0:T], in0=pb[:, 0:T], scalar1=RMS_EPS, scalar2=None, op0=ALU.add), reads=[pkk], writes=["sqc"])
                S.op("act", lambda e: e.activation(out=sqc[:, 0:T], in_=sqc[:, 0:T], func=AF.Sqrt), reads=["sqc"], writes=["sqc"])
                S.op("dve", lambda e: e.reciprocal(out=sqc[:, 0:T], in_=sqc[:, 0:T]), reads=["sqc"], writes=["sqc"])
                S.op("dve", lambda e, h=h: e.tensor_tensor(out=sqc[:, 0:T], in0=sqc[:, 0:T], in1=oTt[i2][:, h, 0:T], op=ALU.mult), reads=["sqc", f"coT{i2}"], writes=["sqc"])
                S.op("dve", lambda e, h=h: e.scalar_tensor_tensor(out=ydn[:, h, 0:T], in0=sqc[:, 0:T], scalar=pv[:, PV_ONG:PV_ONG + 1],
                                                                  in1=zgt[i2][:, h, 0:T], op0=ALU.mult, op1=ALU.mult),
                     reads=["sqc", "pv", f"czg{i2}"], writes=[f"ydn{h}"])
            def wout_stage(s):
                ts = slice(s * 128, (s + 1) * 128)
                rr = rrs[s % 2]
                for half in range(2):
                    pb, pkk = bank()
                    for kc in range(8):
                        lhs = yct[i2][:, kc, ts] if kc < 4 else ydn[:, kc - 4, ts]
                        lk = f"cyc{i2}" if kc < 4 else f"ydn{kc - 4}"
                        S.op("pe", lambda e, pb=pb, lhs=lhs, kc=kc, half=half: e.matmul(pb[:, :], lhsT=lhs, rhs=wo[:, kc, half * 512:(half + 1) * 512],
                                                                                        start=(kc == 0), stop=(kc == 7)),
                             reads=[lk, f"wo{kc}"], writes=[pkk])
                    S.op("dve", lambda e, pb=pb, s=s, half=half, rr=rr: e.scalar_tensor_tensor(out=rr[:, half * 512:(half + 1) * 512], in0=xt[i2][:, s, half * 512:(half + 1) * 512],
                                                                                               scalar=ALPHA, in1=pb[:, :], op0=ALU.mult, op1=ALU.add),
                         reads=[pkk, f"cxt{i2}"], writes=[f"rr{s % 2}"])

            wout_stage(0)
            for s in range(nsub):
                sg_ = (tt0 // 128) + s
                ts = slice(s * 128, (s + 1) * 128)
                if s + 1 < nsub:
                    wout_stage(s + 1)
                layer_norm_rows(rrs[s % 2][:, :], f"rr{s % 2}", x1[:, :], "x1", 0, 1, (st, mv))
                if moe:
                    S.dma("pool", scr["x1f"][t0 + s * 128:t0 + (s + 1) * 128, :], x1[:, :], reads=["x1"], writes=["x1f_d"])
                else:
                    S.op("act", lambda e, sg_=sg_: e.activation(out=accb[:, sg_, :], in_=x1[:, :], func=AF.Copy, scale=ALPHA), reads=["x1"], writes=[f"accb{sg_}"])
                for kc in range(8):
                    if kc % 4 == 0:
                        pb, pkk = bank()
                    q4 = kc % 4
                    S.op("pe", lambda e, pb=pb, kc=kc, q4=q4: e.transpose(out=pb[:, q4 * 128:(q4 + 1) * 128], in_=x1[:, kc * 128:(kc + 1) * 128], identity=ident[:]),
                         reads=["x1", "c_ident"], writes=[pkk])
                    if q4 == 3:
                        k0 = kc - 3
                        if not moe:
                            S.op("act", lambda e, pb=pb, k0=k0, sg_=sg_: e.activation(out=x1T[:, k0:k0 + 4, sg_ * 128:(sg_ + 1) * 128],
                                                                                     in_=pb[:, :].rearrange("p (k t) -> p k t", t=128), func=AF.Copy),
                                 reads=[pkk], writes=[f"x1T{sg_}"])
                        if moe:
                            S.op("dve", lambda e, pb=pb, k0=k0: e.tensor_copy(out=x1f[:, k0:k0 + 4, :], in_=pb[:, :].rearrange("p (k t) -> p k t", t=128)),
                                 reads=[pkk], writes=[f"x1f{k0}"])
                if moe:
                    pb, pkk = bank()
                    for kc in range(8):
                        S.op("pe", lambda e, pb=pb, kc=kc: e.matmul(pb[:, 0:NEXP], lhsT=x1f[:, kc, :], rhs=rw[:, kc, :], start=(kc == 0), stop=(kc == 7)),
                             reads=[f"x1f{(kc // 4) * 4}", "rw"], writes=[pkk])
                    L = lgt
                    S.op("dve", lambda e, pb=pb: e.tensor_copy(out=L[:, 0, :], in_=pb[:, 0:NEXP]), reads=[pkk], writes=["lgt"])
                    S.op("dve", lambda e: e.tensor_reduce(out=L[:, 7, 0:1], in_=L[:, 0, :], axis=AX.X, op=ALU.max), reads=["lgt"], writes=["lgt"])
                    S.op("dve", lambda e, sg_=sg_: e.tensor_scalar(out=m1A[:, sg_, :], in0=L[:, 0, :], scalar1=L[:, 7, 0:1], scalar2=None, op0=ALU.is_equal),
                         reads=["lgt"], writes=["m1A"])
                    S.op("dve", lambda e, sg_=sg_: e.tensor_scalar(out=L[:, 1, :], in0=m1A[:, sg_, :], scalar1=-1e30, scalar2=None, op0=ALU.mult),
                         reads=["lgt", "m1A"], writes=["lgt"])
                    S.op("dve", lambda e: e.tensor_tensor(out=L[:, 1, :], in0=L[:, 1, :], in1=L[:, 0, :], op=ALU.add), reads=["lgt"], writes=["lgt"])
                    S.op("dve", lambda e: e.tensor_reduce(out=L[:, 7, 1:2], in_=L[:, 1, :], axis=AX.X, op=ALU.max), reads=["lgt"], writes=["lgt"])
                    S.op("dve", lambda e: e.tensor_scalar(out=L[:, 2, :], in0=L[:, 0, :], scalar1=L[:, 7, 1:2], scalar2=None, op0=ALU.is_ge), reads=["lgt"], writes=["lgt"])
                    S.op("dve", lambda e: e.tensor_scalar(out=L[:, 3, :], in0=L[:, 0, :], scalar1=L[:, 7, 0:1], scalar2=None, op0=ALU.subtract), reads=["lgt"], writes=["lgt"])
                    S.op("act", lambda e: e.activation(out=L[:, 3, :], in_=L[:, 3, :], func=AF.Exp), reads=["lgt"], writes=["lgt"])
                    S.op("dve", lambda e: e.tensor_tensor(out=L[:, 3, :], in0=L[:, 3, :], in1=L[:, 2, :], op=ALU.mult), reads=["lgt"], writes=["lgt"])
                    S.op("dve", lambda e: e.tensor_reduce(out=L[:, 7, 2:3], in_=L[:, 3, :], axis=AX.X, op=ALU.add), reads=["lgt"], writes=["lgt"])
                    S.op("dve", lambda e: e.reciprocal(out=L[:, 7, 3:4], in_=L[:, 7, 2:3]), reads=["lgt"], writes=["lgt"])
                    S.op("dve", lambda e, sg_=sg_: e.tensor_scalar(out=combA[:, sg_, :], in0=L[:, 3, :], scalar1=L[:, 7, 3:4], scalar2=None, op0=ALU.mult),
                         reads=["lgt"], writes=["combA"])
                    S.op("dve", lambda e, sg_=sg_: e.tensor_copy(out=selA[:, sg_, :], in_=L[:, 2, :]), reads=["lgt"], writes=["selA"])
                    pb, pkk = bank()
                    S.op("pe", lambda e, pb=pb, sg_=sg_: e.matmul(pb[:, 0:NEXP], lhsT=UT[:], rhs=selA[:, sg_, :], start=True, stop=True),
                         reads=["UT", "selA"], writes=[pkk])
                    S.op("pe", lambda e, pb=pb, sg_=sg_: e.matmul(pb[:, NEXP:2 * NEXP], lhsT=ones[:], rhs=selA[:, sg_, :], start=True, stop=True),
                         reads=["c_ones", "selA"], writes=[pkk])
                    S.op("dve", lambda e, pb=pb, sg_=sg_: e.tensor_tensor(out=rankA[:, sg_, :], in0=pb[:, 0:NEXP], in1=base[:], op=ALU.add),
                         reads=[pkk, "base"], writes=["rankA"])
                    S.op("dve", lambda e, pb=pb: e.tensor_tensor(out=base[:], in0=pb[:, NEXP:2 * NEXP], in1=base[:], op=ALU.add),
                         reads=[pkk, "base"], writes=["base"])
        cx.pop()
        if moe:
            moe_sparse(cx, S_core, NT, ffn, scr, xout, lnp, layer_norm_rows, selA, m1A, combA, rankA, base)
            continue
        cx.push()
        wg = [cx.sb(f"wg{i}", [128, 8, FG * 128], BF16) for i in range(2)]
        wu = [cx.sb(f"wu{i}", [128, 8, FG * 128], BF16) for i in range(2)]
        wd = [cx.sb(f"wd{i}", [128, FG, D], BF16) for i in range(2)]
        hT = cx.sb("hT", [128, FG, G], BF16)
        sgb = [cx.sb(f"sgb{i}", [128, 512]) for i in range(2)]
        yo = [cx.sb(f"yo{i}", [128, D]) for i in range(2)]
        st = cx.sb("bnst2", [128, 12])
        mv = cx.sb("bnmv2", [128, 4])
        units = [(e_, f0, nf) for e_ in range(nexp) for (f0, nf) in fgroups]

        def load_w(ui):
            e_, f0, nf = units[ui]
            i2 = ui % 2
            gsrc = ffn["wg"][e_, :, f0 * 128:(f0 + nf) * 128].rearrange("(kc p) n -> p kc n", p=128)
            usrc = ffn["wu"][e_, :, f0 * 128:(f0 + nf) * 128].rearrange("(kc p) n -> p kc n", p=128)
            dsrc = ffn["wd"][e_, f0 * 128:(f0 + nf) * 128, :].rearrange("(f p) n -> p f n", p=128)
            S.dma("pool", wg[i2][:, :, 0:nf * 128], gsrc, writes=[f"wg{i2}"])
            S.dma("pool", wu[i2][:, :, 0:nf * 128], usrc, writes=[f"wu{i2}"])
            S.dma("pool", wd[i2][:, 0:nf, :], dsrc, writes=[f"wd{i2}"])

        load_w(0)
        cnt = 0
        for ui, (e_, f0, nf) in enumerate(units):
            i2 = ui % 2
            if ui + 1 < len(units):
                load_w(ui + 1)
            for tt0 in range(0, G, T):
                for f in range(nf):
                    pg, pgk = bank()
                    pu, puk = bank()
                    for kc in range(8):
                        S.op("pe", lambda e, pg=pg, kc=kc, f=f: e.matmul(pg[:, 0:T], lhsT=wg[i2][:, kc, f * 128:(f + 1) * 128], rhs=x1T[:, kc, tt0:tt0 + T],
                                                                         start=(kc == 0), stop=(kc == 7)),
                             reads=[f"wg{i2}"] + [f"x1T{(tt0 // 128) + s}" for s in range(T // 128)], writes=[pgk])
                    for kc in range(8):
                        S.op("pe", lambda e, pu=pu, kc=kc, f=f: e.matmul(pu[:, 0:T], lhsT=wu[i2][:, kc, f * 128:(f + 1) * 128], rhs=x1T[:, kc, tt0:tt0 + T],
                                                                         start=(kc == 0), stop=(kc == 7)),
                             reads=[f"wu{i2}"] + [f"x1T{(tt0 // 128) + s}" for s in range(T // 128)], writes=[puk])
                    sb_ = sgb[cnt % 2]
                    sk = f"sgb{cnt % 2}"
                    cnt += 1
                    S.op("act", lambda e, pg=pg, sb_=sb_: e.activation(out=sb_[:, 0:T], in_=pg[:, 0:T], func=AF.Silu), reads=[pgk], writes=[sk])
                    S.op("dve", lambda e, pu=pu, sb_=sb_, f=f: e.tensor_tensor(out=hT[:, f, tt0:tt0 + T], in0=sb_[:, 0:T], in1=pu[:, 0:T], op=ALU.mult),
                         reads=[puk, sk], writes=[f"hT{f}_{tt0}"])
            for sg_ in range(nsubG):
                tt0 = (sg_ * 128 // T) * T
                for half in range(2):
                    pb, pkk = bank()
                    for f in range(nf):
                        S.op("pe", lambda e, pb=pb, f=f, sg_=sg_, half=half: e.matmul(pb[:, :], lhsT=hT[:, f, sg_ * 128:(sg_ + 1) * 128],
                                                                                      rhs=wd[i2][:, f, half * 512:(half + 1) * 512],
                                                                                      start=(f == 0), stop=(f == nf - 1)),
                             reads=[f"hT{f}_{tt0}", f"wd{i2}"], writes=[pkk])
                    if moe:
                        S.op("dve", lambda e, pb=pb, sg_=sg_, half=half, e_=e_: e.scalar_tensor_tensor(
                            out=accb[:, sg_, half * 512:(half + 1) * 512], in0=pb[:, :], scalar=comb[:, sg_, e_:e_ + 1],
                            in1=accb[:, sg_, half * 512:(half + 1) * 512], op0=ALU.mult, op1=ALU.add),
                             reads=[pkk, "comb", f"accb{sg_}"], writes=[f"accb{sg_}"])
                    else:
                        S.op("dve", lambda e, pb=pb, sg_=sg_, half=half: e.tensor_tensor(
                            out=accb[:, sg_, half * 512:(half + 1) * 512], in0=pb[:, :],
                            in1=accb[:, sg_, half * 512:(half + 1) * 512], op=ALU.add),
                             reads=[pkk, f"accb{sg_}"], writes=[f"accb{sg_}"])
        for sg_ in range(nsubG):
            yb, yk = yo[sg_ % 2], f"yo{sg_ % 2}"
            layer_norm_rows(accb[:, sg_, :], f"accb{sg_}", yb[:, :], yk, 2, 3, (st, mv))
            S.dma("sp", xout[g0 + sg_ * 128:g0 + (sg_ + 1) * 128, :], yb[:, :], reads=[yk], writes=[f"xout{sg_ % 2}"])
        cx.pop()
    cx.pop()


def moe_sparse(cx, S_core, NT, ffn, scr, xout, lnp, layer_norm_rows, selA, m1A, combA, rankA, base):
    nc, S, c = cx.nc, cx.S, cx.c
    ident = c["ident"]
    ps, pk = cx.ps, cx.psk
    nsub = S_core // 128
    x1f_d, s2t_d, y_d = scr["x1f"], scr["s2t"], scr["y"]
    cx.push()
    pcn = cx.sb("pcn", [128, NEXP])
    tmp8 = cx.sb("tmp8", [128, NEXP])
    offs = cx.sb("offs", [128, NEXP])
    ends = cx.sb("ends", [128, NEXP])
    S.op("dve", lambda e: e.tensor_scalar(out=pcn[:], in0=base[:], scalar1=0.0, scalar2=None, op0=ALU.is_gt), reads=["base"], writes=["pcn"])
    for k in range(1, 2 * S_core // 512 + 1):
        S.op("dve", lambda e, k=k: e.tensor_scalar(out=tmp8[:], in0=base[:], scalar1=512.0 * k, scalar2=None, op0=ALU.is_gt), reads=["base"], writes=["tmp8"])
        S.op("dve", lambda e: e.tensor_tensor(out=pcn[:], in0=pcn[:], in1=tmp8[:], op=ALU.add), reads=["tmp8", "pcn"], writes=["pcn"])
    S.op("dve", lambda e: e.tensor_scalar(out=pcn[:], in0=pcn[:], scalar1=512.0, scalar2=None, op0=ALU.mult), reads=["pcn"], writes=["pcn"])
    S.op("dve", lambda e: e.memset(offs[:], 0.0), writes=["offs"])
    for e_ in range(1, NEXP):
        S.op("dve", lambda e, e_=e_: e.tensor_tensor(out=offs[:, e_:e_ + 1], in0=offs[:, e_ - 1:e_], in1=pcn[:, e_ - 1:e_], op=ALU.add),
             reads=["offs", "pcn"], writes=["offs"])
    S.op("dve", lambda e: e.tensor_tensor(out=ends[:], in0=offs[:], in1=pcn[:], op=ALU.add), reads=["offs", "pcn"], writes=["ends"])
    eidf = cx.sb("eidf", [128, NT])
    eidi = cx.sb("eidi", [128, NT], I32)
    for i in range(NT):
        S.op("dve", lambda e, i=i: e.tensor_scalar(out=tmp8[:], in0=ends[:], scalar1=512.0 * i, scalar2=None, op0=ALU.is_le), reads=["ends"], writes=["tmp8"])
        S.op("dve", lambda e, i=i: e.tensor_reduce(out=eidf[:, i:i + 1], in_=tmp8[:], axis=AX.X, op=ALU.add), reads=["tmp8"], writes=["eidf"])
    S.op("dve", lambda e: e.tensor_scalar(out=eidf[:], in0=eidf[:], scalar1=float(NEXP - 1), scalar2=None, op0=ALU.min), reads=["eidf"], writes=["eidf"])
    NFG = DFF_EXP // 128 // 4
    gi_ = cx.sb("gi_", [128, NFG], I32)
    gf_ = cx.sb("gf_", [128, NFG])
    tabGf = cx.sb("tabGf", [128, NT, NFG])
    tabDf = cx.sb("tabDf", [128, NT, NFG])
    tabG = cx.sb("tabG", [128, NT, NFG], I32)
    tabD = cx.sb("tabD", [128, NT, NFG], I32)
    e1 = cx.sb("e1", [128, NT])
    S.op("pool", lambda e: e.iota(gi_[:], pattern=[[1, NFG]], base=0, channel_multiplier=0), writes=["gi_"])
    S.op("dve", lambda e: e.tensor_copy(out=gf_[:], in_=gi_[:]), reads=["gi_"], writes=["gf_"])
    S.op("dve", lambda e: e.tensor_scalar(out=e1[:], in0=eidf[:], scalar1=float(D * DFF_EXP // 512), scalar2=None, op0=ALU.mult), reads=["eidf"], writes=["e1"])
    for i in range(NT):
        S.op("dve", lambda e, i=i: e.tensor_scalar(out=tabGf[:, i, :], in0=gf_[:], scalar1=e1[:, i:i + 1], scalar2=512.0, op0=ALU.add, op1=ALU.mult),
             reads=["gf_", "e1"], writes=["tabGf"])
    S.op("dve", lambda e: e.tensor_scalar(out=e1[:], in0=eidf[:], scalar1=float(D * DFF_EXP // 524288), scalar2=None, op0=ALU.mult), reads=["eidf", "tabGf"], writes=["e1"])
    for i in range(NT):
        S.op("dve", lambda e, i=i: e.tensor_scalar(out=tabDf[:, i, :], in0=gf_[:], scalar1=e1[:, i:i + 1], scalar2=524288.0, op0=ALU.add, op1=ALU.mult),
             reads=["gf_", "e1"], writes=["tabDf"])
    S.op("dve", lambda e: e.tensor_copy(out=tabG[:], in_=tabGf[:]), reads=["tabGf"], writes=["tabG"])
    S.op("dve", lambda e: e.tensor_copy(out=tabD[:], in_=tabDf[:]), reads=["tabDf"], writes=["tabD"])
    posf = cx.sb("posf", [128, 2, nsub])
    gts = cx.sb("gts", [128, 2, nsub])
    tmpb = cx.sb("tmpb", [128, nsub, NEXP])
    m2A = cx.sb("m2A", [128, nsub, NEXP])
    for sg in range(nsub):
        S.op("dve", lambda e, sg=sg: e.tensor_tensor(out=rankA[:, sg, :], in0=rankA[:, sg, :], in1=offs[:], op=ALU.add),
             reads=["rankA", "offs"], writes=["rankA"])
    S.op("dve", lambda e: e.tensor_tensor(out=m2A[:], in0=selA[:], in1=m1A[:], op=ALU.subtract), reads=["selA", "m1A"], writes=["m2A"])
    for w, mk, mkey in ((0, m1A, "m1A"), (1, m2A, "m2A")):
        S.op("dve", lambda e, mk=mk: e.tensor_tensor(out=tmpb[:], in0=mk[:], in1=rankA[:], op=ALU.mult), reads=[mkey, "rankA"], writes=["tmpb"])
        S.op("dve", lambda e, w=w: e.tensor_reduce(out=posf[:, w, :], in_=tmpb[:], axis=AX.X, op=ALU.add), reads=["tmpb"], writes=["posf"])
        S.op("dve", lambda e, mk=mk: e.tensor_tensor(out=tmpb[:], in0=mk[:], in1=combA[:], op=ALU.mult), reads=[mkey, "combA"], writes=["tmpb"])
        S.op("dve", lambda e, w=w: e.tensor_reduce(out=gts[:, w, :], in_=tmpb[:], axis=AX.X, op=ALU.add), reads=["tmpb"], writes=["gts"])
    posi = cx.sb("posi", [128, 2, nsub], I32)
    tokid = cx.sb("tokid", [128, nsub], I32)
    zer = cx.sb("zer", [128, NT * 4], I32)
    idx_all = cx.sb("idx_all", [128, NT * 4], I32)
    S.op("dve", lambda e: e.tensor_copy(out=posi[:], in_=posf[:]), reads=["posf"], writes=["posi"])
    S.op("pool", lambda e: e.iota(tokid[:], pattern=[[128, nsub]], base=0, channel_multiplier=1), writes=["tokid"])
    S.op("pool", lambda e: e.memset(zer[:], 0), writes=["zer"])
    S.dma("sp", s2t_d.rearrange("(p j) o -> p (j o)", p=128), zer[:, :], reads=["zer"], writes=["s2t_d"])
    for w in range(2):
        for sg in range(nsub):
            S.ind_dma(s2t_d[:, :], tokid[:, sg:sg + 1], posi[:, w, sg:sg + 1], False, reads=["tokid", "posi", "s2t_d"], writes=[f"s2t_{w}_{sg}"])
    S.dma("sp", idx_all[:, :], s2t_d.rearrange("(j p) o -> p (j o)", p=128),
          reads=["s2t_d"] + [f"s2t_{w}_{sg}" for w in range(2) for sg in range(nsub)], writes=["idx_all"], allow_slow_non_contiguous=True)

    cx.push()
    FG = 4
    nfc = DFF_EXP // 128
    fgroups = [(f0, min(FG, nfc - f0)) for f0 in range(0, nfc, FG)]
    wg = [cx.sb(f"swg{i}", [128, 8, FG * 128], BF16) for i in range(2)]
    wu = [cx.sb(f"swu{i}", [128, 8, FG * 128], BF16) for i in range(2)]
    wd = [cx.sb(f"swd{i}", [128, FG, D], BF16) for i in range(2)]
    hT = cx.sb("shT", [128, FG, 512], BF16)
    sgb = [cx.sb(f"ssgb{i}", [128, 512]) for i in range(2)]
    xg = [cx.sb(f"xg{i}", [128, D]) for i in range(2)]
    xgT = [cx.sb(f"xgT{i}", [128, 8, 512], BF16) for i in range(2)]
    yacc = [cx.sb(f"yacc{i}", [128, 4, D]) for i in range(2)]
    pctr = [0]

    def bank():
        i = pctr[0] % 8
        pctr[0] += 1
        return ps[i], pk[i]

    regG = nc.gpsimd.alloc_register("offg_reg")
    regD = nc.gpsimd.alloc_register("offd_reg")
    units = [(i, f0, nf) for i in range(NT) for (f0, nf) in fgroups]
    RH = type(regG)

    def dyn_dma(dst, tens, val, ap, key):
        src = bass.AP(tensor=tens.tensor, offset=val, ap=ap)
        S.dma("pool", dst, src, writes=[key])
        js = nc.instruction_to_json(S.last_inst.ins)
        return set(re.findall(r'"reg_ap_offset": "([^"]+)"', js))

    def load_w(ui):
        i, f0, nf = units[ui]
        i2 = ui % 2
        g = f0 // FG
        S._deps("pool", ["tabG", "tabD"], [])
        nc.gpsimd.reg_load(regG, tabG[0:1, i, g:g + 1])
        nc.gpsimd.reg_load(regD, tabD[0:1, i, g:g + 1])
        vG = nc.gpsimd.snap(regG)
        vD = nc.gpsimd.snap(regD)
        tmps = set()
        tmps |= dyn_dma(wg[i2][:, :, 0:nf * 128], ffn["wg"], vG, [[DFF_EXP, 128], [128 * DFF_EXP, 8], [1, nf * 128]], f"swg{i2}")
        tmps |= dyn_dma(wu[i2][:, :, 0:nf * 128], ffn["wu"], vG, [[DFF_EXP, 128], [128 * DFF_EXP, 8], [1, nf * 128]], f"swu{i2}")
        tmps |= dyn_dma(wd[i2][:, 0:nf, :], ffn["wd"], vD, [[D, 128], [128 * D, nf], [1, D]], f"swd{i2}")
        for t in tmps:
            nc.gpsimd.free_register(RH(name=t, engine=regG.engine))
        nc.gpsimd.free_register(vG.val)
        nc.gpsimd.free_register(vD.val)

    def gather_tile(i):
        t2 = i % 2
        for s_ in range(4):
            j = i * 4 + s_
            xb, xk = xg[j % 2], f"xg{j % 2}"
            S.ind_dma(xb[:, :], x1f_d[:, :], idx_all[:, j:j + 1], True, reads=["idx_all"], writes=[xk])
            for kc in range(8):
                if kc % 4 == 0:
                    pb, pkk = bank()
                q4 = kc % 4
                S.op("pe", lambda e, pb=pb, kc=kc, q4=q4, xb=xb: e.transpose(out=pb[:, q4 * 128:(q4 + 1) * 128], in_=xb[:, kc * 128:(kc + 1) * 128], identity=ident[:]),
                     reads=[xk, "c_ident"], writes=[pkk])
                if q4 == 3:
                    k0 = kc - 3
                    eng = "act" if k0 == 0 else "dve"
                    if eng == "act":
                        S.op("act", lambda e, pb=pb, k0=k0, s_=s_: e.activation(out=xgT[t2][:, k0:k0 + 4, s_ * 128:(s_ + 1) * 128],
                                                                                in_=pb[:, :].rearrange("p (k t) -> p k t", t=128), func=AF.Copy),
                             reads=[pkk], writes=[f"xgT{t2}_{s_}a"])
                    else:
                        S.op("dve", lambda e, pb=pb, k0=k0, s_=s_: e.tensor_copy(out=xgT[t2][:, k0:k0 + 4, s_ * 128:(s_ + 1) * 128],
                                                                                 in_=pb[:, :].rearrange("p (k t) -> p k t", t=128)),
                             reads=[pkk], writes=[f"xgT{t2}_{s_}d"])

    load_w(0)
    gather_tile(0)
    cnt = 0
    for ui, (i, f0, nf) in enumerate(units):
        i2 = ui % 2
        t2 = i % 2
        if ui + 1 < len(units):
            load_w(ui + 1)
        if f0 == 0 and i + 1 < NT:
            gather_tile(i + 1)
        xkeys = [f"xgT{t2}_{s_}{a}" for s_ in range(4) for a in "ad"]
        for f in range(nf):
            pg, pgk = bank()
            pu, puk = bank()
            for kc in range(8):
                S.op("pe", lambda e, pg=pg, kc=kc, f=f: e.matmul(pg[:, :], lhsT=wg[i2][:, kc, f * 128:(f + 1) * 128], rhs=xgT[t2][:, kc, :],
                                                                 start=(kc == 0), stop=(kc == 7)),
                     reads=[f"swg{i2}"] + xkeys, writes=[pgk])
            for kc in range(8):
                S.op("pe", lambda e, pu=pu, kc=kc, f=f: e.matmul(pu[:, :], lhsT=wu[i2][:, kc, f * 128:(f + 1) * 128], rhs=xgT[t2][:, kc, :],
                                                                 start=(kc == 0), stop=(kc == 7)),
                     reads=[f"swu{i2}"] + xkeys, writes=[puk])
            sb_ = sgb[cnt % 2]
            sk = f"ssgb{cnt % 2}"
            cnt += 1
            S.op("act", lambda e, pg=pg, sb_=sb_: e.activation(out=sb_[:, :], in_=pg[:, :], func=AF.Silu), reads=[pgk], writes=[sk])
            S.op("dve", lambda e, pu=pu, sb_=sb_, f=f: e.tensor_tensor(out=hT[:, f, :], in0=sb_[:, :], in1=pu[:, :], op=ALU.mult),
                 reads=[puk, sk], writes=[f"shT{f}"])
        for s_ in range(4):
            for half in range(2):
                pb, pkk = bank()
                for f in range(nf):
                    S.op("pe", lambda e, pb=pb, f=f, s_=s_, half=half: e.matmul(pb[:, :], lhsT=hT[:, f, s_ * 128:(s_ + 1) * 128],
                                                                                rhs=wd[i2][:, f, half * 512:(half + 1) * 512],
                                                                                start=(f == 0), stop=(f == nf - 1)),
                         reads=[f"shT{f}", f"swd{i2}"], writes=[pkk])
                yk = f"yacc{t2}_{s_}"
                if f0 == 0:
                    S.op("act", lambda e, pb=pb, s_=s_, half=half: e.activation(out=yacc[t2][:, s_, half * 512:(half + 1) * 512], in_=pb[:, :], func=AF.Copy),
                         reads=[pkk], writes=[yk])
                else:
                    S.op("dve", lambda e, pb=pb, s_=s_, half=half: e.tensor_tensor(out=yacc[t2][:, s_, half * 512:(half + 1) * 512], in0=pb[:, :],
                                                                                   in1=yacc[t2][:, s_, half * 512:(half + 1) * 512], op=ALU.add),
                         reads=[pkk, yk], writes=[yk])
        if f0 + nf == nfc:
            S.dma("sp", y_d[i * 512:(i + 1) * 512, :].rearrange("(s p) d -> p s d", p=128), yacc[t2][:, :, :],
                  reads=[f"yacc{t2}_{s_}" for s_ in range(4)], writes=[f"y_d{t2}"])
    cx.pop()
    ya = [cx.sb(f"ya{i}", [128, D]) for i in range(2)]
    yb = [cx.sb(f"yb{i}", [128, D]) for i in range(2)]
    x1b = [cx.sb(f"x1b{i}", [128, D]) for i in range(2)]
    yo = [cx.sb(f"syo{i}", [128, D]) for i in range(2)]
    st = cx.sb("bnst3", [128, 12])
    mv = cx.sb("bnmv3", [128, 4])
    def comb_load(sg):
        i2 = sg % 2
        S.dma("sp", x1b[i2][:, :], x1f_d[sg * 128:(sg + 1) * 128, :], writes=[f"x1b{i2}"])
        S.ind_dma(ya[i2][:, :], y_d[:, :], posi[:, 0, sg:sg + 1], True, reads=["posi"], writes=[f"ya{i2}"])
        S.ind_dma(yb[i2][:, :], y_d[:, :], posi[:, 1, sg:sg + 1], True, reads=["posi"], writes=[f"yb{i2}"])

    comb_load(0)
    for sg in range(nsub):
        i2 = sg % 2
        if sg + 1 < nsub:
            comb_load(sg + 1)
        S.op("act", lambda e: e.activation(out=x1b[i2][:, :], in_=x1b[i2][:, :], func=AF.Copy, scale=ALPHA), reads=[f"x1b{i2}"], writes=[f"x1b{i2}"])
        S.op("dve", lambda e, sg=sg: e.scalar_tensor_tensor(out=x1b[i2][:, :], in0=ya[i2][:, :], scalar=gts[:, 0, sg:sg + 1], in1=x1b[i2][:, :],
                                                            op0=ALU.mult, op1=ALU.add), reads=[f"ya{i2}", "gts", f"x1b{i2}"], writes=[f"x1b{i2}"])
        S.op("dve", lambda e, sg=sg: e.scalar_tensor_tensor(out=x1b[i2][:, :], in0=yb[i2][:, :], scalar=gts[:, 1, sg:sg + 1], in1=x1b[i2][:, :],
                                                            op0=ALU.mult, op1=ALU.add), reads=[f"yb{i2}", "gts", f"x1b{i2}"], writes=[f"x1b{i2}"])
        layer_norm_rows(x1b[i2][:, :], f"x1b{i2}", yo[i2][:, :], f"syo{i2}", 2, 3, (st, mv))
        S.dma("sp", xout[sg * 128:(sg + 1) * 128, :], yo[i2][:, :], reads=[f"syo{i2}"], writes=[f"xout{i2}"])
    cx.pop()


GROUPS = [[0, 1, 2, 3], [4, 5, 6, 7]]


def phase_x(cx, st_d, stall_d, pv_d, sin):
    nc, S, c = cx.nc, cx.S, cx.c
    ident = c["ident"]
    ps, pk = cx.ps, cx.psk
    cx.push()
    pv = cx.sb("pvx", [128, NPV])
    S.dma("sp", pv[:], pv_d[:, :], writes=["pv"])
    S.coll("AllGather", st_d[:, :], stall_d[:, :], GROUPS, writes=["stall"])
    Gt = cx.sb("Gt", [128, 4, 4, 256])
    for sg in range(3):
        S.dma("sp", Gt[:, sg, :, :], stall_d[sg, :].rearrange("(h d c) -> d h c", h=4, d=128), reads=["stall"], writes=[f"Gt{sg}"])
    pmt = [cx.sb(f"pmt{i}", [128, 128]) for i in range(2)]
    t2 = cx.sb("t2", [128, 128])
    t3 = cx.sb("t3", [128, 128])
    for h in range(4):
        for i, sg in enumerate((1, 2)):
            S.op("pe", lambda e, sg=sg, i=i: e.transpose(out=ps[i][:, 0:128], in_=Gt[:, sg, h, 128:256], identity=ident[:]),
                 reads=[f"Gt{sg}", "c_ident"], writes=[pk[i]])
            S.op("dve", lambda e, i=i: e.tensor_copy(out=pmt[i][:], in_=ps[i][:, 0:128]), reads=[pk[i]], writes=[f"pmt{i}"])
        S.op("pe", lambda e: e.matmul(ps[2][:, 0:128], lhsT=pmt[0][:], rhs=Gt[:, 0, h, 0:128], start=True, stop=True),
             reads=["pmt0", "Gt0"], writes=[pk[2]])
        S.op("dve", lambda e: e.tensor_tensor(out=t2[:], in0=Gt[:, 1, h, 0:128], in1=ps[2][:, 0:128], op=ALU.add), reads=[pk[2], "Gt1"], writes=["t2"])
        S.op("pe", lambda e: e.matmul(ps[3][:, 0:128], lhsT=pmt[1][:], rhs=t2[:], start=True, stop=True), reads=["pmt1", "t2"], writes=[pk[3]])
        S.op("dve", lambda e: e.tensor_tensor(out=t3[:], in0=Gt[:, 2, h, 0:128], in1=ps[3][:, 0:128], op=ALU.add), reads=[pk[3], "Gt2"], writes=["t3"])
        S.op("dve", lambda e: e.tensor_scalar(out=sin[h][:, :], in0=Gt[:, 0, h, 0:128], scalar1=pv[:, PV_MSEG + 1:PV_MSEG + 2], scalar2=None, op0=ALU.mult),
             reads=["Gt0", "pv"], writes=[f"sin{h}"])
        S.op("dve", lambda e: e.scalar_tensor_tensor(out=sin[h][:, :], in0=t2[:], scalar=pv[:, PV_MSEG + 2:PV_MSEG + 3], in1=sin[h][:, :],
                                                     op0=ALU.mult, op1=ALU.add), reads=["t2", "pv", f"sin{h}"], writes=[f"sin{h}"])
        S.op("dve", lambda e: e.scalar_tensor_tensor(out=sin[h][:, :], in0=t3[:], scalar=pv[:, PV_MSEG + 3:PV_MSEG + 4], in1=sin[h][:, :],
                                                     op0=ALU.mult, op1=ALU.add), reads=["t3", "pv", f"sin{h}"], writes=[f"sin{h}"])
    cx.pop()


def phase_h(cx, S_core, x1in, hall_d, pv_d):
    S = cx.S
    cx.push()
    pv = cx.sb("pvh", [128, NPV])
    S.dma("sp", pv[:], pv_d[:, :], writes=["pv"])
    S.coll("AllGather", x1in[S_core:S_core + HALO, :], hall_d[:, :], GROUPS, writes=["hall"])
    ht = [cx.sb(f"ht{i}", [128, D]) for i in range(2)]
    hacc = cx.sb("hacc", [128, D])
    for r in range(4):
        S.dma("sp", ht[r % 2][:, :], hall_d[r * 128:(r + 1) * 128, :], reads=["hall"], writes=[f"ht{r % 2}"])
        if r == 0:
            S.op("dve", lambda e: e.tensor_scalar(out=hacc[:, :], in0=ht[0][:, :], scalar1=pv[:, PV_MPRED:PV_MPRED + 1], scalar2=None, op0=ALU.mult),
                 reads=["ht0", "pv"], writes=["hacc"])
        else:
            S.op("dve", lambda e, r=r: e.scalar_tensor_tensor(out=hacc[:, :], in0=ht[r % 2][:, :], scalar=pv[:, PV_MPRED + r:PV_MPRED + r + 1],
                                                              in1=hacc[:, :], op0=ALU.mult, op1=ALU.add),
                 reads=[f"ht{r % 2}", "pv", "hacc"], writes=["hacc"])
    S.dma("sp", x1in[0:HALO, :], hacc[:, :], reads=["hacc"], writes=["x1halo"])
    cx.pop()


def build_fused(S_core, depth=2):
    nc = bass.Bass("TRN2", target_bir_lowering=False)
    cx = Ctx(nc)
    cx.push()
    cx.consts()
    di = lambda n, s: nc.dram_tensor(n, s, F32, kind="ExternalInput").ap()
    dn = lambda n, s: nc.dram_tensor(n, s, F32).ap()
    xin0 = di("xin0", [HALO + S_core, D])
    win = di("win", [depth, D, IN_COLS])
    wout = di("wout", [depth, D, D])
    pv = di("pv", [depth, 128, NPV])
    lnrows = di("lnrows", [depth, 4, 128, D])
    dense = {"moe": False, "wg": di("dwg", [1, D, DFF_DENSE]), "wu": di("dwu", [1, D, DFF_DENSE]), "wd": di("dwd", [1, DFF_DENSE, D])}
    moe = {"moe": True, "router": di("router", [D, NEXP]), "wg": di("mwg", [NEXP, D, DFF_EXP]),
           "wu": di("mwu", [NEXP, D, DFF_EXP]), "wd": di("mwd", [NEXP, DFF_EXP, D])}
    xout = nc.dram_tensor("xout", [S_core, D], F32, kind="ExternalOutput").ap()
    yc = dn("yc_i", [512, S_core])
    zg = dn("zg_i", [512, S_core])
    qkv = dn("qkv_i", [4, 3, 128, S_core])
    gb = dn("gb_i", [4, 2, S_core])
    oc = dn("oc_i", [2, 512, S_core])
    st = dn("st_i", [1, 4 * 128 * 256])
    stall = dn("stall_i", [4, 4 * 128 * 256])
    x1in = dn("x1in_i", [HALO + S_core, D])
    hall = dn("hall_i", [4 * HALO, D])
    stv = st[0, :].rearrange("(h d c) -> h d c", h=4, d=128)
    NTs = 2 * S_core // 512 + NEXP - 1
    scr = {"x1f": dn("x1f_i", [S_core, D]), "s2t": nc.dram_tensor("s2t_i", [NTs * 512, 1], I32).ap(), "y": dn("y_i", [NTs * 512, D])}
    for l in range(depth):
        cx.push()
        sin = [cx.sb(f"sin{h}", [128, 128]) for h in range(4)]
        xin = xin0 if l == 0 else x1in
        phase_a(cx, S_core, xin, win[l], pv[l], yc, zg, qkv, gb)
        phase_b(cx, S_core, qkv, gb, pv[l], oc, stv)
        phase_x(cx, st, stall, pv[l], sin)
        last = (l == depth - 1)
        dst = xout if last else x1in[HALO:HALO + S_core, :]
        phase_c(cx, S_core, oc, yc, zg, xin, wout[l], lnrows[l], pv[l], moe if l % 2 == 1 else dense, dst, sin, scr)
        if not last:
            phase_h(cx, S_core, x1in, hall, pv[l])
        cx.pop()
    cx.pop()
    return nc


_CACHE = {}


def _pvec(inp, l, seg, S_core):
    f = np.float32
    pv = np.zeros((128, NPV), f)
    dw = np.asarray(inp["conv_dw_w"][l], f)
    pv[:, PV_DWW:PV_DWW + 124] = dw.reshape(31, 4, 128).transpose(2, 1, 0).reshape(128, 124)
    pv[:, PV_DWB:PV_DWB + 4] = np.asarray(inp["conv_dw_b"][l], f).reshape(4, 128).T
    pv[:, PV_CLG:PV_CLG + 4] = np.asarray(inp["conv_ln_g"][l], f).reshape(4, 128).T
    pv[:, PV_CLB:PV_CLB + 4] = np.asarray(inp["conv_ln_b"][l], f).reshape(4, 128).T
    sc = np.asarray(inp["short_conv_w"][l], f)
    pv[:, PV_SCW:PV_SCW + 48] = sc.reshape(4, 12, 128).transpose(2, 1, 0).reshape(128, 48)
    pv[:, PV_ONG] = np.asarray(inp["out_norm_g"][l], f)
    alog = np.asarray(inp["a_log"][l], f)
    dtb = np.asarray(inp["dt_bias"][l], f)
    p = np.arange(128)
    h128 = np.minimum(p // (S_core // 128), 3)
    pv[:, PV_ALOG128] = alog[h128]
    pv[:, PV_DTB128] = dtb[h128]
    for i in range(2):
        h64 = np.minimum((i * 128 + p) // (S_core // 64), 3)
        pv[:, PV_ALOG64 + i] = alog[h64]
        pv[:, PV_DTB64 + i] = dtb[h64]
    pv[:, PV_MSEG + seg] = 1.0
    if seg >= 1:
        pv[:, PV_MPRED + seg - 1] = 1.0
    return pv


def kernel(**inp):
    x = np.asarray(inp["x"], np.float32)
    B, S_tot, _ = x.shape
    S_core = S_tot // NSEG
    cores = list(range(NCORES))
    depth = inp["w_in"].shape[0]
    if ("f", S_core) not in _CACHE:
        _CACHE[("f", S_core)] = build_fused(S_core, depth)
    prog = _CACHE[("f", S_core)]
    ca = lambda a: np.ascontiguousarray(np.asarray(a, dtype=np.float32))
    lnrows = ca(np.stack([np.stack([np.broadcast_to(np.asarray(inp[k][l], np.float32), (128, D))
                                    for k in ("ln_mix_g", "ln_mix_b", "ln_ffn_g", "ln_ffn_b")], 0) for l in range(depth)], 0))
    shared = {"win": ca(inp["w_in"]), "wout": ca(inp["w_out"]), "lnrows": lnrows,
              "dwg": ca(inp["ffn_w_gate"][0:1]), "dwu": ca(inp["ffn_w_up"][0:1]), "dwd": ca(inp["ffn_w_down"][0:1]),
              "router": ca(inp["router_w"][0]), "mwg": ca(inp["moe_w_gate"][0]), "mwu": ca(inp["moe_w_up"][0]),
              "mwd": ca(inp["moe_w_down"][0])}
    in_maps = []
    for cidx in cores:
        b, sg = divmod(cidx, NSEG)
        buf = np.zeros((HALO + S_core, D), np.float32)
        lo = sg * S_core - HALO
        if lo >= 0:
            buf[:] = x[b, lo:lo + HALO + S_core]
        else:
            buf[HALO:] = x[b, 0:S_core]
        d = {"xin0": buf, "pv": ca(np.stack([_pvec(inp, l, sg, S_core) for l in range(depth)], 0))}
        d.update(shared)
        in_maps.append(d)
    res = run_bass_kernel_spmd(prog, in_maps, core_ids=cores).results
    out = np.empty_like(x)
    for cidx in cores:
        b, sg = divmod(cidx, NSEG)
        out[b, sg * S_core:(sg + 1) * S_core] = res[cidx]["xout"]
    return out
```

```python
import re
import numpy as np
import concourse.bass as bass
import concourse.mybir as mybir
from concourse.bass_utils import run_bass_kernel_spmd

F32 = mybir.dt.float32
BF16 = mybir.dt.bfloat16
I32 = mybir.dt.int32
AF = mybir.ActivationFunctionType
ALU = mybir.AluOpType
AX = mybir.AxisListType

D = 1024
NCORES = 8
NSEG = 4
HALO = 128
CW = 31
ALPHA = 4.0 ** 0.25
LN_EPS = 1e-5
RMS_EPS = 1e-6
L2_EPS = 1e-6
IN_COLS = 3080
DFF_DENSE = 2816
DFF_EXP = 3584
NEXP = 8
PV_DWW = 0
PV_DWB = 124
PV_CLG = 128
PV_CLB = 132
PV_SCW = 136
PV_ONG = 184
PV_ALOG128 = 185
PV_ALOG64 = 186
PV_DTB128 = 188
PV_DTB64 = 189
PV_MSEG = 192
PV_MPRED = 196
NPV = 200


class Sched:
    SEM_MAX = 30000

    def __init__(self, nc, dma_ring=12):
        self.nc = nc
        self.engs = {"pe": nc.tensor, "dve": nc.vector, "act": nc.scalar, "pool": nc.gpsimd, "sp": nc.sync}
        self.cnt = {e: 0 for e in self.engs}
        self.csem = {e: [] for e in self.engs}
        self.ring = {}
        self.ring_n = dma_ring
        self.dcnt = {e: 0 for e in self.engs}
        self.last_w = {}
        self.readers = {}
        self.seen = {e: {} for e in self.engs}
        self.nsem = 0
        self.ctoks = []

    def _newsem(self, name):
        self.nsem += 1
        return self.nc.alloc_semaphore(name=f"{name}_{self.nsem}")

    def _wait(self, eng, tok):
        sem, val, semid, src = tok
        if self.seen[eng].get(semid, 0) >= val:
            return
        if src == eng and eng == "pe":
            return
        self.engs[eng].wait_ge(sem, val)
        self.seen[eng][semid] = val

    def _deps(self, eng, reads, writes):
        toks = []
        for k in reads:
            if k in self.last_w:
                toks.append(self.last_w[k])
        for k in writes:
            if k in self.last_w:
                toks.append(self.last_w[k])
            toks.extend(self.readers.get(k, {}).values())
        for t in toks:
            self._wait(eng, t)

    def _record(self, tok, reads, writes):
        for k in reads:
            d = self.readers.setdefault(k, {})
            old = d.get(tok[2])
            if old is None or old[1] < tok[1]:
                d[tok[2]] = tok
        for k in writes:
            self.last_w[k] = tok
            self.readers[k] = {}

    def op(self, eng, fn, reads=(), writes=()):
        writes = list(writes) + [k for k in reads if k.startswith("ps")]
        reads = [k for k in reads if not k.startswith("ps")]
        self._deps(eng, reads, writes)
        inst = fn(self.engs[eng])
        n = self.cnt[eng]
        si, v = divmod(n, self.SEM_MAX)
        if si >= len(self.csem[eng]):
            self.csem[eng].append(self._newsem(f"c_{eng}"))
        sem = self.csem[eng][si]
        inst.then_inc(sem, 1)
        self.cnt[eng] = n + 1
        tok = (sem, v + 1, f"c_{eng}_{si}", eng)
        self._record(tok, reads, writes)
        return tok

    def dma(self, eng, out, in_, reads=(), writes=(), **kw):
        if eng not in self.ring:
            self.ring[eng] = [[self._newsem(f"d_{eng}"), 0] for _ in range(self.ring_n)]
        i = self.dcnt[eng]
        slot = self.ring[eng][i % self.ring_n]
        semid = f"d_{eng}_{i % self.ring_n}"
        if slot[1] > 0:
            self._wait(eng, (slot[0], 16 * slot[1], semid, "dma"))
        self._deps(eng, reads, writes)
        inst = self.engs[eng].dma_start(out=out, in_=in_, **kw)
        self.last_inst = inst
        slot[1] += 1
        inst.then_inc(slot[0], 16)
        self.dcnt[eng] = i + 1
        tok = (slot[0], 16 * slot[1], semid, "dma")
        self._record(tok, reads, writes)
        return tok

    def ind_dma(self, out, in_, idx_ap, gather, reads=(), writes=()):
        eng = "pool"
        if eng not in self.ring:
            self.ring[eng] = [[self._newsem(f"d_{eng}"), 0] for _ in range(self.ring_n)]
        i = self.dcnt[eng]
        slot = self.ring[eng][i % self.ring_n]
        semid = f"d_{eng}_{i % self.ring_n}"
        if slot[1] > 0:
            self._wait(eng, (slot[0], 16 * slot[1], semid, "dma"))
        self._deps(eng, reads, writes)
        off = bass.IndirectOffsetOnAxis(ap=idx_ap, axis=0)
        if gather:
            inst = self.nc.gpsimd.indirect_dma_start(out=out, out_offset=None, in_=in_, in_offset=off)
        else:
            inst = self.nc.gpsimd.indirect_dma_start(out=out, out_offset=off, in_=in_, in_offset=None)
        slot[1] += 1
        inst.then_inc(slot[0], 16)
        self.dcnt[eng] = i + 1
        tok = (slot[0], 16 * slot[1], semid, "dma")
        self._record(tok, reads, writes)
        return tok

    def coll(self, kind, in_ap, out_ap, groups, reads=(), writes=()):
        self._deps("pool", reads, writes)
        sem = self._newsem("cc")
        inst = self.nc.gpsimd.collective_compute(kind, ALU.bypass, replica_groups=groups, ins=[in_ap], outs=[out_ap])
        inst.then_inc(sem)
        tok = (sem, 1, f"cc_{self.nsem}", "dma")
        self.ctoks.append(tok)
        self._record(tok, reads, writes)
        return tok

    def barrier(self):
        toks = list(self.ctoks)
        for e in self.engs:
            n = self.cnt[e]
            if n:
                si, v = divmod(n - 1, self.SEM_MAX)
                toks.append((self.csem[e][si], v + 1, f"c_{e}_{si}", e))
        for q, slots in self.ring.items():
            for j, (sem, c) in enumerate(slots):
                if c:
                    toks.append((sem, 16 * c, f"d_{q}_{j}", "dma"))
        for e in self.engs:
            for t in toks:
                if t[3] == e:
                    continue
                self._wait(e, t)
        self.last_w = {}
        self.readers = {}


class Ctx:
    def __init__(self, nc):
        self.nc = nc
        self.S = Sched(nc)
        self.scopes = []
        self.ps = [nc.alloc_psum_tensor(f"psb{i}", [128, 512], F32) for i in range(8)]
        self.psk = [f"ps{i}" for i in range(8)]
        self.uid = 0

    def sb(self, name, shape, dt=F32):
        self.uid += 1
        g = self.nc.sbuf_tensor(f"{name}_{self.uid}", list(shape), dt)
        t = g.__enter__()
        self.scopes[-1].append(g)
        return t

    def push(self):
        self.scopes.append([])

    def pop(self):
        self.S.barrier()
        for g in reversed(self.scopes.pop()):
            g.__exit__(None, None, None)

    def consts(self):
        S = self.S
        c = {}
        c["ident"] = self.sb("ident", [128, 128])
        c["ones"] = self.sb("ones", [128, 128])
        S.op("pool", lambda e: e.memset(c["ident"][:], 1.0), writes=["c_ident"])
        S.op("pool", lambda e: e.affine_select(out=c["ident"][:], in_=c["ident"][:], pattern=[[-1, 128]],
                                               compare_op=ALU.is_equal, fill=0.0, base=0, channel_multiplier=1),
             reads=["c_ident"], writes=["c_ident"])
        S.op("pool", lambda e: e.memset(c["ones"][:], 1.0), writes=["c_ones"])
        self.c = c
        return c


def phase_a(cx, S_core, xin, win, pv_d, yc_d, zg_d, qkv_d, gb_d):
    nc, S, c = cx.nc, cx.S, cx.c
    T = min(256, S_core)
    cx.push()
    wbf = cx.sb("wbf", [128, 8, IN_COLS], BF16)
    winv = win.rearrange("(kc p) n -> p kc n", p=128)
    for kc in range(8):
        S.dma("pool", wbf[:, kc, :], winv[:, kc, :], writes=[f"wbf{kc}"])
    wkeys = [f"wbf{kc}" for kc in range(8)]
    pv = cx.sb("pv", [128, NPV])
    S.dma("sp", pv[:], pv_d[:, :], writes=["pv"])
    o512 = cx.sb("o512", [128, 128])
    S.op("pool", lambda e: e.memset(o512[:], 1.0 / 512.0), writes=["o512"])
    xt = [cx.sb(f"xt{i}", [128, 2, D]) for i in range(2)]
    xT = cx.sb("xT", [128, 8, T], BF16)
    U = cx.sb("U", [128, 4, 30 + T], BF16)
    dgw = cx.sb("dgw", [128, 4, CW, 128], BF16)
    for cc in range(4):
        for j in range(CW):
            eng = "pool" if (cc * CW + j) % 2 == 0 else "dve"
            S.op(eng, lambda e, cc=cc, j=j: e.tensor_scalar(out=dgw[:, cc, j, :], in0=c["ident"][:],
                                                            scalar1=pv[:, PV_DWW + cc * 31 + j:PV_DWW + cc * 31 + j + 1], scalar2=None, op0=ALU.mult),
                 reads=["c_ident", "pv"], writes=[f"dgw{cc}_{eng}"])
    PRE = cx.sb("PRE", [128, 12, 3 + T])
    sg = cx.sb("sg", [128, 4, T])
    acc = cx.sb("acc", [128, 4, T])
    sq = [cx.sb(f"sq{i}", [128, T]) for i in range(2)]
    mean = cx.sb("mean", [128, T])
    rstd = cx.sb("rstd", [128, T])
    ycs = cx.sb("ycs", [128, 4, T])
    zgs = cx.sb("zgs", [128, 4, T])
    qa = [cx.sb(f"qa{i}", [128, T]) for i in range(12)]
    lg = cx.sb("lg", [8, T])
    ps, pk = cx.ps, cx.psk
    pctr = [0]

    def bank():
        i = pctr[0] % 8
        pctr[0] += 1
        return ps[i], pk[i]

    ntiles = S_core // T
    tiles = [(-1, 128)] + [(i, T) for i in range(ntiles)]

    def xload(it):
        ti, Tt = tiles[it]
        r0 = 0 if ti < 0 else HALO + ti * T
        S.dma("sp", xt[it % 2][:, 0:Tt // 128, :], xin[r0:r0 + Tt, :].rearrange("(s p) d -> p s d", p=128), writes=[f"xt{it % 2}"])

    def stage1(it):
        ti, Tt = tiles[it]
        nsub = Tt // 128
        xb = xt[it % 2]
        xk = f"xt{it % 2}"
        if it < 2:
            xload(it)
        for kc in range(8):
            pb, pkk = bank()
            if kc == 7 and it + 2 < len(tiles):
                pass
            for s_ in range(nsub):
                S.op("pe", lambda e, s_=s_, kc=kc, pb=pb: e.transpose(out=pb[:, s_ * 128:(s_ + 1) * 128],
                                                                       in_=xb[:, s_, kc * 128:(kc + 1) * 128],
                                                                       identity=c["ident"][:]),
                     reads=[xk, "c_ident"], writes=[pkk])
            if kc % 2 == 0:
                S.op("act", lambda e, kc=kc, pb=pb: e.activation(out=xT[:, kc, 0:Tt], in_=pb[:, 0:Tt], func=AF.Copy),
                     reads=[pkk], writes=[f"xT{kc}"])
            else:
                S.op("dve", lambda e, kc=kc, pb=pb: e.tensor_copy(out=xT[:, kc, 0:Tt], in_=pb[:, 0:Tt]),
                     reads=[pkk], writes=[f"xT{kc}"])
        xTk = [f"xT{kc}" for kc in range(8)]
        if it + 2 < len(tiles):
            xload(it + 2)

        def hchunk(oc):
            pb, pkk = bank()
            for kc in range(8):
                S.op("pe", lambda e, kc=kc, pb=pb: e.matmul(pb[:, 0:Tt], lhsT=wbf[:, kc, oc * 128:(oc + 1) * 128],
                                                            rhs=xT[:, kc, 0:Tt], start=(kc == 0), stop=(kc == 7)),
                     reads=[wkeys[kc], xTk[kc]], writes=[pkk])
            return pb, pkk

        for cc in range(4):
            pb, pkk = hchunk(4 + cc)
            S.op("act", lambda e, cc=cc, pb=pb: e.activation(out=sg[:, cc, 0:Tt], in_=pb[:, 0:Tt], func=AF.Sigmoid),
                 reads=[pkk], writes=[f"sg{cc}"])
        for cc in range(4):
            pb, pkk = hchunk(cc)
            S.op("dve", lambda e, cc=cc, pb=pb: e.tensor_tensor(out=U[:, cc, 30:30 + Tt], in0=pb[:, 0:Tt],
                                                                in1=sg[:, cc, 0:Tt], op=ALU.mult),
                 reads=[pkk, f"sg{cc}"], writes=[f"U{cc}"])
        for j in range(12):
            pb, pkk = hchunk(8 + j)
            S.op("act", lambda e, j=j, pb=pb: e.activation(out=PRE[:, j, 3:3 + Tt], in_=pb[:, 0:Tt], func=AF.Copy),
                 reads=[pkk], writes=[f"PRE{j}"])
        if ti < 0:
            for cc in range(4):
                S.op("pool", lambda e, cc=cc: e.tensor_copy(out=U[:, cc, 0:30], in_=U[:, cc, Tt:Tt + 30]),
                     reads=[f"U{cc}"], writes=[f"U{cc}"])
            for j in range(12):
                S.op("pool", lambda e, j=j: e.tensor_copy(out=PRE[:, j, 0:3], in_=PRE[:, j, Tt:Tt + 3]),
                     reads=[f"PRE{j}"], writes=[f"PRE{j}"])
            return
        t0 = ti * T
        for cc in range(4):
            pb, pkk = hchunk(20 + cc)
            S.op("act", lambda e, cc=cc, pb=pb: e.activation(out=zgs[:, cc, 0:Tt], in_=pb[:, 0:Tt], func=AF.Silu),
                 reads=[pkk], writes=[f"zgs{cc}"])
            S.dma("pool", zg_d[cc * 128:(cc + 1) * 128, t0:t0 + Tt], zgs[:, cc, 0:Tt], reads=[f"zgs{cc}"], writes=[f"zg_d{cc}"])
        pb, pkk = bank()
        for kc in range(8):
            S.op("pe", lambda e, kc=kc, pb=pb: e.matmul(pb[0:8, 0:Tt], lhsT=wbf[:, kc, 3072:3080], rhs=xT[:, kc, 0:Tt],
                                                        start=(kc == 0), stop=(kc == 7)),
                 reads=[wkeys[kc], xTk[kc]], writes=[pkk])
        S.op("act", lambda e, pb=pb: e.activation(out=lg[0:8, 0:Tt], in_=pb[0:8, 0:Tt], func=AF.Copy), reads=[pkk], writes=["lg"])
        for w in range(2):
            for h in range(4):
                S.dma("pool", gb_d[h, w:w + 1, t0:t0 + Tt], lg[w * 4 + h:w * 4 + h + 1, 0:Tt], reads=["lg"], writes=[f"gb_d{w}{h}"])

    def conv(it):
        ti, Tt = tiles[it]
        for cc in range(4):
            pb, pkk = bank()
            for j in range(CW):
                S.op("pe", lambda e, cc=cc, j=j, pb=pb: e.matmul(pb[:, 0:Tt], lhsT=dgw[:, cc, j, :], rhs=U[:, cc, j:j + Tt],
                                                                 start=(j == 0), stop=(j == CW - 1)),
                     reads=[f"dgw{cc}_pool", f"dgw{cc}_dve", f"U{cc}"], writes=[pkk])
            S.op("act", lambda e, cc=cc, pb=pb: e.activation(out=acc[:, cc, 0:Tt], in_=pb[:, 0:Tt], func=AF.Identity,
                                                             bias=pv[:, PV_DWB + cc:PV_DWB + cc + 1]),
                 reads=[pkk, "pv"], writes=[f"acc{cc}"])
            S.op("pool", lambda e, cc=cc: e.tensor_copy(out=U[:, cc, 0:30], in_=U[:, cc, Tt:Tt + 30]),
                 reads=[f"U{cc}"], writes=[f"U{cc}"])

    def shortconv(it):
        ti, Tt = tiles[it]
        t0 = ti * T
        for j in range(12):
            which, h = j // 4, j % 4
            qb, qk = qa[j], f"qa{j}"
            S.op("dve", lambda e, j=j, qb=qb: e.tensor_scalar(out=qb[:, 0:Tt], in0=PRE[:, j, 0:Tt],
                                                              scalar1=pv[:, PV_SCW + j * 4:PV_SCW + j * 4 + 1], scalar2=None,
                                                              op0=ALU.mult),
                 reads=[f"PRE{j}", "pv"], writes=[qk])
            for tp in range(1, 4):
                S.op("dve", lambda e, j=j, tp=tp, qb=qb: e.scalar_tensor_tensor(
                    out=qb[:, 0:Tt], in0=PRE[:, j, tp:tp + Tt], scalar=pv[:, PV_SCW + j * 4 + tp:PV_SCW + j * 4 + tp + 1],
                    in1=qb[:, 0:Tt], op0=ALU.mult, op1=ALU.add),
                     reads=[f"PRE{j}", qk], writes=[qk])
            S.op("pool", lambda e, j=j: e.tensor_copy(out=PRE[:, j, 0:3], in_=PRE[:, j, Tt:Tt + 3]),
                 reads=[f"PRE{j}"], writes=[f"PRE{j}"])
            S.op("act", lambda e, qb=qb: e.activation(out=qb[:, 0:Tt], in_=qb[:, 0:Tt], func=AF.Silu), reads=[qk], writes=[qk])
            if which == 2:
                S.dma("pool", qkv_d[h, which, :, t0:t0 + Tt], qb[:, 0:Tt], reads=[qk], writes=[f"qkv_d{j}"])

    def finish(it):
        ti, Tt = tiles[it]
        t0 = ti * T
        pm, pmk = bank()
        pq, pqk = bank()
        for cc in range(4):
            S.op("pe", lambda e, cc=cc: e.matmul(pm[:, 0:Tt], lhsT=o512[:], rhs=acc[:, cc, 0:Tt], start=(cc == 0), stop=(cc == 3)),
                 reads=["o512", f"acc{cc}"], writes=[pmk])
        for cc in range(4):
            sb_, sk = sq[cc % 2], f"sq{cc % 2}"
            S.op("act", lambda e, cc=cc, sb_=sb_: e.activation(out=sb_[:, 0:Tt], in_=acc[:, cc, 0:Tt], func=AF.Square),
                 reads=[f"acc{cc}"], writes=[sk])
            S.op("pe", lambda e, cc=cc, sb_=sb_: e.matmul(pq[:, 0:Tt], lhsT=o512[:], rhs=sb_[:, 0:Tt], start=(cc == 0), stop=(cc == 3)),
                 reads=["o512", sk], writes=[pqk])
        S.op("act", lambda e: e.activation(out=mean[:, 0:Tt], in_=pm[:, 0:Tt], func=AF.Copy), reads=[pmk], writes=["mean"])
        S.op("dve", lambda e: e.tensor_tensor(out=rstd[:, 0:Tt], in0=mean[:, 0:Tt], in1=mean[:, 0:Tt], op=ALU.mult),
             reads=["mean"], writes=["rstd"])
        S.op("dve", lambda e: e.tensor_tensor(out=rstd[:, 0:Tt], in0=pq[:, 0:Tt], in1=rstd[:, 0:Tt], op=ALU.subtract),
             reads=[pqk, "rstd"], writes=["rstd"])
        S.op("dve", lambda e: e.tensor_scalar(out=rstd[:, 0:Tt], in0=rstd[:, 0:Tt], scalar1=0.0, scalar2=LN_EPS,
                                              op0=ALU.max, op1=ALU.add), reads=["rstd"], writes=["rstd"])
        S.op("act", lambda e: e.activation(out=rstd[:, 0:Tt], in_=rstd[:, 0:Tt], func=AF.Sqrt), reads=["rstd"], writes=["rstd"])
        S.op("dve", lambda e: e.reciprocal(out=rstd[:, 0:Tt], in_=rstd[:, 0:Tt]), reads=["rstd"], writes=["rstd"])
        for cc in range(4):
            S.op("dve", lambda e, cc=cc: e.tensor_tensor(out=acc[:, cc, 0:Tt], in0=acc[:, cc, 0:Tt], in1=mean[:, 0:Tt], op=ALU.subtract),
                 reads=[f"acc{cc}", "mean"], writes=[f"acc{cc}"])
            S.op("dve", lambda e, cc=cc: e.tensor_tensor(out=acc[:, cc, 0:Tt], in0=acc[:, cc, 0:Tt], in1=rstd[:, 0:Tt], op=ALU.mult),
                 reads=[f"acc{cc}", "rstd"], writes=[f"acc{cc}"])
            S.op("act", lambda e, cc=cc: e.activation(out=ycs[:, cc, 0:Tt], in_=acc[:, cc, 0:Tt], func=AF.Silu,
                                                      scale=pv[:, PV_CLG + cc:PV_CLG + cc + 1],
                                                      bias=pv[:, PV_CLB + cc:PV_CLB + cc + 1]),
                 reads=[f"acc{cc}", "pv"], writes=[f"ycs{cc}"])
            S.dma("pool", yc_d[cc * 128:(cc + 1) * 128, t0:t0 + Tt], ycs[:, cc, 0:Tt], reads=[f"ycs{cc}"], writes=[f"yc_d{cc}"])
        for j in range(8):
            which, h = j // 4, j % 4
            qb, qk = qa[j], f"qa{j}"
            sb_, sk = sq[j % 2], f"sq{j % 2}"
            pb, pkk = bank()
            S.op("act", lambda e, qb=qb, sb_=sb_: e.activation(out=sb_[:, 0:Tt], in_=qb[:, 0:Tt], func=AF.Square), reads=[qk], writes=[sk])
            S.op("pe", lambda e, pb=pb, sb_=sb_: e.matmul(pb[:, 0:Tt], lhsT=c["ones"][:], rhs=sb_[:, 0:Tt], start=True, stop=True),
                 reads=["c_ones", sk], writes=[pkk])
            sc = 128.0 if which == 0 else 1.0
            S.op("dve", lambda e, pb=pb, sc=sc, sb_=sb_: e.tensor_scalar(out=sb_[:, 0:Tt], in0=pb[:, 0:Tt], scalar1=L2_EPS, scalar2=sc,
                                                                         op0=ALU.add, op1=ALU.mult), reads=[pkk], writes=[sk])
            S.op("act", lambda e, sb_=sb_: e.activation(out=sb_[:, 0:Tt], in_=sb_[:, 0:Tt], func=AF.Sqrt), reads=[sk], writes=[sk])
            S.op("dve", lambda e, sb_=sb_: e.reciprocal(out=sb_[:, 0:Tt], in_=sb_[:, 0:Tt]), reads=[sk], writes=[sk])
            S.op("dve", lambda e, qb=qb, sb_=sb_: e.tensor_tensor(out=qb[:, 0:Tt], in0=qb[:, 0:Tt], in1=sb_[:, 0:Tt], op=ALU.mult),
                 reads=[qk, sk], writes=[qk])
            S.dma("pool", qkv_d[h, which, :, t0:t0 + Tt], qb[:, 0:Tt], reads=[qk], writes=[f"qkv_d{j}"])

    stage1(0)
    stage1(1)
    for it in range(1, len(tiles)):
        conv(it)
        shortconv(it)
        if it + 1 < len(tiles):
            stage1(it + 1)
        finish(it)
    cx.pop()


def phase_b(cx, S_core, qkv_r, gb_r, pv_d, oc_d, st_d):
    nc, S, c = cx.nc, cx.S, cx.c
    ident, ones = c["ident"], c["ones"]
    cx.push()
    NB = 4 * S_core // 128
    NCH = 2 * NB
    rps128 = S_core // 128
    rps64 = S_core // 64
    pv = cx.sb("pvb", [128, NPV])
    S.dma("sp", pv[:], pv_d[:, :], writes=["pv"])
    triBD = cx.sb("triBD", [128, 128])
    maskpos = cx.sb("maskpos", [128, 128])
    strict = cx.sb("strict", [128, 128])
    selL = cx.sb("selL", [128, 128])
    sel63 = cx.sb("sel63", [128, 128])
    negexpa = cx.sb("negexpa", [128, 3])
    S.op("pool", lambda e: e.memset(triBD[:], 1.0), writes=["triBD"])
    S.op("pool", lambda e: e.affine_select(out=triBD[:], in_=triBD[:], pattern=[[1, 128]], compare_op=ALU.is_ge,
                                           fill=0.0, base=0, channel_multiplier=-1), reads=["triBD"], writes=["triBD"])
    S.op("pool", lambda e: e.memset(triBD[0:64, 64:128], 0.0), reads=["triBD"], writes=["triBD"])
    S.op("pool", lambda e: e.memset(maskpos[:], 0.0), writes=["maskpos"])
    S.op("pool", lambda e: e.affine_select(out=maskpos[:], in_=maskpos[:], pattern=[[-1, 128]], compare_op=ALU.is_ge,
                                           fill=1e9, base=0, channel_multiplier=1), reads=["maskpos"], writes=["maskpos"])
    S.op("pool", lambda e: e.memset(maskpos[64:128, 0:64], 1e9), reads=["maskpos"], writes=["maskpos"])
    S.op("pool", lambda e: e.memset(strict[:], 1.0), writes=["strict"])
    S.op("pool", lambda e: e.affine_select(out=strict[:], in_=strict[:], pattern=[[-1, 128]], compare_op=ALU.is_ge,
                                           fill=0.0, base=-1, channel_multiplier=1), reads=["strict"], writes=["strict"])
    S.op("pool", lambda e: e.memset(strict[64:128, 0:64], 0.0), reads=["strict"], writes=["strict"])
    S.op("pool", lambda e: e.memset(selL[:], 1.0), writes=["selL"])
    S.op("pool", lambda e: e.affine_select(out=selL[:], in_=selL[:], pattern=[[-64, 2], [0, 64]], compare_op=ALU.is_equal,
                                           fill=0.0, base=-63, channel_multiplier=1), reads=["selL"], writes=["selL"])
    S.op("pool", lambda e: e.memset(sel63[:], 1.0), writes=["sel63"])
    S.op("pool", lambda e: e.affine_select(out=sel63[:], in_=sel63[:], pattern=[[0, 128]], compare_op=ALU.is_equal,
                                           fill=0.0, base=-63, channel_multiplier=1), reads=["sel63"], writes=["sel63"])
    S.op("act", lambda e: e.activation(out=negexpa[:], in_=pv[:, PV_ALOG128:PV_ALOG128 + 3], func=AF.Exp), reads=["pv"], writes=["nea"])
    S.op("dve", lambda e: e.tensor_scalar(out=negexpa[:], in0=negexpa[:], scalar1=-1.0, scalar2=None, op0=ALU.mult),
         reads=["nea"], writes=["nea"])
    ps, pk = cx.ps, cx.psk

    def load_rows(w, width, rps, nrows, name):
        ntile = (nrows + 127) // 128
        tl = [cx.sb(f"{name}{i}", [128, width]) for i in range(ntile)]
        for seg in range(NSEG):
            r = seg * rps
            S.dma("sp", tl[r // 128][r % 128:r % 128 + rps, :], gb_r[seg, w, :].rearrange("(r t) -> r t", t=width),
                  writes=[f"{name}{r // 128}"])
        return tl, ntile

    def gbeta_rows(tl_b, tl_a, ntile, nrows, width, name, c0):
        tmp = cx.sb(f"{name}_tmp", [128, width])
        tmp2 = cx.sb(f"{name}_tmp2", [128, width])
        for i in range(ntile):
            n_p = min(128, nrows - i * 128)
            kb, ka = f"{name}b{i}", f"{name}a{i}"
            S.op("act", lambda e, i=i: e.activation(out=tl_b[i][0:n_p, :], in_=tl_b[i][0:n_p, :], func=AF.Sigmoid), reads=[kb], writes=[kb])
            S.op("dve", lambda e, i=i: e.tensor_scalar(out=tl_a[i][0:n_p, :], in0=tl_a[i][0:n_p, :], scalar1=pv[0:n_p, PV_DTB128 + c0 + i:PV_DTB128 + c0 + i + 1],
                                                       scalar2=None, op0=ALU.add), reads=[ka, "pv"], writes=[ka])
            S.op("act", lambda e, i=i: e.activation(out=tmp[0:n_p, :], in_=tl_a[i][0:n_p, :], func=AF.Abs),
                 reads=[ka], writes=[f"{name}tmp"])
            S.op("act", lambda e: e.activation(out=tmp[0:n_p, :], in_=tmp[0:n_p, :], func=AF.Exp, scale=-1.0), reads=[f"{name}tmp"], writes=[f"{name}tmp"])
            S.op("act", lambda e: e.activation(out=tmp[0:n_p, :], in_=tmp[0:n_p, :], func=AF.Ln, bias=1.0), reads=[f"{name}tmp"], writes=[f"{name}tmp"])
            S.op("dve", lambda e, i=i: e.tensor_scalar(out=tmp2[0:n_p, :], in0=tl_a[i][0:n_p, :], scalar1=0.0, scalar2=None, op0=ALU.max),
                 reads=[ka], writes=[f"{name}tmp2"])
            S.op("dve", lambda e: e.tensor_tensor(out=tmp[0:n_p, :], in0=tmp[0:n_p, :], in1=tmp2[0:n_p, :], op=ALU.add),
                 reads=[f"{name}tmp", f"{name}tmp2"], writes=[f"{name}tmp"])
            S.op("dve", lambda e, i=i: e.tensor_scalar(out=tl_a[i][0:n_p, :], in0=tmp[0:n_p, :], scalar1=negexpa[0:n_p, c0 + i:c0 + i + 1], scalar2=None, op0=ALU.mult),
                 reads=[f"{name}tmp", "nea"], writes=[ka])

    def transpose_rows(tl, ntile, nrows, width, dst, dkey, key):
        for i in range(ntile):
            n_p = min(128, nrows - i * 128)
            S.op("pe", lambda e, i=i: e.transpose(out=ps[6][0:width, 0:n_p], in_=tl[i][0:n_p, :], identity=ident[0:n_p, 0:n_p]),
                 reads=[f"{key}{i}", "c_ident"], writes=[pk[6]])
            S.op("dve", lambda e, i=i: e.tensor_copy(out=dst[0:width, i * 128:i * 128 + n_p], in_=ps[6][0:width, 0:n_p]),
                 reads=[pk[6]], writes=[dkey])

    b128r, nt128 = load_rows(0, 128, rps128, NB, "r128b")
    a128r, _ = load_rows(1, 128, rps128, NB, "r128a")
    b64r, nt64 = load_rows(0, 64, rps64, NCH, "r64b")
    a64r, _ = load_rows(1, 64, rps64, NCH, "r64a")
    gbeta_rows(b128r, a128r, nt128, NB, 128, "r128", 0)
    gbeta_rows(b64r, a64r, nt64, NCH, 64, "r64", 1)
    beta128 = cx.sb("beta128", [128, NB])
    g128 = cx.sb("g128", [128, NB])
    gc128 = cx.sb("gc128", [128, NB])
    egc128 = cx.sb("egc128", [128, NB])
    nbeta128 = cx.sb("nbeta128", [128, NB])
    beg128 = cx.sb("beg128", [128, NB])
    g64 = cx.sb("g64", [64, NCH])
    gc64 = cx.sb("gc64", [64, NCH])
    kdsc64 = cx.sb("kdsc64", [64, NCH])
    egl = cx.sb("egl", [128, NCH])
    transpose_rows(b128r, nt128, NB, 128, beta128, "beta128", "r128b")
    transpose_rows(a128r, nt128, NB, 128, g128, "g128", "r128a")
    transpose_rows(a64r, nt64, NCH, 64, g64, "g64", "r64a")
    S.op("pe", lambda e: e.matmul(ps[6][:, 0:NB], lhsT=triBD[:], rhs=g128[:, :], start=True, stop=True), reads=["triBD", "g128"], writes=[pk[6]])
    S.op("dve", lambda e: e.tensor_copy(out=gc128[:, :], in_=ps[6][:, 0:NB]), reads=[pk[6]], writes=["gc128"])
    S.op("act", lambda e: e.activation(out=egc128[:, :], in_=gc128[:, :], func=AF.Exp), reads=["gc128"], writes=["egc128"])
    S.op("dve", lambda e: e.tensor_scalar(out=nbeta128[:, :], in0=beta128[:, :], scalar1=-1.0, scalar2=None, op0=ALU.mult),
         reads=["beta128"], writes=["nbeta128"])
    S.op("dve", lambda e: e.tensor_tensor(out=beg128[:, :], in0=beta128[:, :], in1=egc128[:, :], op=ALU.mult),
         reads=["beta128", "egc128"], writes=["beg128"])
    S.op("pe", lambda e: e.matmul(ps[7][0:64, 0:NCH], lhsT=triBD[0:64, 0:64], rhs=g64[:, :], start=True, stop=True),
         reads=["triBD", "g64"], writes=[pk[7]])
    S.op("dve", lambda e: e.tensor_copy(out=gc64[:, :], in_=ps[7][0:64, 0:NCH]), reads=[pk[7]], writes=["gc64"])
    S.op("pe", lambda e: e.matmul(ps[6][0:64, 0:NCH], lhsT=sel63[0:64, 0:64], rhs=gc64[:, :], start=True, stop=True),
         reads=["sel63", "gc64"], writes=[pk[6]])
    S.op("dve", lambda e: e.tensor_tensor(out=kdsc64[:, :], in0=ps[6][0:64, 0:NCH], in1=gc64[:, :], op=ALU.subtract),
         reads=[pk[6], "gc64"], writes=["kdsc64"])
    S.op("act", lambda e: e.activation(out=kdsc64[:, :], in_=kdsc64[:, :], func=AF.Exp), reads=["kdsc64"], writes=["kdsc64"])
    S.op("pe", lambda e: e.matmul(ps[7][:, 0:NCH], lhsT=sel63[0:64, :], rhs=gc64[:, :], start=True, stop=True),
         reads=["sel63", "gc64"], writes=[pk[7]])
    S.op("act", lambda e: e.activation(out=egl[:, :], in_=ps[7][:, 0:NCH], func=AF.Exp), reads=[pk[7]], writes=["egl"])

    NBh = S_core // 128
    GRP = min(4, NBh)
    qkv = [cx.sb(f"qkv{i}", [128, 3, GRP * 128]) for i in range(2)]
    oc = [cx.sb(f"oc{i}", [128, 2, GRP * 128]) for i in range(2)]
    St = [[cx.sb(f"St{h}_{i}", [128, 256]) for i in range(2)] for h in range(4)]
    sidx = [0, 0, 0, 0]
    for h in range(4):
        S.op("pool", lambda e, h=h: e.memset(St[h][0][:, 0:128], 0.0), writes=[f"St{h}_0"])
        S.op("pool", lambda e, h=h: e.tensor_copy(out=St[h][0][:, 128:256], in_=ident[:]), reads=["c_ident", f"St{h}_0"], writes=[f"St{h}_0"])
    W = {}
    for par in range(2):
        for b in range(GRP):
            for nm, shp in [("kbg", [128, 128]), ("vb", [128, 128]), ("kdec", [64, 256]), ("dg", [128, 256]), ("tmp", [128, 128]),
                            ("Dm", [128, 128]), ("Ds", [128, 128]), ("TT", [128, 128]), ("Pf", [128, 128]),
                            ("attn", [128, 128]), ("attnT", [64, 256]), ("qg", [128, 128]), ("u", [64, 2, 256]), ("wT", [128, 128])]:
                W[(nm, b, par)] = cx.sb(f"{nm}{b}_{par}", shp)
            for nm in ("P", "PT", "TTb"):
                W[(nm, b, par)] = cx.sb(f"{nm}{b}_{par}", [128, 128], BF16)
            S.op("pool", lambda e, b=b, par=par: e.memset(W[("u", b, par)][:, :, :], 0.0), writes=[f"u{b}_{par}"])
    vnew = [cx.sb(f"vnew{i}", [64, 256]) for i in range(2)]
    pctr = [0]

    def bank():
        i = pctr[0] % 5
        pctr[0] += 1
        return ps[i], pk[i]

    groups = [(g, h) for g in range(NBh // GRP) for h in range(4)]
    blocks = list(range(GRP))

    def prepass(gi):
        g, h = groups[gi]
        par = gi % 2
        st = g * GRP * 128
        qb = qkv[par]
        qk = f"qkv{par}"
        K = lambda nm, b: f"{nm}{b}_{par}"
        Wp = lambda nm, b: W[(nm, b, par)]
        bk = {}
        stages = []


        def s_p1():
            for b in blocks:
                n = h * NBh + g * GRP + b
                cs = slice(b * 128, (b + 1) * 128)
                pb, pkk = bank()
                S.op("pe", lambda e, pb=pb, cs=cs: e.transpose(out=pb[:, 0:128], in_=qb[:, 1, cs], identity=ident[:]), reads=[qk, "c_ident"], writes=[pkk])
                S.op("pe", lambda e, pb=pb, cs=cs: e.transpose(out=pb[:, 128:256], in_=qb[:, 2, cs], identity=ident[:]), reads=[qk, "c_ident"], writes=[pkk])
                S.op("pe", lambda e, pb=pb, b=b: e.transpose(out=pb[0:64, 256:384], in_=qb[:, 1, b * 128 + 64:b * 128 + 128], identity=ident[:]),
                     reads=[qk, "c_ident"], writes=[pkk])
                S.op("act", lambda e, pb=pb, b=b, n=n: e.activation(out=Wp("kbg", b)[:], in_=pb[:, 0:128], func=AF.Copy, scale=beg128[:, n:n + 1]),
                     reads=[pkk, "beg128"], writes=[K("kbg", b)])
                S.op("dve", lambda e, pb=pb, b=b, n=n: e.tensor_scalar(out=Wp("vb", b)[:], in0=pb[:, 128:256], scalar1=beta128[:, n:n + 1], scalar2=None, op0=ALU.mult),
                     reads=[pkk, "beta128"], writes=[K("vb", b)])
                S.op("dve", lambda e, pb=pb, b=b, n=n: e.tensor_scalar(out=Wp("kdec", b)[:, 0:128], in0=pb[0:64, 0:128], scalar1=kdsc64[:, 2 * n:2 * n + 1], scalar2=None, op0=ALU.mult),
                     reads=[pkk, "kdsc64"], writes=[K("kdec", b)])
                S.op("act", lambda e, pb=pb, b=b, n=n: e.activation(out=Wp("kdec", b)[:, 128:256], in_=pb[0:64, 256:384], func=AF.Copy, scale=kdsc64[:, 2 * n + 1:2 * n + 2]),
                     reads=[pkk, "kdsc64"], writes=[K("kdec", b) + "b"])
                S.op("pool", lambda e, b=b, n=n: e.tensor_scalar(out=Wp("dg", b)[:, 0:128], in0=ident[:], scalar1=gc128[:, n:n + 1], scalar2=None, op0=ALU.mult),
                     reads=["c_ident", "gc128"], writes=[K("dg", b)])
                S.op("pool", lambda e, b=b, n=n: e.tensor_scalar(out=Wp("dg", b)[:, 128:256], in0=ident[:], scalar1=egc128[:, n:n + 1], scalar2=None, op0=ALU.mult),
                     reads=["c_ident", "egc128"], writes=[K("dg", b)])
        stages.append(s_p1)

        def s_p2():
            for b in blocks:
                cs = slice(b * 128, (b + 1) * 128)
                pb, pkk = bank()
                bk[b] = (pb, pkk)
                S.op("pe", lambda e, pb=pb, cs=cs: e.matmul(pb[:, 0:128], lhsT=qb[:, 1, cs], rhs=qb[:, 1, cs], start=True, stop=True), reads=[qk], writes=[pkk])
                S.op("pe", lambda e, pb=pb, cs=cs: e.matmul(pb[:, 128:256], lhsT=qb[:, 0, cs], rhs=qb[:, 1, cs], start=True, stop=True), reads=[qk], writes=[pkk])
                S.op("pe", lambda e, pb=pb, b=b: e.matmul(pb[:, 256:512], lhsT=ones[:], rhs=Wp("dg", b)[:, :], start=True, stop=True),
                     reads=["c_ones", K("dg", b)], writes=[pkk])
        stages.append(s_p2)

        def s_p3():
            for b in blocks:
                n = h * NBh + g * GRP + b
                cs = slice(b * 128, (b + 1) * 128)
                pb, pkk = bk[b]
                S.op("dve", lambda e, pb=pb, b=b, n=n: e.scalar_tensor_tensor(out=Wp("tmp", b)[:], in0=pb[:, 256:384], scalar=gc128[:, n:n + 1],
                                                                              in1=maskpos[:], op0=ALU.subtract, op1=ALU.add),
                     reads=[pkk, "gc128", "maskpos"], writes=[K("tmp", b)])
                S.op("act", lambda e, b=b: e.activation(out=Wp("Dm", b)[:], in_=Wp("tmp", b)[:], func=AF.Exp, scale=-1.0),
                     reads=[K("tmp", b)], writes=[K("Dm", b)])
                S.op("pool", lambda e, b=b: e.tensor_tensor(out=Wp("Ds", b)[:], in0=Wp("Dm", b)[:], in1=strict[:], op=ALU.mult),
                     reads=[K("Dm", b), "strict"], writes=[K("Ds", b)])
                S.op("dve", lambda e, pb=pb, b=b, n=n: e.scalar_tensor_tensor(out=Wp("Pf", b)[:], in0=pb[:, 0:128], scalar=nbeta128[:, n:n + 1],
                                                                              in1=Wp("Ds", b)[:], op0=ALU.mult, op1=ALU.mult),
                     reads=[pkk, "nbeta128", K("Ds", b)], writes=[K("Pf", b)])
                S.op("pool", lambda e, b=b: e.tensor_copy(out=Wp("P", b)[:], in_=Wp("Pf", b)[:]), reads=[K("Pf", b)], writes=[K("P", b)])
                S.op("dve", lambda e, pb=pb, b=b: e.tensor_tensor(out=Wp("attn", b)[:], in0=pb[:, 128:256], in1=Wp("Dm", b)[:], op=ALU.mult),
                     reads=[pkk, K("Dm", b)], writes=[K("attn", b)])
                S.op("dve", lambda e, pb=pb, b=b, cs=cs: e.tensor_tensor(out=Wp("qg", b)[:], in0=qb[:, 0, cs], in1=pb[:, 384:512], op=ALU.mult),
                     reads=[pkk, qk], writes=[K("qg", b)])
        stages.append(s_p3)

        def s_p4():
            for b in blocks:
                pb, pkk = bank()
                S.op("pe", lambda e, pb=pb, b=b: e.transpose(out=pb[:, 0:128], in_=Wp("Pf", b)[:], identity=ident[:]), reads=[K("Pf", b), "c_ident"], writes=[pkk])
                S.op("pe", lambda e, pb=pb, b=b: e.transpose(out=pb[0:64, 128:256], in_=Wp("attn", b)[:, 0:64], identity=ident[:]), reads=[K("attn", b), "c_ident"], writes=[pkk])
                S.op("pe", lambda e, pb=pb, b=b: e.transpose(out=pb[0:64, 256:384], in_=Wp("attn", b)[:, 64:128], identity=ident[:]), reads=[K("attn", b), "c_ident"], writes=[pkk])
                S.op("act", lambda e, pb=pb, b=b: e.activation(out=Wp("PT", b)[:], in_=pb[:, 0:128], func=AF.Copy), reads=[pkk], writes=[K("PT", b)])
                S.op("dve", lambda e, pb=pb, b=b: e.tensor_tensor(out=Wp("TT", b)[:], in0=pb[:, 0:128], in1=ident[:], op=ALU.add),
                     reads=[pkk, "c_ident"], writes=[K("TT", b)])
                S.op("pool", lambda e, b=b: e.tensor_copy(out=Wp("TTb", b)[:], in_=Wp("TT", b)[:]), reads=[K("TT", b)], writes=[K("TTb", b)])
                S.op("act", lambda e, pb=pb, b=b: e.activation(out=Wp("attnT", b)[:, :], in_=pb[0:64, 128:384], func=AF.Copy), reads=[pkk], writes=[K("attnT", b)])
        stages.append(s_p4)

        for lvl in range(1, 6):
            def s_sq(lvl=lvl):
                for b in blocks:
                    pb, pkk = bank()
                    bk[b] = (pb, pkk)
                    S.op("pe", lambda e, pb=pb, b=b: e.matmul(pb[:, 0:128], lhsT=Wp("PT", b)[:], rhs=Wp("P", b)[:], start=True, stop=True),
                         reads=[K("PT", b), K("P", b)], writes=[pkk])
                    if lvl < 5:
                        S.op("pe", lambda e, pb=pb, b=b: e.matmul(pb[:, 128:256], lhsT=Wp("P", b)[:], rhs=Wp("PT", b)[:], start=True, stop=True),
                             reads=[K("PT", b), K("P", b)], writes=[pkk])
                for b in blocks:
                    pb, pkk = bk[b]
                    S.op("act", lambda e, pb=pb, b=b: e.activation(out=Wp("P", b)[:], in_=pb[:, 0:128], func=AF.Copy), reads=[pkk], writes=[K("P", b)])
                    if lvl < 5:
                        S.op("dve", lambda e, pb=pb, b=b: e.tensor_copy(out=Wp("PT", b)[:], in_=pb[:, 128:256]), reads=[pkk], writes=[K("PT", b)])
            stages.append(s_sq)

            def s_tt(lvl=lvl):
                for b in blocks:
                    pb, pkk = bank()
                    bk[b] = (pb, pkk)
                    S.op("pe", lambda e, pb=pb, b=b: e.matmul(pb[:, 0:128], lhsT=Wp("P", b)[:], rhs=Wp("TTb", b)[:], start=True, stop=True),
                         reads=[K("P", b), K("TTb", b)], writes=[pkk])
                for b in blocks:
                    pb, pkk = bk[b]
                    S.op("dve", lambda e, pb=pb, b=b: e.tensor_tensor(out=Wp("TT", b)[:], in0=Wp("TT", b)[:], in1=pb[:, 0:128], op=ALU.add),
                         reads=[pkk, K("TT", b)], writes=[K("TT", b)])
                    if lvl < 5:
                        S.op("pool", lambda e, b=b: e.tensor_copy(out=Wp("TTb", b)[:], in_=Wp("TT", b)[:]), reads=[K("TT", b)], writes=[K("TTb", b)])
            stages.append(s_tt)

        def s_p6():
            for b in blocks:
                pb, pkk = bank()
                S.op("pe", lambda e, pb=pb, b=b: e.matmul(pb[0:64, 0:128], lhsT=Wp("TT", b)[:, 0:64], rhs=Wp("vb", b)[:], start=True, stop=True),
                     reads=[K("TT", b), K("vb", b)], writes=[pkk])
                S.op("pe", lambda e, pb=pb, b=b: e.matmul(pb[0:64, 128:256], lhsT=Wp("TT", b)[:, 64:128], rhs=Wp("vb", b)[:], start=True, stop=True),
                     reads=[K("TT", b), K("vb", b)], writes=[pkk])
                S.op("pe", lambda e, pb=pb, b=b: e.matmul(pb[:, 256:384], lhsT=Wp("kbg", b)[:], rhs=Wp("TT", b)[:], start=True, stop=True),
                     reads=[K("TT", b), K("kbg", b)], writes=[pkk])
                S.op("act", lambda e, pb=pb, b=b: e.activation(out=Wp("u", b)[:, :, 0:128], in_=pb[0:64, 0:256].rearrange("p (c e) -> p c e", e=128), func=AF.Copy),
                     reads=[pkk], writes=[K("u", b)])
                S.op("dve", lambda e, pb=pb, b=b: e.tensor_copy(out=Wp("wT", b)[:], in_=pb[:, 256:384]), reads=[pkk], writes=[K("wT", b)])
        stages.append(s_p6)
        return stages

    def seqsteps(gi):
        g, h = groups[gi]
        par = gi % 2
        st = g * GRP * 128
        ob = oc[par]
        ok = f"oc{par}"
        K = lambda nm, b: f"{nm}{b}_{par}"
        Wp = lambda nm, b: W[(nm, b, par)]
        steps = []
        for b in blocks:
            for ch in range(2):
                def s_chunk(b=b, ch=ch):
                    n = h * NBh + g * GRP + b
                    ci = 2 * n + ch
                    si = sidx[h]
                    Sc, Sn = St[h][si], St[h][1 - si]
                    Sck, Snk = f"St{h}_{si}", f"St{h}_{1 - si}"
                    vb_, vk = vnew[ci % 2], f"vnew{ci % 2}"
                    S.op("pe", lambda e: e.matmul(ps[5][0:64, 0:256], lhsT=Wp("wT", b)[:, ch * 64:(ch + 1) * 64], rhs=Sc[:, :], start=True, stop=True),
                         reads=[K("wT", b), Sck], writes=[pk[5]])
                    S.op("dve", lambda e: e.tensor_tensor(out=vb_[:, :], in0=Wp("u", b)[:, ch, :], in1=ps[5][0:64, 0:256], op=ALU.subtract),
                         reads=[K("u", b), pk[5]], writes=[vk])
                    for part in range(2):
                        S.op("pe", lambda e, part=part: e.matmul(ps[6][:, part * 64:part * 64 + 64], lhsT=Sc[:, part * 128:(part + 1) * 128],
                                                                 rhs=Wp("qg", b)[:, ch * 64:(ch + 1) * 64], start=True, stop=False),
                             reads=[Sck, K("qg", b)], writes=[pk[6]])
                        S.op("pe", lambda e, part=part: e.matmul(ps[6][:, part * 64:part * 64 + 64], lhsT=vb_[:, part * 128:(part + 1) * 128],
                                                                 rhs=Wp("attnT", b)[:, ch * 128 + ch * 64:ch * 128 + ch * 64 + 64],
                                                                 start=False, stop=True),
                             reads=[vk, K("attnT", b)], writes=[pk[6]])
                    S.op("act", lambda e: e.activation(out=ob[:, :, b * 128 + ch * 64:b * 128 + ch * 64 + 64],
                                                       in_=ps[6][:, 0:128].rearrange("p (w c) -> p w c", c=64), func=AF.Copy),
                         reads=[pk[6]], writes=[ok])
                    S.op("pe", lambda e: e.matmul(ps[7][:, 0:256], lhsT=Wp("kdec", b)[:, ch * 128:(ch + 1) * 128], rhs=vb_[:, :], start=True, stop=True),
                         reads=[K("kdec", b), K("kdec", b) + "b", vk], writes=[pk[7]])
                    S.op("dve", lambda e: e.scalar_tensor_tensor(out=Sn[:, :], in0=Sc[:, :], scalar=egl[:, ci:ci + 1], in1=ps[7][:, 0:256],
                                                                 op0=ALU.mult, op1=ALU.add),
                         reads=[Sck, "egl", pk[7]], writes=[Snk])
                    sidx[h] = 1 - si
                steps.append(s_chunk)

        def s_out():
            S.dma("sp", oc_d[:, h * 128:(h + 1) * 128, st:st + GRP * 128].rearrange("w e t -> e w t"), ob[:, :, :], reads=[ok], writes=[f"oc_d{par}"])
        steps.append(s_out)
        return steps

    def qload(gi):
        g, h = groups[gi]
        st = g * GRP * 128
        S.dma("sp", qkv[gi % 2][:, :, :], qkv_r[h, :, :, st:st + GRP * 128].rearrange("w d t -> d w t"), writes=[f"qkv{gi % 2}"])

    qload(0)
    if len(groups) > 1:
        qload(1)
    for f in prepass(0):
        f()
    for gi in range(len(groups)):
        nxt = prepass(gi + 1) if gi + 1 < len(groups) else []
        if nxt:
            if gi + 2 < len(groups):
                nxt.insert(3, lambda gi=gi: qload(gi + 2))
        seq = seqsteps(gi)
        n_n, n_s = len(nxt), len(seq)
        si_ = 0
        for k in range(n_n):
            nxt[k]()
            while si_ < n_s and (si_ + 1) * n_n <= (k + 1) * n_s:
                seq[si_]()
                si_ += 1
        while si_ < n_s:
            seq[si_]()
            si_ += 1
    for h in range(4):
        S.dma("sp", st_d[h, :, :], St[h][sidx[h]][:, :], reads=[f"St{h}_{sidx[h]}"], writes=[f"st_d{h}"])
    cx.pop()


def phase_c(cx, S_core, oc_d, yc_d, zg_d, xin, wout, lnrows, pv_d, ffn, xout, sin, scr=None):
    nc, S, c = cx.nc, cx.S, cx.c
    ident, ones = c["ident"], c["ones"]
    ps, pk = cx.ps, cx.psk
    moe = ffn["moe"]
    G = S_core if moe else min(1024, S_core)
    T = min(512, G)
    nsubG = G // 128
    NT = 2 * S_core // 512 + NEXP - 1
    nexp = NEXP if moe else 1
    dff = DFF_EXP if moe else DFF_DENSE
    nfc = dff // 128
    FG = 4
    fgroups = [(f0, min(FG, nfc - f0)) for f0 in range(0, nfc, FG)]
    cx.push()
    pv = cx.sb("pvc", [128, NPV])
    S.dma("sp", pv[:], pv_d[:, :], writes=["pv"])
    o128 = cx.sb("o128", [128, 128])
    S.op("pool", lambda e: e.memset(o128[:], 1.0 / 128.0), writes=["o128"])
    lnp = cx.sb("lnp", [128, 4, D])
    for i in range(4):
        S.dma("sp", lnp[:, i, :], lnrows[i, :, :], writes=[f"lnp{i}"])
    wo = cx.sb("wo", [128, 8, D], BF16)
    wov = wout.rearrange("(kc p) n -> p kc n", p=128)
    for kc in range(8):
        S.dma("pool", wo[:, kc, :], wov[:, kc, :], writes=[f"wo{kc}"])
    if not moe:
        accb = cx.sb("accb", [128, nsubG, D])
        x1T = cx.sb("x1T", [128, 8, G], BF16)
    if moe:
        rw = cx.sb("rw", [128, 8, NEXP])
        S.dma("sp", rw[:, :, :], ffn["router"].rearrange("(kc p) n -> p kc n", p=128), writes=["rw"])
        selA = cx.sb("selA", [128, nsubG, NEXP])
        m1A = cx.sb("m1A", [128, nsubG, NEXP])
        combA = cx.sb("combA", [128, nsubG, NEXP])
        rankA = cx.sb("rankA", [128, nsubG, NEXP])
        base = cx.sb("base", [128, NEXP])
        UT = cx.sb("UT", [128, 128])
        S.op("pool", lambda e: e.memset(base[:], 0.0), writes=["base"])
        S.op("pool", lambda e: e.memset(UT[:], 1.0), writes=["UT"])
        S.op("pool", lambda e: e.affine_select(out=UT[:], in_=UT[:], pattern=[[1, 128]], compare_op=ALU.is_ge,
                                               fill=0.0, base=-1, channel_multiplier=-1), reads=["UT"], writes=["UT"])
    pctr = [0]

    def bank():
        i = pctr[0] % 8
        pctr[0] += 1
        return ps[i], pk[i]

    def layer_norm_rows(src, skey, dst, dkey, gi_, bi_, small):
        st, mv = small
        for hh in range(2):
            S.op("dve", lambda e, hh=hh: e.bn_stats(out=st[:, hh * 6:(hh + 1) * 6], in_=src[:, hh * 512:(hh + 1) * 512]), reads=[skey], writes=["bnst"])
        S.op("dve", lambda e: e.bn_aggr(out=mv[:, 0:2], in_=st[:, 0:12]), reads=["bnst"], writes=["bnmv"])
        S.op("dve", lambda e: e.tensor_scalar(out=mv[:, 2:3], in0=mv[:, 1:2], scalar1=LN_EPS, scalar2=None, op0=ALU.add), reads=["bnmv"], writes=["bnr"])
        S.op("act", lambda e: e.activation(out=mv[:, 2:3], in_=mv[:, 2:3], func=AF.Sqrt), reads=["bnr"], writes=["bnr"])
        S.op("dve", lambda e: e.reciprocal(out=mv[:, 2:3], in_=mv[:, 2:3]), reads=["bnr"], writes=["bnr"])
        S.op("dve", lambda e: e.tensor_scalar(out=mv[:, 3:4], in0=mv[:, 0:1], scalar1=mv[:, 2:3], scalar2=-1.0, op0=ALU.mult, op1=ALU.mult),
             reads=["bnmv", "bnr"], writes=["bnnb"])
        S.op("act", lambda e: e.activation(out=dst, in_=src, func=AF.Identity, scale=mv[:, 2:3], bias=mv[:, 3:4]),
             reads=[skey, "bnr", "bnnb"], writes=[dkey])
        S.op("pool", lambda e: e.tensor_tensor(out=dst, in0=dst, in1=lnp[:, gi_, :], op=ALU.mult), reads=[dkey, f"lnp{gi_}"], writes=[dkey])
        S.op("dve", lambda e: e.tensor_tensor(out=dst, in0=dst, in1=lnp[:, bi_, :], op=ALU.add), reads=[dkey, f"lnp{bi_}"], writes=[dkey])

    for g0 in range(0, S_core, G):
        cx.push()
        xt = [cx.sb(f"cxt{i}", [128, 4, D]) for i in range(2)]
        oTt = [cx.sb(f"coT{i}", [128, 4, 512]) for i in range(2)]
        zgt = [cx.sb(f"czg{i}", [128, 4, 512]) for i in range(2)]
        cTt = [cx.sb(f"ccT{i}", [128, 4, 512]) for i in range(2)]
        yct = [cx.sb(f"cyc{i}", [128, 4, 512], BF16) for i in range(2)]
        ydn = cx.sb("ydn", [128, 4, 512], BF16)
        sqc = cx.sb("sqc", [128, 512])
        rrs = [cx.sb(f"rr{i}", [128, D]) for i in range(2)]
        x1 = cx.sb("x1", [128, D])
        st = cx.sb("bnst", [128, 12])
        mv = cx.sb("bnmv", [128, 4])
        x1f = cx.sb("x1f", [128, 8, 128]) if moe else None
        lgt = cx.sb("lgt", [128, 8, 8]) if moe else None
        tile_starts = list(range(0, G, T))

        def load_tile(it):
            t0 = g0 + tile_starts[it]
            i2 = it % 2
            nsub = T // 128
            S.dma("sp", xt[i2][:, 0:nsub, :], xin[HALO + t0:HALO + t0 + T, :].rearrange("(s p) d -> p s d", p=128), writes=[f"cxt{i2}"])
            S.dma("sp", oTt[i2][:, :, 0:T], oc_d[0, :, t0:t0 + T].rearrange("(h e) t -> e h t", e=128), writes=[f"coT{i2}"])
            S.dma("sp", cTt[i2][:, :, 0:T], oc_d[1, :, t0:t0 + T].rearrange("(h e) t -> e h t", e=128), writes=[f"ccT{i2}"])
            S.dma("sp", zgt[i2][:, :, 0:T], zg_d[:, t0:t0 + T].rearrange("(h e) t -> e h t", e=128), writes=[f"czg{i2}"])
            S.dma("pool", yct[i2][:, :, 0:T], yc_d[:, t0:t0 + T].rearrange("(h e) t -> e h t", e=128), writes=[f"cyc{i2}"])

        load_tile(0)
        for it, tt0 in enumerate(tile_starts):
            t0 = g0 + tt0
            i2 = it % 2
            nsub = T // 128
            if it + 1 < len(tile_starts):
                load_tile(it + 1)
            for h in range(4):
                pb, pkk = bank()
                S.op("pe", lambda e, pb=pb, h=h: e.matmul(pb[:, 0:T], lhsT=sin[h][:, :], rhs=cTt[i2][:, h, 0:T], start=True, stop=True),
                     reads=[f"sin{h}", f"ccT{i2}"], writes=[pkk])
                S.op("dve", lambda e, pb=pb, h=h: e.tensor_tensor(out=oTt[i2][:, h, 0:T], in0=oTt[i2][:, h, 0:T], in1=pb[:, 0:T], op=ALU.add),
                     reads=[pkk, f"coT{i2}"], writes=[f"coT{i2}"])
                pb, pkk = bank()
                S.op("act", lambda e, h=h: e.activation(out=sqc[:, 0:T], in_=oTt[i2][:, h, 0:T], func=AF.Square), reads=[f"coT{i2}"], writes=["sqc"])
                S.op("pe", lambda e, pb=pb: e.matmul(pb[:, 0:T], lhsT=o128[:], rhs=sqc[:, 0:T], start=True, stop=True), reads=["o128", "sqc"], writes=[pkk])
                S.op("dve", lambda e, pb=pb: e.tensor_scalar(out=sqc[:, 0:T], in0=pb[:, 0:T], scalar1=RMS_EPS, scalar2=None, op0=ALU.add), reads=[pkk], writes=["sqc"])
                S.op("act", lambda e: e.activation(out=sqc[:, 0:T], in_=sqc[:, 0:T], func=AF.Sqrt), reads=["sqc"], writes=["sqc"])
                S.op("dve", lambda e: e.reciprocal(out=sqc[:, 0:T], in_=sqc[:, 0:T]), reads=["sqc"], writes=["sqc"])
                S.op("dve", lambda e, h=h: e.tensor_tensor(out=sqc[:, 0:T], in0=sqc[:, 0:T], in1=oTt[i2][:, h, 0:T], op=ALU.mult), reads=["sqc", f"coT{i2}"], writes=["sqc"])
                S.op("dve", lambda e, h=h: e.scalar_tensor_tensor(out=ydn[:, h, 0:T], in0=sqc[:, 0:T], scalar=pv[:, PV_ONG:PV_ONG + 1],
                                                                  in1=zgt[i2][:, h, 0:T], op0=ALU.mult, op1=ALU.mult),
                     reads=["sqc", "pv", f"czg{i2}"], writes=[f"ydn{h}"])
            def wout_stage(s):
                ts = slice(s * 128, (s + 1) * 128)
                rr = rrs[s % 2]
                for half in range(2):
                    pb, pkk = bank()
                    for kc in range(8):
                        lhs = yct[i2][:, kc, ts] if kc < 4 else ydn[:, kc - 4, ts]
                        lk = f"cyc{i2}" if kc < 4 else f"ydn{kc - 4}"
                        S.op("pe", lambda e, pb=pb, lhs=lhs, kc=kc, half=half: e.matmul(pb[:, :], lhsT=lhs, rhs=wo[:, kc, half * 512:(half + 1) * 512],
                                                                                        start=(kc == 0), stop=(kc == 7)),
                             reads=[lk, f"wo{kc}"], writes=[pkk])
                    S.op("dve", lambda e, pb=pb, s=s, half=half, rr=rr: e.scalar_tensor_tensor(out=rr[:, half * 512:(half + 1) * 512], in0=xt[i2][:, s, half * 512:(half + 1) * 512],
                                                                                               scalar=ALPHA, in1=pb[:, :], op0=ALU.mult, op1=ALU.add),
                         reads=[pkk, f"cxt{i2}"], writes=[f"rr{s % 2}"])

            wout_stage(0)
            for s in range(nsub):
                sg_ = (tt0 // 128) + s
                ts = slice(s * 128, (s + 1) * 128)
                if s + 1 < nsub:
                    wout_stage(s + 1)
                layer_norm_rows(rrs[s % 2][:, :], f"rr{s % 2}", x1[:, :], "x1", 0, 1, (st, mv))
                if moe:
                    S.dma("pool", scr["x1f"][t0 + s * 128:t0 + (s + 1) * 128, :], x1[:, :], reads=["x1"], writes=["x1f_d"])
                else:
                    S.op("act", lambda e, sg_=sg_: e.activation(out=accb[:, sg_, :], in_=x1[:, :], func=AF.Copy, scale=ALPHA), reads=["x1"], writes=[f"accb{sg_}"])
                for kc in range(8):
                    if kc % 4 == 0:
                        pb, pkk = bank()
                    q4 = kc % 4
                    S.op("pe", lambda e, pb=pb, kc=kc, q4=q4: e.transpose(out=pb[:, q4 * 128:(q4 + 1) * 128], in_=x1[:, kc * 128:(kc + 1) * 128], identity=ident[:]),
                         reads=["x1", "c_ident"], writes=[pkk])
                    if q4 == 3:
                        k0 = kc - 3
                        if not moe:
                            S.op("act", lambda e, pb=pb, k0=k0, sg_=sg_: e.activation(out=x1T[:, k0:k0 + 4, sg_ * 128:(sg_ + 1) * 128],
                                                                                     in_=pb[:, :].rearrange("p (k t) -> p k t", t=128), func=AF.Copy),
                                 reads=[pkk], writes=[f"x1T{sg_}"])
                        if moe:
                            S.op("dve", lambda e, pb=pb, k0=k0: e.tensor_copy(out=x1f[:, k0:k0 + 4, :], in_=pb[:, :].rearrange("p (k t) -> p k t", t=128)),
                                 reads=[pkk], writes=[f"x1f{k0}"])
                if moe:
                    pb, pkk = bank()
                    for kc in range(8):
                        S.op("pe", lambda e, pb=pb, kc=kc: e.matmul(pb[:, 0:NEXP], lhsT=x1f[:, kc, :], rhs=rw[:, kc, :], start=(kc == 0), stop=(kc == 7)),
                             reads=[f"x1f{(kc // 4) * 4}", "rw"], writes=[pkk])
                    L = lgt
                    S.op("dve", lambda e, pb=pb: e.tensor_copy(out=L[:, 0, :], in_=pb[:, 0:NEXP]), reads=[pkk], writes=["lgt"])
                    S.op("dve", lambda e: e.tensor_reduce(out=L[:, 7, 0:1], in_=L[:, 0, :], axis=AX.X, op=ALU.max), reads=["lgt"], writes=["lgt"])
                    S.op("dve", lambda e, sg_=sg_: e.tensor_scalar(out=m1A[:, sg_, :], in0=L[:, 0, :], scalar1=L[:, 7, 0:1], scalar2=None, op0=ALU.is_equal),
                         reads=["lgt"], writes=["m1A"])
                    S.op("dve", lambda e, sg_=sg_: e.tensor_scalar(out=L[:, 1, :], in0=m1A[:, sg_, :], scalar1=-1e30, scalar2=None, op0=ALU.mult),
                         reads=["lgt", "m1A"], writes=["lgt"])
                    S.op("dve", lambda e: e.tensor_tensor(out=L[:, 1, :], in0=L[:, 1, :], in1=L[:, 0, :], op=ALU.add), reads=["lgt"], writes=["lgt"])
                    S.op("dve", lambda e: e.tensor_reduce(out=L[:, 7, 1:2], in_=L[:, 1, :], axis=AX.X, op=ALU.max), reads=["lgt"], writes=["lgt"])
                    S.op("dve", lambda e: e.tensor_scalar(out=L[:, 2, :], in0=L[:, 0, :], scalar1=L[:, 7, 1:2], scalar2=None, op0=ALU.is_ge), reads=["lgt"], writes=["lgt"])
                    S.op("dve", lambda e: e.tensor_scalar(out=L[:, 3, :], in0=L[:, 0, :], scalar1=L[:, 7, 0:1], scalar2=None, op0=ALU.subtract), reads=["lgt"], writes=["lgt"])
                    S.op("act", lambda e: e.activation(out=L[:, 3, :], in_=L[:, 3, :], func=AF.Exp), reads=["lgt"], writes=["lgt"])
                    S.op("dve", lambda e: e.tensor_tensor(out=L[:, 3, :], in0=L[:, 3, :], in1=L[:, 2, :], op=ALU.mult), reads=["lgt"], writes=["lgt"])
                    S.op("dve", lambda e: e.tensor_reduce(out=L[:, 7, 2:3], in_=L[:, 3, :], axis=AX.X, op=ALU.add), reads=["lgt"], writes=["lgt"])
                    S.op("dve", lambda e: e.reciprocal(out=L[:, 7, 3:4], in_=L[:, 7, 2:3]), reads=["lgt"], writes=["lgt"])
                    S.op("dve", lambda e, sg_=sg_: e.tensor_scalar(out=combA[:, sg_, :], in0=L[:, 3, :], scalar1=L[:, 7, 3:4], scalar2=None, op0=ALU.mult),
                         reads=["lgt"], writes=["combA"])
                    S.op("dve", lambda e, sg_=sg_: e.tensor_copy(out=selA[:, sg_, :], in_=L[:, 2, :]), reads=["lgt"], writes=["selA"])
                    pb, pkk = bank()
                    S.op("pe", lambda e, pb=pb, sg_=sg_: e.matmul(pb[:, 0:NEXP], lhsT=UT[:], rhs=selA[:, sg_, :], start=True, stop=True),
                         reads=["UT", "selA"], writes=[pkk])
                    S.op("pe", lambda e, pb=pb, sg_=sg_: e.matmul(pb[:, NEXP:2 * NEXP], lhsT=ones[:], rhs=selA[:, sg_, :], start=True, stop=True),
                         reads=["c_ones", "selA"], writes=[pkk])
                    S.op("dve", lambda e, pb=pb, sg_=sg_: e.tensor_tensor(out=rankA[:, sg_, :], in0=pb[:, 0:NEXP], in1=base[:], op=ALU.add),
                         reads=[pkk, "base"], writes=["rankA"])
                    S.op("dve", lambda e, pb=pb: e.tensor_tensor(out=base[:], in0=pb[:, NEXP:2 * NEXP], in1=base[:], op=ALU.add),
                         reads=[pkk, "base"], writes=["base"])
        cx.pop()
        if moe:
            moe_sparse(cx, S_core, NT, ffn, scr, xout, lnp, layer_norm_rows, selA, m1A, combA, rankA, base)
            continue
        cx.push()
        wg = [cx.sb(f"wg{i}", [128, 8, FG * 128], BF16) for i in range(2)]
        wu = [cx.sb(f"wu{i}", [128, 8, FG * 128], BF16) for i in range(2)]
        wd = [cx.sb(f"wd{i}", [128, FG, D], BF16) for i in range(2)]
        hT = cx.sb("hT", [128, FG, G], BF16)
        sgb = [cx.sb(f"sgb{i}", [128, 512]) for i in range(2)]
        yo = [cx.sb(f"yo{i}", [128, D]) for i in range(2)]
        st = cx.sb("bnst2", [128, 12])
        mv = cx.sb("bnmv2", [128, 4])
        units = [(e_, f0, nf) for e_ in range(nexp) for (f0, nf) in fgroups]

        def load_w(ui):
            e_, f0, nf = units[ui]
            i2 = ui % 2
            gsrc = ffn["wg"][e_, :, f0 * 128:(f0 + nf) * 128].rearrange("(kc p) n -> p kc n", p=128)
            usrc = ffn["wu"][e_, :, f0 * 128:(f0 + nf) * 128].rearrange("(kc p) n -> p kc n", p=128)
            dsrc = ffn["wd"][e_, f0 * 128:(f0 + nf) * 128, :].rearrange("(f p) n -> p f n", p=128)
            S.dma("pool", wg[i2][:, :, 0:nf * 128], gsrc, writes=[f"wg{i2}"])
            S.dma("pool", wu[i2][:, :, 0:nf * 128], usrc, writes=[f"wu{i2}"])
            S.dma("pool", wd[i2][:, 0:nf, :], dsrc, writes=[f"wd{i2}"])

        load_w(0)
        cnt = 0
        for ui, (e_, f0, nf) in enumerate(units):
            i2 = ui % 2
            if ui + 1 < len(units):
                load_w(ui + 1)
            for tt0 in range(0, G, T):
                for f in range(nf):
                    pg, pgk = bank()
                    pu, puk = bank()
                    for kc in range(8):
                        S.op("pe", lambda e, pg=pg, kc=kc, f=f: e.matmul(pg[:, 0:T], lhsT=wg[i2][:, kc, f * 128:(f + 1) * 128], rhs=x1T[:, kc, tt0:tt0 + T],
                                                                         start=(kc == 0), stop=(kc == 7)),
                             reads=[f"wg{i2}"] + [f"x1T{(tt0 // 128) + s}" for s in range(T // 128)], writes=[pgk])
                    for kc in range(8):
                        S.op("pe", lambda e, pu=pu, kc=kc, f=f: e.matmul(pu[:, 0:T], lhsT=wu[i2][:, kc, f * 128:(f + 1) * 128], rhs=x1T[:, kc, tt0:tt0 + T],
                                                                         start=(kc == 0), stop=(kc == 7)),
                             reads=[f"wu{i2}"] + [f"x1T{(tt0 // 128) + s}" for s in range(T // 128)], writes=[puk])
                    sb_ = sgb[cnt % 2]
                    sk = f"sgb{cnt % 2}"
                    cnt += 1
                    S.op("act", lambda e, pg=pg, sb_=sb_: e.activation(out=sb_[:, 0:T], in_=pg[:, 0:T], func=AF.Silu), reads=[pgk], writes=[sk])
                    S.op("dve", lambda e, pu=pu, sb_=sb_, f=f: e.tensor_tensor(out=hT[:, f, tt0:tt0 + T], in0=sb_[:, 0:T], in1=pu[:, 0:T], op=ALU.mult),
                         reads=[puk, sk], writes=[f"hT{f}_{tt0}"])
            for sg_ in range(nsubG):
                tt0 = (sg_ * 128 // T) * T
                for half in range(2):
                    pb, pkk = bank()
                    for f in range(nf):
                        S.op("pe", lambda e, pb=pb, f=f, sg_=sg_, half=half: e.matmul(pb[:, :], lhsT=hT[:, f, sg_ * 128:(sg_ + 1) * 128],
                                                                                      rhs=wd[i2][:, f, half * 512:(half + 1) * 512],
                                                                                      start=(f == 0), stop=(f == nf - 1)),
                             reads=[f"hT{f}_{tt0}", f"wd{i2}"], writes=[pkk])
                    if moe:
                        S.op("dve", lambda e, pb=pb, sg_=sg_, half=half, e_=e_: e.scalar_tensor_tensor(
                            out=accb[:, sg_, half * 512:(half + 1) * 512], in0=pb[:, :], scalar=comb[:, sg_, e_:e_ + 1],
                            in1=accb[:, sg_, half * 512:(half + 1) * 512], op0=ALU.mult, op1=ALU.add),
                             reads=[pkk, "comb", f"accb{sg_}"], writes=[f"accb{sg_}"])
                    else:
                        S.op("dve", lambda e, pb=pb, sg_=sg_, half=half: e.tensor_tensor(
                            out=accb[:, sg_, half * 512:(half + 1) * 512], in0=pb[:, :],
                            in1=accb[:, sg_, half * 512:(half + 1) * 512], op=ALU.add),
                             reads=[pkk, f"accb{sg_}"], writes=[f"accb{sg_}"])
        for sg_ in range(nsubG):
            yb, yk = yo[sg_ % 2], f"yo{sg_ % 2}"
            layer_norm_rows(accb[:, sg_, :], f"accb{sg_}", yb[:, :], yk, 2, 3, (st, mv))
            S.dma("sp", xout[g0 + sg_ * 128:g0 + (sg_ + 1) * 128, :], yb[:, :], reads=[yk], writes=[f"xout{sg_ % 2}"])
        cx.pop()
    cx.pop()


def moe_sparse(cx, S_core, NT, ffn, scr, xout, lnp, layer_norm_rows, selA, m1A, combA, rankA, base):
    nc, S, c = cx.nc, cx.S, cx.c
    ident = c["ident"]
    ps, pk = cx.ps, cx.psk
    nsub = S_core // 128
    x1f_d, s2t_d, y_d = scr["x1f"], scr["s2t"], scr["y"]
    cx.push()
    pcn = cx.sb("pcn", [128, NEXP])
    tmp8 = cx.sb("tmp8", [128, NEXP])
    offs = cx.sb("offs", [128, NEXP])
    ends = cx.sb("ends", [128, NEXP])
    S.op("dve", lambda e: e.tensor_scalar(out=pcn[:], in0=base[:], scalar1=0.0, scalar2=None, op0=ALU.is_gt), reads=["base"], writes=["pcn"])
    for k in range(1, 2 * S_core // 512 + 1):
        S.op("dve", lambda e, k=k: e.tensor_scalar(out=tmp8[:], in0=base[:], scalar1=512.0 * k, scalar2=None, op0=ALU.is_gt), reads=["base"], writes=["tmp8"])
        S.op("dve", lambda e: e.tensor_tensor(out=pcn[:], in0=pcn[:], in1=tmp8[:], op=ALU.add), reads=["tmp8", "pcn"], writes=["pcn"])
    S.op("dve", lambda e: e.tensor_scalar(out=pcn[:], in0=pcn[:], scalar1=512.0, scalar2=None, op0=ALU.mult), reads=["pcn"], writes=["pcn"])
    S.op("dve", lambda e: e.memset(offs[:], 0.0), writes=["offs"])
    for e_ in range(1, NEXP):
        S.op("dve", lambda e, e_=e_: e.tensor_tensor(out=offs[:, e_:e_ + 1], in0=offs[:, e_ - 1:e_], in1=pcn[:, e_ - 1:e_], op=ALU.add),
             reads=["offs", "pcn"], writes=["offs"])
    S.op("dve", lambda e: e.tensor_tensor(out=ends[:], in0=offs[:], in1=pcn[:], op=ALU.add), reads=["offs", "pcn"], writes=["ends"])
    eidf = cx.sb("eidf", [128, NT])
    eidi = cx.sb("eidi", [128, NT], I32)
    for i in range(NT):
        S.op("dve", lambda e, i=i: e.tensor_scalar(out=tmp8[:], in0=ends[:], scalar1=512.0 * i, scalar2=None, op0=ALU.is_le), reads=["ends"], writes=["tmp8"])
        S.op("dve", lambda e, i=i: e.tensor_reduce(out=eidf[:, i:i + 1], in_=tmp8[:], axis=AX.X, op=ALU.add), reads=["tmp8"], writes=["eidf"])
    S.op("dve", lambda e: e.tensor_scalar(out=eidf[:], in0=eidf[:], scalar1=float(NEXP - 1), scalar2=None, op0=ALU.min), reads=["eidf"], writes=["eidf"])
    NFG = DFF_EXP // 128 // 4
    gi_ = cx.sb("gi_", [128, NFG], I32)
    gf_ = cx.sb("gf_", [128, NFG])
    tabGf = cx.sb("tabGf", [128, NT, NFG])
    tabDf = cx.sb("tabDf", [128, NT, NFG])
    tabG = cx.sb("tabG", [128, NT, NFG], I32)
    tabD = cx.sb("tabD", [128, NT, NFG], I32)
    e1 = cx.sb("e1", [128, NT])
    S.op("pool", lambda e: e.iota(gi_[:], pattern=[[1, NFG]], base=0, channel_multiplier=0), writes=["gi_"])
    S.op("dve", lambda e: e.tensor_copy(out=gf_[:], in_=gi_[:]), reads=["gi_"], writes=["gf_"])
    S.op("dve", lambda e: e.tensor_scalar(out=e1[:], in0=eidf[:], scalar1=float(D * DFF_EXP // 512), scalar2=None, op0=ALU.mult), reads=["eidf"], writes=["e1"])
    for i in range(NT):
        S.op("dve", lambda e, i=i: e.tensor_scalar(out=tabGf[:, i, :], in0=gf_[:], scalar1=e1[:, i:i + 1], scalar2=512.0, op0=ALU.add, op1=ALU.mult),
             reads=["gf_", "e1"], writes=["tabGf"])
    S.op("dve", lambda e: e.tensor_scalar(out=e1[:], in0=eidf[:], scalar1=float(D * DFF_EXP // 524288), scalar2=None, op0=ALU.mult), reads=["eidf", "tabGf"], writes=["e1"])
    for i in range(NT):
        S.op("dve", lambda e, i=i: e.tensor_scalar(out=tabDf[:, i, :], in0=gf_[:], scalar1=e1[:, i:i + 1], scalar2=524288.0, op0=ALU.add, op1=ALU.mult),
             reads=["gf_", "e1"], writes=["tabDf"])
    S.op("dve", lambda e: e.tensor_copy(out=tabG[:], in_=tabGf[:]), reads=["tabGf"], writes=["tabG"])
    S.op("dve", lambda e: e.tensor_copy(out=tabD[:], in_=tabDf[:]), reads=["tabDf"], writes=["tabD"])
    posf = cx.sb("posf", [128, 2, nsub])
    gts = cx.sb("gts", [128, 2, nsub])
    tmpb = cx.sb("tmpb", [128, nsub, NEXP])
    m2A = cx.sb("m2A", [128, nsub, NEXP])
    for sg in range(nsub):
        S.op("dve", lambda e, sg=sg: e.tensor_tensor(out=rankA[:, sg, :], in0=rankA[:, sg, :], in1=offs[:], op=ALU.add),
             reads=["rankA", "offs"], writes=["rankA"])
    S.op("dve", lambda e: e.tensor_tensor(out=m2A[:], in0=selA[:], in1=m1A[:], op=ALU.subtract), reads=["selA", "m1A"], writes=["m2A"])
    for w, mk, mkey in ((0, m1A, "m1A"), (1, m2A, "m2A")):
        S.op("dve", lambda e, mk=mk: e.tensor_tensor(out=tmpb[:], in0=mk[:], in1=rankA[:], op=ALU.mult), reads=[mkey, "rankA"], writes=["tmpb"])
        S.op("dve", lambda e, w=w: e.tensor_reduce(out=posf[:, w, :], in_=tmpb[:], axis=AX.X, op=ALU.add), reads=["tmpb"], writes=["posf"])
        S.op("dve", lambda e, mk=mk: e.tensor_tensor(out=tmpb[:], in0=mk[:], in1=combA[:], op=ALU.mult), reads=[mkey, "combA"], writes=["tmpb"])
        S.op("dve", lambda e, w=w: e.tensor_reduce(out=gts[:, w, :], in_=tmpb[:], axis=AX.X, op=ALU.add), reads=["tmpb"], writes=["gts"])
    posi = cx.sb("posi", [128, 2, nsub], I32)
    tokid = cx.sb("tokid", [128, nsub], I32)
    zer = cx.sb("zer", [128, NT * 4], I32)
    idx_all = cx.sb("idx_all", [128, NT * 4], I32)
    S.op("dve", lambda e: e.tensor_copy(out=posi[:], in_=posf[:]), reads=["posf"], writes=["posi"])
    S.op("pool", lambda e: e.iota(tokid[:], pattern=[[128, nsub]], base=0, channel_multiplier=1), writes=["tokid"])
    S.op("pool", lambda e: e.memset(zer[:], 0), writes=["zer"])
    S.dma("sp", s2t_d.rearrange("(p j) o -> p (j o)", p=128), zer[:, :], reads=["zer"], writes=["s2t_d"])
    for w in range(2):
        for sg in range(nsub):
            S.ind_dma(s2t_d[:, :], tokid[:, sg:sg + 1], posi[:, w, sg:sg + 1], False, reads=["tokid", "posi", "s2t_d"], writes=[f"s2t_{w}_{sg}"])
    S.dma("sp", idx_all[:, :], s2t_d.rearrange("(j p) o -> p (j o)", p=128),
          reads=["s2t_d"] + [f"s2t_{w}_{sg}" for w in range(2) for sg in range(nsub)], writes=["idx_all"], allow_slow_non_contiguous=True)

    cx.push()
    FG = 4
    nfc = DFF_EXP // 128
    fgroups = [(f0, min(FG, nfc - f0)) for f0 in range(0, nfc, FG)]
    wg = [cx.sb(f"swg{i}", [128, 8, FG * 128], BF16) for i in range(2)]
    wu = [cx.sb(f"swu{i}", [128, 8, FG * 128], BF16) for i in range(2)]
    wd = [cx.sb(f"swd{i}", [128, FG, D], BF16) for i in range(2)]
    hT = cx.sb("shT", [128, FG, 512], BF16)
    sgb = [cx.sb(f"ssgb{i}", [128, 512]) for i in range(2)]
    xg = [cx.sb(f"xg{i}", [128, D]) for i in range(2)]
    xgT = [cx.sb(f"xgT{i}", [128, 8, 512], BF16) for i in range(2)]
    yacc = [cx.sb(f"yacc{i}", [128, 4, D]) for i in range(2)]
    pctr = [0]

    def bank():
        i = pctr[0] % 8
        pctr[0] += 1
        return ps[i], pk[i]

    regG = nc.gpsimd.alloc_register("offg_reg")
    regD = nc.gpsimd.alloc_register("offd_reg")
    units = [(i, f0, nf) for i in range(NT) for (f0, nf) in fgroups]
    RH = type(regG)

    def dyn_dma(dst, tens, val, ap, key):
        src = bass.AP(tensor=tens.tensor, offset=val, ap=ap)
        S.dma("pool", dst, src, writes=[key])
        js = nc.instruction_to_json(S.last_inst.ins)
        return set(re.findall(r'"reg_ap_offset": "([^"]+)"', js))

    def load_w(ui):
        i, f0, nf = units[ui]
        i2 = ui % 2
        g = f0 // FG
        S._deps("pool", ["tabG", "tabD"], [])
        nc.gpsimd.reg_load(regG, tabG[0:1, i, g:g + 1])
        nc.gpsimd.reg_load(regD, tabD[0:1, i, g:g + 1])
        vG = nc.gpsimd.snap(regG)
        vD = nc.gpsimd.snap(regD)
        tmps = set()
        tmps |= dyn_dma(wg[i2][:, :, 0:nf * 128], ffn["wg"], vG, [[DFF_EXP, 128], [128 * DFF_EXP, 8], [1, nf * 128]], f"swg{i2}")
        tmps |= dyn_dma(wu[i2][:, :, 0:nf * 128], ffn["wu"], vG, [[DFF_EXP, 128], [128 * DFF_EXP, 8], [1, nf * 128]], f"swu{i2}")
        tmps |= dyn_dma(wd[i2][:, 0:nf, :], ffn["wd"], vD, [[D, 128], [128 * D, nf], [1, D]], f"swd{i2}")
        for t in tmps:
            nc.gpsimd.free_register(RH(name=t, engine=regG.engine))
        nc.gpsimd.free_register(vG.val)
        nc.gpsimd.free_register(vD.val)

    def gather_tile(i):
        t2 = i % 2
        for s_ in range(4):
            j = i * 4 + s_
            xb, xk = xg[j % 2], f"xg{j % 2}"
            S.ind_dma(xb[:, :], x1f_d[:, :], idx_all[:, j:j + 1], True, reads=["idx_all"], writes=[xk])
            for kc in range(8):
                if kc % 4 == 0:
                    pb, pkk = bank()
                q4 = kc % 4
                S.op("pe", lambda e, pb=pb, kc=kc, q4=q4, xb=xb: e.transpose(out=pb[:, q4 * 128:(q4 + 1) * 128], in_=xb[:, kc * 128:(kc + 1) * 128], identity=ident[:]),
                     reads=[xk, "c_ident"], writes=[pkk])
                if q4 == 3:
                    k0 = kc - 3
                    eng = "act" if k0 == 0 else "dve"
                    if eng == "act":
                        S.op("act", lambda e, pb=pb, k0=k0, s_=s_: e.activation(out=xgT[t2][:, k0:k0 + 4, s_ * 128:(s_ + 1) * 128],
                                                                                in_=pb[:, :].rearrange("p (k t) -> p k t", t=128), func=AF.Copy),
                             reads=[pkk], writes=[f"xgT{t2}_{s_}a"])
                    else:
                        S.op("dve", lambda e, pb=pb, k0=k0, s_=s_: e.tensor_copy(out=xgT[t2][:, k0:k0 + 4, s_ * 128:(s_ + 1) * 128],
                                                                                 in_=pb[:, :].rearrange("p (k t) -> p k t", t=128)),
                             reads=[pkk], writes=[f"xgT{t2}_{s_}d"])

    load_w(0)
    gather_tile(0)
    cnt = 0
    for ui, (i, f0, nf) in enumerate(units):
        i2 = ui % 2
        t2 = i % 2
        if ui + 1 < len(units):
            load_w(ui + 1)
        if f0 == 0 and i + 1 < NT:
            gather_tile(i + 1)
        xkeys = [f"xgT{t2}_{s_}{a}" for s_ in range(4) for a in "ad"]
        for f in range(nf):
            pg, pgk = bank()
            pu, puk = bank()
            for kc in range(8):
                S.op("pe", lambda e, pg=pg, kc=kc, f=f: e.matmul(pg[:, :], lhsT=wg[i2][:, kc, f * 128:(f + 1) * 128], rhs=xgT[t2][:, kc, :],
                                                                 start=(kc == 0), stop=(kc == 7)),
                     reads=[f"swg{i2}"] + xkeys, writes=[pgk])
            for kc in range(8):
                S.op("pe", lambda e, pu=pu, kc=kc, f=f: e.matmul(pu[:, :], lhsT=wu[i2][:, kc, f * 128:(f + 1) * 128], rhs=xgT[t2][:, kc, :],
                                                                 start=(kc == 0), stop=(kc == 7)),
                     reads=[f"swu{i2}"] + xkeys, writes=[puk])
            sb_ = sgb[cnt % 2]
            sk = f"ssgb{cnt % 2}"
            cnt += 1
            S.op("act", lambda e, pg=pg, sb_=sb_: e.activation(out=sb_[:, :], in_=pg[:, :], func=AF.Silu), reads=[pgk], writes=[sk])
            S.op("dve", lambda e, pu=pu, sb_=sb_, f=f: e.tensor_tensor(out=hT[:, f, :], in0=sb_[:, :], in1=pu[:, :], op=ALU.mult),
                 reads=[puk, sk], writes=[f"shT{f}"])
        for s_ in range(4):
            for half in range(2):
                pb, pkk = bank()
                for f in range(nf):
                    S.op("pe", lambda e, pb=pb, f=f, s_=s_, half=half: e.matmul(pb[:, :], lhsT=hT[:, f, s_ * 128:(s_ + 1) * 128],
                                                                                rhs=wd[i2][:, f, half * 512:(half + 1) * 512],
                                                                                start=(f == 0), stop=(f == nf - 1)),
                         reads=[f"shT{f}", f"swd{i2}"], writes=[pkk])
                yk = f"yacc{t2}_{s_}"
                if f0 == 0:
                    S.op("act", lambda e, pb=pb, s_=s_, half=half: e.activation(out=yacc[t2][:, s_, half * 512:(half + 1) * 512], in_=pb[:, :], func=AF.Copy),
                         reads=[pkk], writes=[yk])
                else:
                    S.op("dve", lambda e, pb=pb, s_=s_, half=half: e.tensor_tensor(out=yacc[t2][:, s_, half * 512:(half + 1) * 512], in0=pb[:, :],
                                                                                   in1=yacc[t2][:, s_, half * 512:(half + 1) * 512], op=ALU.add),
                         reads=[pkk, yk], writes=[yk])
        if f0 + nf == nfc:
            S.dma("sp", y_d[i * 512:(i + 1) * 512, :].rearrange("(s p) d -> p s d", p=128), yacc[t2][:, :, :],
                  reads=[f"yacc{t2}_{s_}" for s_ in range(4)], writes=[f"y_d{t2}"])
    cx.pop()
    ya = [cx.sb(f"ya{i}", [128, D]) for i in range(2)]
    yb = [cx.sb(f"yb{i}", [128, D]) for i in range(2)]
    x1b = [cx.sb(f"x1b{i}", [128, D]) for i in range(2)]
    yo = [cx.sb(f"syo{i}", [128, D]) for i in range(2)]
    st = cx.sb("bnst3", [128, 12])
    mv = cx.sb("bnmv3", [128, 4])
    def comb_load(sg):
        i2 = sg % 2
        S.dma("sp", x1b[i2][:, :], x1f_d[sg * 128:(sg + 1) * 128, :], writes=[f"x1b{i2}"])
        S.ind_dma(ya[i2][:, :], y_d[:, :], posi[:, 0, sg:sg + 1], True, reads=["posi"], writes=[f"ya{i2}"])
        S.ind_dma(yb[i2][:, :], y_d[:, :], posi[:, 1, sg:sg + 1], True, reads=["posi"], writes=[f"yb{i2}"])

    comb_load(0)
    for sg in range(nsub):
        i2 = sg % 2
        if sg + 1 < nsub:
            comb_load(sg + 1)
        S.op("act", lambda e: e.activation(out=x1b[i2][:, :], in_=x1b[i2][:, :], func=AF.Copy, scale=ALPHA), reads=[f"x1b{i2}"], writes=[f"x1b{i2}"])
        S.op("dve", lambda e, sg=sg: e.scalar_tensor_tensor(out=x1b[i2][:, :], in0=ya[i2][:, :], scalar=gts[:, 0, sg:sg + 1], in1=x1b[i2][:, :],
                                                            op0=ALU.mult, op1=ALU.add), reads=[f"ya{i2}", "gts", f"x1b{i2}"], writes=[f"x1b{i2}"])
        S.op("dve", lambda e, sg=sg: e.scalar_tensor_tensor(out=x1b[i2][:, :], in0=yb[i2][:, :], scalar=gts[:, 1, sg:sg + 1], in1=x1b[i2][:, :],
                                                            op0=ALU.mult, op1=ALU.add), reads=[f"yb{i2}", "gts", f"x1b{i2}"], writes=[f"x1b{i2}"])
        layer_norm_rows(x1b[i2][:, :], f"x1b{i2}", yo[i2][:, :], f"syo{i2}", 2, 3, (st, mv))
        S.dma("sp", xout[sg * 128:(sg + 1) * 128, :], yo[i2][:, :], reads=[f"syo{i2}"], writes=[f"xout{i2}"])
    cx.pop()


GROUPS = [[0, 1, 2, 3], [4, 5, 6, 7]]


def phase_x(cx, st_d, stall_d, pv_d, sin):
    nc, S, c = cx.nc, cx.S, cx.c
    ident = c["ident"]
    ps, pk = cx.ps, cx.psk
    cx.push()
    pv = cx.sb("pvx", [128, NPV])
    S.dma("sp", pv[:], pv_d[:, :], writes=["pv"])
    S.coll("AllGather", st_d[:, :], stall_d[:, :], GROUPS, writes=["stall"])
    Gt = cx.sb("Gt", [128, 4, 4, 256])
    for sg in range(3):
        S.dma("sp", Gt[:, sg, :, :], stall_d[sg, :].rearrange("(h d c) -> d h c", h=4, d=128), reads=["stall"], writes=[f"Gt{sg}"])
    pmt = [cx.sb(f"pmt{i}", [128, 128]) for i in range(2)]
    t2 = cx.sb("t2", [128, 128])
    t3 = cx.sb("t3", [128, 128])
    for h in range(4):
        for i, sg in enumerate((1, 2)):
            S.op("pe", lambda e, sg=sg, i=i: e.transpose(out=ps[i][:, 0:128], in_=Gt[:, sg, h, 128:256], identity=ident[:]),
                 reads=[f"Gt{sg}", "c_ident"], writes=[pk[i]])
            S.op("dve", lambda e, i=i: e.tensor_copy(out=pmt[i][:], in_=ps[i][:, 0:128]), reads=[pk[i]], writes=[f"pmt{i}"])
        S.op("pe", lambda e: e.matmul(ps[2][:, 0:128], lhsT=pmt[0][:], rhs=Gt[:, 0, h, 0:128], start=True, stop=True),
             reads=["pmt0", "Gt0"], writes=[pk[2]])
        S.op("dve", lambda e: e.tensor_tensor(out=t2[:], in0=Gt[:, 1, h, 0:128], in1=ps[2][:, 0:128], op=ALU.add), reads=[pk[2], "Gt1"], writes=["t2"])
        S.op("pe", lambda e: e.matmul(ps[3][:, 0:128], lhsT=pmt[1][:], rhs=t2[:], start=True, stop=True), reads=["pmt1", "t2"], writes=[pk[3]])
        S.op("dve", lambda e: e.tensor_tensor(out=t3[:], in0=Gt[:, 2, h, 0:128], in1=ps[3][:, 0:128], op=ALU.add), reads=[pk[3], "Gt2"], writes=["t3"])
        S.op("dve", lambda e: e.tensor_scalar(out=sin[h][:, :], in0=Gt[:, 0, h, 0:128], scalar1=pv[:, PV_MSEG + 1:PV_MSEG + 2], scalar2=None, op0=ALU.mult),
             reads=["Gt0", "pv"], writes=[f"sin{h}"])
        S.op("dve", lambda e: e.scalar_tensor_tensor(out=sin[h][:, :], in0=t2[:], scalar=pv[:, PV_MSEG + 2:PV_MSEG + 3], in1=sin[h][:, :],
                                                     op0=ALU.mult, op1=ALU.add), reads=["t2", "pv", f"sin{h}"], writes=[f"sin{h}"])
        S.op("dve", lambda e: e.scalar_tensor_tensor(out=sin[h][:, :], in0=t3[:], scalar=pv[:, PV_MSEG + 3:PV_MSEG + 4], in1=sin[h][:, :],
                                                     op0=ALU.mult, op1=ALU.add), reads=["t3", "pv", f"sin{h}"], writes=[f"sin{h}"])
    cx.pop()


def phase_h(cx, S_core, x1in, hall_d, pv_d):
    S = cx.S
    cx.push()
    pv = cx.sb("pvh", [128, NPV])
    S.dma("sp", pv[:], pv_d[:, :], writes=["pv"])
    S.coll("AllGather", x1in[S_core:S_core + HALO, :], hall_d[:, :], GROUPS, writes=["hall"])
    ht = [cx.sb(f"ht{i}", [128, D]) for i in range(2)]
    hacc = cx.sb("hacc", [128, D])
    for r in range(4):
        S.dma("sp", ht[r % 2][:, :], hall_d[r * 128:(r + 1) * 128, :], reads=["hall"], writes=[f"ht{r % 2}"])
        if r == 0:
            S.op("dve", lambda e: e.tensor_scalar(out=hacc[:, :], in0=ht[0][:, :], scalar1=pv[:, PV_MPRED:PV_MPRED + 1], scalar2=None, op0=ALU.mult),
                 reads=["ht0", "pv"], writes=["hacc"])
        else:
            S.op("dve", lambda e, r=r: e.scalar_tensor_tensor(out=hacc[:, :], in0=ht[r % 2][:, :], scalar=pv[:, PV_MPRED + r:PV_MPRED + r + 1],
                                                              in1=hacc[:, :], op0=ALU.mult, op1=ALU.add),
                 reads=[f"ht{r % 2}", "pv", "hacc"], writes=["hacc"])
    S.dma("sp", x1in[0:HALO, :], hacc[:, :], reads=["hacc"], writes=["x1halo"])
    cx.pop()


def build_fused(S_core, depth=2):
    nc = bass.Bass("TRN2", target_bir_lowering=False)
    cx = Ctx(nc)
    cx.push()
    cx.consts()
    di = lambda n, s: nc.dram_tensor(n, s, F32, kind="ExternalInput").ap()
    dn = lambda n, s: nc.dram_tensor(n, s, F32).ap()
    xin0 = di("xin0", [HALO + S_core, D])
    win = di("win", [depth, D, IN_COLS])
    wout = di("wout", [depth, D, D])
    pv = di("pv", [depth, 128, NPV])
    lnrows = di("lnrows", [depth, 4, 128, D])
    dense = {"moe": False, "wg": di("dwg", [1, D, DFF_DENSE]), "wu": di("dwu", [1, D, DFF_DENSE]), "wd": di("dwd", [1, DFF_DENSE, D])}
    moe = {"moe": True, "router": di("router", [D, NEXP]), "wg": di("mwg", [NEXP, D, DFF_EXP]),
           "wu": di("mwu", [NEXP, D, DFF_EXP]), "wd": di("mwd", [NEXP, DFF_EXP, D])}
    xout = nc.dram_tensor("xout", [S_core, D], F32, kind="ExternalOutput").ap()
    yc = dn("yc_i", [512, S_core])
    zg = dn("zg_i", [512, S_core])
    qkv = dn("qkv_i", [4, 3, 128, S_core])
    gb = dn("gb_i", [4, 2, S_core])
    oc = dn("oc_i", [2, 512, S_core])
    st = dn("st_i", [1, 4 * 128 * 256])
    stall = dn("stall_i", [4, 4 * 128 * 256])
    x1in = dn("x1in_i", [HALO + S_core, D])
    hall = dn("hall_i", [4 * HALO, D])
    stv = st[0, :].rearrange("(h d c) -> h d c", h=4, d=128)
    NTs = 2 * S_core // 512 + NEXP - 1
    scr = {"x1f": dn("x1f_i", [S_core, D]), "s2t": nc.dram_tensor("s2t_i", [NTs * 512, 1], I32).ap(), "y": dn("y_i", [NTs * 512, D])}
    for l in range(depth):
        cx.push()
        sin = [cx.sb(f"sin{h}", [128, 128]) for h in range(4)]
        xin = xin0 if l == 0 else x1in
        phase_a(cx, S_core, xin, win[l], pv[l], yc, zg, qkv, gb)
        phase_b(cx, S_core, qkv, gb, pv[l], oc, stv)
        phase_x(cx, st, stall, pv[l], sin)
        last = (l == depth - 1)
        dst = xout if last else x1in[HALO:HALO + S_core, :]
        phase_c(cx, S_core, oc, yc, zg, xin, wout[l], lnrows[l], pv[l], moe if l % 2 == 1 else dense, dst, sin, scr)
        if not last:
            phase_h(cx, S_core, x1in, hall, pv[l])
        cx.pop()
    cx.pop()
    return nc


_CACHE = {}


def _pvec(inp, l, seg, S_core):
    f = np.float32
    pv = np.zeros((128, NPV), f)
    dw = np.asarray(inp["conv_dw_w"][l], f)
    pv[:, PV_DWW:PV_DWW + 124] = dw.reshape(31, 4, 128).transpose(2, 1, 0).reshape(128, 124)
    pv[:, PV_DWB:PV_DWB + 4] = np.asarray(inp["conv_dw_b"][l], f).reshape(4, 128).T
    pv[:, PV_CLG:PV_CLG + 4] = np.asarray(inp["conv_ln_g"][l], f).reshape(4, 128).T
    pv[:, PV_CLB:PV_CLB + 4] = np.asarray(inp["conv_ln_b"][l], f).reshape(4, 128).T
    sc = np.asarray(inp["short_conv_w"][l], f)
    pv[:, PV_SCW:PV_SCW + 48] = sc.reshape(4, 12, 128).transpose(2, 1, 0).reshape(128, 48)
    pv[:, PV_ONG] = np.asarray(inp["out_norm_g"][l], f)
    alog = np.asarray(inp["a_log"][l], f)
    dtb = np.asarray(inp["dt_bias"][l], f)
    p = np.arange(128)
    h128 = np.minimum(p // (S_core // 128), 3)
    pv[:, PV_ALOG128] = alog[h128]
    pv[:, PV_DTB128] = dtb[h128]
    for i in range(2):
        h64 = np.minimum((i * 128 + p) // (S_core // 64), 3)
        pv[:, PV_ALOG64 + i] = alog[h64]
        pv[:, PV_DTB64 + i] = dtb[h64]
    pv[:, PV_MSEG + seg] = 1.0
    if seg >= 1:
        pv[:, PV_MPRED + seg - 1] = 1.0
    return pv


def kernel(**inp):
    x = np.asarray(inp["x"], np.float32)
    B, S_tot, _ = x.shape
    S_core = S_tot // NSEG
    cores = list(range(NCORES))
    depth = inp["w_in"].shape[0]
    if ("f", S_core) not in _CACHE:
        _CACHE[("f", S_core)] = build_fused(S_core, depth)
    prog = _CACHE[("f", S_core)]
    ca = lambda a: np.ascontiguousarray(np.asarray(a, dtype=np.float32))
    lnrows = ca(np.stack([np.stack([np.broadcast_to(np.asarray(inp[k][l], np.float32), (128, D))
                                    for k in ("ln_mix_g", "ln_mix_b", "ln_ffn_g", "ln_ffn_b")], 0) for l in range(depth)], 0))
    shared = {"win": ca(inp["w_in"]), "wout": ca(inp["w_out"]), "lnrows": lnrows,
              "dwg": ca(inp["ffn_w_gate"][0:1]), "dwu": ca(inp["ffn_w_up"][0:1]), "dwd": ca(inp["ffn_w_down"][0:1]),
              "router": ca(inp["router_w"][0]), "mwg": ca(inp["moe_w_gate"][0]), "mwu": ca(inp["moe_w_up"][0]),
              "mwd": ca(inp["moe_w_down"][0])}
    in_maps = []
    for cidx in cores:
        b, sg = divmod(cidx, NSEG)
        buf = np.zeros((HALO + S_core, D), np.float32)
        lo = sg * S_core - HALO
        if lo >= 0:
            buf[:] = x[b, lo:lo + HALO + S_core]
        else:
            buf[HALO:] = x[b, 0:S_core]
        d = {"xin0": buf, "pv": ca(np.stack([_pvec(inp, l, sg, S_core) for l in range(depth)], 0))}
        d.update(shared)
        in_maps.append(d)
    res = run_bass_kernel_spmd(prog, in_maps, core_ids=cores).results
    out = np.empty_like(x)
    for cidx in cores:
        b, sg = divmod(cidx, NSEG)
        out[b, sg * S_core:(sg + 1) * S_core] = res[cidx]["xout"]
    return out
```

```python
import re
import numpy as np
import concourse.bass as bass
import concourse.mybir as mybir
from concourse.bass_utils import run_bass_kernel_spmd

F32 = mybir.dt.float32
BF16 = mybir.dt.bfloat16
I32 = mybir.dt.int32
AF = mybir.ActivationFunctionType
ALU = mybir.AluOpType
AX = mybir.AxisListType

D = 1024
NCORES = 8
NSEG = 4
HALO = 128
CW = 31
ALPHA = 4.0 ** 0.25
LN_EPS = 1e-5
RMS_EPS = 1e-6
L2_EPS = 1e-6
IN_COLS = 3080
DFF_DENSE = 2816
DFF_EXP = 3584
NEXP = 8
PV_DWW = 0
PV_DWB = 124
PV_CLG = 128
PV_CLB = 132
PV_SCW = 136
PV_ONG = 184
PV_ALOG128 = 185
PV_ALOG64 = 186
PV_DTB128 = 188
PV_DTB64 = 189
PV_MSEG = 192
PV_MPRED = 196
NPV = 200


class Sched:
    SEM_MAX = 30000

    def __init__(self, nc, dma_ring=12):
        self.nc = nc
        self.engs = {"pe": nc.tensor, "dve": nc.vector, "act": nc.scalar, "pool": nc.gpsimd, "sp": nc.sync}
        self.cnt = {e: 0 for e in self.engs}
        self.csem = {e: [] for e in self.engs}
        self.ring = {}
        self.ring_n = dma_ring
        self.dcnt = {e: 0 for e in self.engs}
        self.last_w = {}
        self.readers = {}
        self.seen = {e: {} for e in self.engs}
        self.nsem = 0
        self.ctoks = []

    def _newsem(self, name):
        self.nsem += 1
        return self.nc.alloc_semaphore(name=f"{name}_{self.nsem}")

    def _wait(self, eng, tok):
        sem, val, semid, src = tok
        if self.seen[eng].get(semid, 0) >= val:
            return
        if src == eng and eng == "pe":
            return
        self.engs[eng].wait_ge(sem, val)
        self.seen[eng][semid] = val

    def _deps(self, eng, reads, writes):
        toks = []
        for k in reads:
            if k in self.last_w:
                toks.append(self.last_w[k])
        for k in writes:
            if k in self.last_w:
                toks.append(self.last_w[k])
            toks.extend(self.readers.get(k, {}).values())
        for t in toks:
            self._wait(eng, t)

    def _record(self, tok, reads, writes):
        for k in reads:
            d = self.readers.setdefault(k, {})
            old = d.get(tok[2])
            if old is None or old[1] < tok[1]:
                d[tok[2]] = tok
        for k in writes:
            self.last_w[k] = tok
            self.readers[k] = {}

    def op(self, eng, fn, reads=(), writes=()):
        writes = list(writes) + [k for k in reads if k.startswith("ps")]
        reads = [k for k in reads if not k.startswith("ps")]
        self._deps(eng, reads, writes)
        inst = fn(self.engs[eng])
        n = self.cnt[eng]
        si, v = divmod(n, self.SEM_MAX)
        if si >= len(self.csem[eng]):
            self.csem[eng].append(self._newsem(f"c_{eng}"))
        sem = self.csem[eng][si]
        inst.then_inc(sem, 1)
        self.cnt[eng] = n + 1
        tok = (sem, v + 1, f"c_{eng}_{si}", eng)
        self._record(tok, reads, writes)
        return tok

    def dma(self, eng, out, in_, reads=(), writes=(), **kw):
        if eng not in self.ring:
            self.ring[eng] = [[self._newsem(f"d_{eng}"), 0] for _ in range(self.ring_n)]
        i = self.dcnt[eng]
        slot = self.ring[eng][i % self.ring_n]
        semid = f"d_{eng}_{i % self.ring_n}"
        if slot[1] > 0:
            self._wait(eng, (slot[0], 16 * slot[1], semid, "dma"))
        self._deps(eng, reads, writes)
        inst = self.engs[eng].dma_start(out=out, in_=in_, **kw)
        self.last_inst = inst
        slot[1] += 1
        inst.then_inc(slot[0], 16)
        self.dcnt[eng] = i + 1
        tok = (slot[0], 16 * slot[1], semid, "dma")
        self._record(tok, reads, writes)
        return tok

    def ind_dma(self, out, in_, idx_ap, gather, reads=(), writes=()):
        eng = "pool"
        if eng not in self.ring:
            self.ring[eng] = [[self._newsem(f"d_{eng}"), 0] for _ in range(self.ring_n)]
        i = self.dcnt[eng]
        slot = self.ring[eng][i % self.ring_n]
        semid = f"d_{eng}_{i % self.ring_n}"
        if slot[1] > 0:
            self._wait(eng, (slot[0], 16 * slot[1], semid, "dma"))
        self._deps(eng, reads, writes)
        off = bass.IndirectOffsetOnAxis(ap=idx_ap, axis=0)
        if gather:
            inst = self.nc.gpsimd.indirect_dma_start(out=out, out_offset=None, in_=in_, in_offset=off)
        else:
            inst = self.nc.gpsimd.indirect_dma_start(out=out, out_offset=off, in_=in_, in_offset=None)
        slot[1] += 1
        inst.then_inc(slot[0], 16)
        self.dcnt[eng] = i + 1
        tok = (slot[0], 16 * slot[1], semid, "dma")
        self._record(tok, reads, writes)
        return tok

    def coll(self, kind, in_ap, out_ap, groups, reads=(), writes=()):
        self._deps("pool", reads, writes)
        sem = self._newsem("cc")
        inst = self.nc.gpsimd.collective_compute(kind, ALU.bypass, replica_groups=groups, ins=[in_ap], outs=[out_ap])
        inst.then_inc(sem)
        tok = (sem, 1, f"cc_{self.nsem}", "dma")
        self.ctoks.append(tok)
        self._record(tok, reads, writes)
        return tok

    def barrier(self):
        toks = list(self.ctoks)
        for e in self.engs:
            n = self.cnt[e]
            if n:
                si, v = divmod(n - 1, self.SEM_MAX)
                toks.append((self.csem[e][si], v + 1, f"c_{e}_{si}", e))
        for q, slots in self.ring.items():
            for j, (sem, c) in enumerate(slots):
                if c:
                    toks.append((sem, 16 * c, f"d_{q}_{j}", "dma"))
        for e in self.engs:
            for t in toks:
                if t[3] == e:
                    continue
                self._wait(e, t)
        self.last_w = {}
        self.readers = {}


class Ctx:
    def __init__(self, nc):
        self.nc = nc
        self.S = Sched(nc)
        self.scopes = []
        self.ps = [nc.alloc_psum_tensor(f"psb{i}", [128, 512], F32) for i in range(8)]
        self.psk = [f"ps{i}" for i in range(8)]
        self.uid = 0

    def sb(self, name, shape, dt=F32):
        self.uid += 1
        g = self.nc.sbuf_tensor(f"{name}_{self.uid}", list(shape), dt)
        t = g.__enter__()
        self.scopes[-1].append(g)
        return t

    def push(self):
        self.scopes.append([])

    def pop(self):
        self.S.barrier()
        for g in reversed(self.scopes.pop()):
            g.__exit__(None, None, None)

    def consts(self):
        S = self.S
        c = {}
        c["ident"] = self.sb("ident", [128, 128])
        c["ones"] = self.sb("ones", [128, 128])
        S.op("pool", lambda e: e.memset(c["ident"][:], 1.0), writes=["c_ident"])
        S.op("pool", lambda e: e.affine_select(out=c["ident"][:], in_=c["ident"][:], pattern=[[-1, 128]],
                                               compare_op=ALU.is_equal, fill=0.0, base=0, channel_multiplier=1),
             reads=["c_ident"], writes=["c_ident"])
        S.op("pool", lambda e: e.memset(c["ones"][:], 1.0), writes=["c_ones"])
        self.c = c
        return c


def phase_a(cx, S_core, xin, win, pv_d, yc_d, zg_d, qkv_d, gb_d):
    nc, S, c = cx.nc, cx.S, cx.c
    T = min(256, S_core)
    cx.push()
    wbf = cx.sb("wbf", [128, 8, IN_COLS], BF16)
    winv = win.rearrange("(kc p) n -> p kc n", p=128)
    for kc in range(8):
        S.dma("pool", wbf[:, kc, :], winv[:, kc, :], writes=[f"wbf{kc}"])
    wkeys = [f"wbf{kc}" for kc in range(8)]
    pv = cx.sb("pv", [128, NPV])
    S.dma("sp", pv[:], pv_d[:, :], writes=["pv"])
    o512 = cx.sb("o512", [128, 128])
    S.op("pool", lambda e: e.memset(o512[:], 1.0 / 512.0), writes=["o512"])
    xt = [cx.sb(f"xt{i}", [128, 2, D]) for i in range(2)]
    xT = cx.sb("xT", [128, 8, T], BF16)
    U = cx.sb("U", [128, 4, 30 + T], BF16)
    dgw = cx.sb("dgw", [128, 4, CW, 128], BF16)
    for cc in range(4):
        for j in range(CW):
            eng = "pool" if (cc * CW + j) % 2 == 0 else "dve"
            S.op(eng, lambda e, cc=cc, j=j: e.tensor_scalar(out=dgw[:, cc, j, :], in0=c["ident"][:],
                                                            scalar1=pv[:, PV_DWW + cc * 31 + j:PV_DWW + cc * 31 + j + 1], scalar2=None, op0=ALU.mult),
                 reads=["c_ident", "pv"], writes=[f"dgw{cc}_{eng}"])
    PRE = cx.sb("PRE", [128, 12, 3 + T])
    sg = cx.sb("sg", [128, 4, T])
    acc = cx.sb("acc", [128, 4, T])
    sq = [cx.sb(f"sq{i}", [128, T]) for i in range(2)]
    mean = cx.sb("mean", [128, T])
    rstd = cx.sb("rstd", [128, T])
    ycs = cx.sb("ycs", [128, 4, T])
    zgs = cx.sb("zgs", [128, 4, T])
    qa = [cx.sb(f"qa{i}", [128, T]) for i in range(12)]
    lg = cx.sb("lg", [8, T])
    ps, pk = cx.ps, cx.psk
    pctr = [0]

    def bank():
        i = pctr[0] % 8
        pctr[0] += 1
        return ps[i], pk[i]

    ntiles = S_core // T
    tiles = [(-1, 128)] + [(i, T) for i in range(ntiles)]

    def xload(it):
        ti, Tt = tiles[it]
        r0 = 0 if ti < 0 else HALO + ti * T
        S.dma("sp", xt[it % 2][:, 0:Tt // 128, :], xin[r0:r0 + Tt, :].rearrange("(s p) d -> p s d", p=128), writes=[f"xt{it % 2}"])

    def stage1(it):
        ti, Tt = tiles[it]
        nsub = Tt // 128
        xb = xt[it % 2]
        xk = f"xt{it % 2}"
        if it < 2:
            xload(it)
        for kc in range(8):
            pb, pkk = bank()
            if kc == 7 and it + 2 < len(tiles):
                pass
            for s_ in range(nsub):
                S.op("pe", lambda e, s_=s_, kc=kc, pb=pb: e.transpose(out=pb[:, s_ * 128:(s_ + 1) * 128],
                                                                       in_=xb[:, s_, kc * 128:(kc + 1) * 128],
                                                                       identity=c["ident"][:]),
                     reads=[xk, "c_ident"], writes=[pkk])
            if kc % 2 == 0:
                S.op("act", lambda e, kc=kc, pb=pb: e.activation(out=xT[:, kc, 0:Tt], in_=pb[:, 0:Tt], func=AF.Copy),
                     reads=[pkk], writes=[f"xT{kc}"])
            else:
                S.op("dve", lambda e, kc=kc, pb=pb: e.tensor_copy(out=xT[:, kc, 0:Tt], in_=pb[:, 0:Tt]),
                     reads=[pkk], writes=[f"xT{kc}"])
        xTk = [f"xT{kc}" for kc in range(8)]
        if it + 2 < len(tiles):
            xload(it + 2)

        def hchunk(oc):
            pb, pkk = bank()
            for kc in range(8):
                S.op("pe", lambda e, kc=kc, pb=pb: e.matmul(pb[:, 0:Tt], lhsT=wbf[:, kc, oc * 128:(oc + 1) * 128],
                                                            rhs=xT[:, kc, 0:Tt], start=(kc == 0), stop=(kc == 7)),
                     reads=[wkeys[kc], xTk[kc]], writes=[pkk])
            return pb, pkk

        for cc in range(4):
            pb, pkk = hchunk(4 + cc)
            S.op("act", lambda e, cc=cc, pb=pb: e.activation(out=sg[:, cc, 0:Tt], in_=pb[:, 0:Tt], func=AF.Sigmoid),
                 reads=[pkk], writes=[f"sg{cc}"])
        for cc in range(4):
            pb, pkk = hchunk(cc)
            S.op("dve", lambda e, cc=cc, pb=pb: e.tensor_tensor(out=U[:, cc, 30:30 + Tt], in0=pb[:, 0:Tt],
                                                                in1=sg[:, cc, 0:Tt], op=ALU.mult),
                 reads=[pkk, f"sg{cc}"], writes=[f"U{cc}"])
        for j in range(12):
            pb, pkk = hchunk(8 + j)
            S.op("act", lambda e, j=j, pb=pb: e.activation(out=PRE[:, j, 3:3 + Tt], in_=pb[:, 0:Tt], func=AF.Copy),
                 reads=[pkk], writes=[f"PRE{j}"])
        if ti < 0:
            for cc in range(4):
                S.op("pool", lambda e, cc=cc: e.tensor_copy(out=U[:, cc, 0:30], in_=U[:, cc, Tt:Tt + 30]),
                     reads=[f"U{cc}"], writes=[f"U{cc}"])
            for j in range(12):
                S.op("pool", lambda e, j=j: e.tensor_copy(out=PRE[:, j, 0:3], in_=PRE[:, j, Tt:Tt + 3]),
                     reads=[f"PRE{j}"], writes=[f"PRE{j}"])
            return
        t0 = ti * T
        for cc in range(4):
            pb, pkk = hchunk(20 + cc)
            S.op("act", lambda e, cc=cc, pb=pb: e.activation(out=zgs[:, cc, 0:Tt], in_=pb[:, 0:Tt], func=AF.Silu),
                 reads=[pkk], writes=[f"zgs{cc}"])
            S.dma("pool", zg_d[cc * 128:(cc + 1) * 128, t0:t0 + Tt], zgs[:, cc, 0:Tt], reads=[f"zgs{cc}"], writes=[f"zg_d{cc}"])
        pb, pkk = bank()
        for kc in range(8):
            S.op("pe", lambda e, kc=kc, pb=pb: e.matmul(pb[0:8, 0:Tt], lhsT=wbf[:, kc, 3072:3080], rhs=xT[:, kc, 0:Tt],
                                                        start=(kc == 0), stop=(kc == 7)),
                 reads=[wkeys[kc], xTk[kc]], writes=[pkk])
        S.op("act", lambda e, pb=pb: e.activation(out=lg[0:8, 0:Tt], in_=pb[0:8, 0:Tt], func=AF.Copy), reads=[pkk], writes=["lg"])
        for w in range(2):
            for h in range(4):
                S.dma("pool", gb_d[h, w:w + 1, t0:t0 + Tt], lg[w * 4 + h:w * 4 + h + 1, 0:Tt], reads=["lg"], writes=[f"gb_d{w}{h}"])

    def conv(it):
        ti, Tt = tiles[it]
        for cc in range(4):
            pb, pkk = bank()
            for j in range(CW):
                S.op("pe", lambda e, cc=cc, j=j, pb=pb: e.matmul(pb[:, 0:Tt], lhsT=dgw[:, cc, j, :], rhs=U[:, cc, j:j + Tt],
                                                                 start=(j == 0), stop=(j == CW - 1)),
                     reads=[f"dgw{cc}_pool", f"dgw{cc}_dve", f"U{cc}"], writes=[pkk])
            S.op("act", lambda e, cc=cc, pb=pb: e.activation(out=acc[:, cc, 0:Tt], in_=pb[:, 0:Tt], func=AF.Identity,
                                                             bias=pv[:, PV_DWB + cc:PV_DWB + cc + 1]),
                 reads=[pkk, "pv"], writes=[f"acc{cc}"])
            S.op("pool", lambda e, cc=cc: e.tensor_copy(out=U[:, cc, 0:30], in_=U[:, cc, Tt:Tt + 30]),
                 reads=[f"U{cc}"], writes=[f"U{cc}"])

    def shortconv(it):
        ti, Tt = tiles[it]
        t0 = ti * T
        for j in range(12):
            which, h = j // 4, j % 4
            qb, qk = qa[j], f"qa{j}"
            S.op("dve", lambda e, j=j, qb=qb: e.tensor_scalar(out=qb[:, 0:Tt], in0=PRE[:, j, 0:Tt],
                                                              scalar1=pv[:, PV_SCW + j * 4:PV_SCW + j * 4 + 1], scalar2=None,
                                                              op0=ALU.mult),
                 reads=[f"PRE{j}", "pv"], writes=[qk])
            for tp in range(1, 4):
                S.op("dve", lambda e, j=j, tp=tp, qb=qb: e.scalar_tensor_tensor(
                    out=qb[:, 0:Tt], in0=PRE[:, j, tp:tp + Tt], scalar=pv[:, PV_SCW + j * 4 + tp:PV_SCW + j * 4 + tp + 1],
                    in1=qb[:, 0:Tt], op0=ALU.mult, op1=ALU.add),
                     reads=[f"PRE{j}", qk], writes=[qk])
            S.op("pool", lambda e, j=j: e.tensor_copy(out=PRE[:, j, 0:3], in_=PRE[:, j, Tt:Tt + 3]),
                 reads=[f"PRE{j}"], writes=[f"PRE{j}"])
            S.op("act", lambda e, qb=qb: e.activation(out=qb[:, 0:Tt], in_=qb[:, 0:Tt], func=AF.Silu), reads=[qk], writes=[qk])
            if which == 2:
                S.dma("pool", qkv_d[h, which, :, t0:t0 + Tt], qb[:, 0:Tt], reads=[qk], writes=[f"qkv_d{j}"])

    def finish(it):
        ti, Tt = tiles[it]
        t0 = ti * T
        pm, pmk = bank()
        pq, pqk = bank()
        for cc in range(4):
            S.op("pe", lambda e, cc=cc: e.matmul(pm[:, 0:Tt], lhsT=o512[:], rhs=acc[:, cc, 0:Tt], start=(cc == 0), stop=(cc == 3)),
                 reads=["o512", f"acc{cc}"], writes=[pmk])
        for cc in range(4):
            sb_, sk = sq[cc % 2], f"sq{cc % 2}"
            S.op("act", lambda e, cc=cc, sb_=sb_: e.activation(out=sb_[:, 0:Tt], in_=acc[:, cc, 0:Tt], func=AF.Square),
                 reads=[f"acc{cc}"], writes=[sk])
            S.op("pe", lambda e, cc=cc, sb_=sb_: e.matmul(pq[:, 0:Tt], lhsT=o512[:], rhs=sb_[:, 0:Tt], start=(cc == 0), stop=(cc == 3)),
                 reads=["o512", sk], writes=[pqk])
        S.op("act", lambda e: e.activation(out=mean[:, 0:Tt], in_=pm[:, 0:Tt], func=AF.Copy), reads=[pmk], writes=["mean"])
        S.op("dve", lambda e: e.tensor_tensor(out=rstd[:, 0:Tt], in0=mean[:, 0:Tt], in1=mean[:, 0:Tt], op=ALU.mult),
             reads=["mean"], writes=["rstd"])
        S.op("dve", lambda e: e.tensor_tensor(out=rstd[:, 0:Tt], in0=pq[:, 0:Tt], in1=rstd[:, 0:Tt], op=ALU.subtract),
             reads=[pqk, "rstd"], writes=["rstd"])
        S.op("dve", lambda e: e.tensor_scalar(out=rstd[:, 0:Tt], in0=rstd[:, 0:Tt], scalar1=0.0, scalar2=LN_EPS,
                                              op0=ALU.max, op1=ALU.add), reads=["rstd"], writes=["rstd"])
        S.op("act", lambda e: e.activation(out=rstd[:, 0:Tt], in_=rstd[:, 0:Tt], func=AF.Sqrt), reads=["rstd"], writes=["rstd"])
        S.op("dve", lambda e: e.reciprocal(out=rstd[:, 0:Tt], in_=rstd[:, 0:Tt]), reads=["rstd"], writes=["rstd"])
        for cc in range(4):
            S.op("dve", lambda e, cc=cc: e.tensor_tensor(out=acc[:, cc, 0:Tt], in0=acc[:, cc, 0:Tt], in1=mean[:, 0:Tt], op=ALU.subtract),
                 reads=[f"acc{cc}", "mean"], writes=[f"acc{cc}"])
            S.op("dve", lambda e, cc=cc: e.tensor_tensor(out=acc[:, cc, 0:Tt], in0=acc[:, cc, 0:Tt], in1=rstd[:, 0:Tt], op=ALU.mult),
                 reads=[f"acc{cc}", "rstd"], writes=[f"acc{cc}"])
            S.op("act", lambda e, cc=cc: e.activation(out=ycs[:, cc, 0:Tt], in_=acc[:, cc, 0:Tt], func=AF.Silu,
                                                      scale=pv[:, PV_CLG + cc:PV_CLG + cc + 1],
                                                      bias=pv[:, PV_CLB + cc:PV_CLB + cc + 1]),
                 reads=[f"acc{cc}", "pv"], writes=[f"ycs{cc}"])
            S.dma("pool", yc_d[cc * 128:(cc + 1) * 128, t0:t0 + Tt], ycs[:, cc, 0:Tt], reads=[f"ycs{cc}"], writes=[f"yc_d{cc}"])
        for j in range(8):
            which, h = j // 4, j % 4
            qb, qk = qa[j], f"qa{j}"
            sb_, sk = sq[j % 2], f"sq{j % 2}"
            pb, pkk = bank()
            S.op("act", lambda e, qb=qb, sb_=sb_: e.activation(out=sb_[:, 0:Tt], in_=qb[:, 0:Tt], func=AF.Square), reads=[qk], writes=[sk])
            S.op("pe", lambda e, pb=pb, sb_=sb_: e.matmul(pb[:, 0:Tt], lhsT=c["ones"][:], rhs=sb_[:, 0:Tt], start=True, stop=True),
                 reads=["c_ones", sk], writes=[pkk])
            sc = 128.0 if which == 0 else 1.0
            S.op("dve", lambda e, pb=pb, sc=sc, sb_=sb_: e.tensor_scalar(out=sb_[:, 0:Tt], in0=pb[:, 0:Tt], scalar1=L2_EPS, scalar2=sc,
                                                                         op0=ALU.add, op1=ALU.mult), reads=[pkk], writes=[sk])
            S.op("act", lambda e, sb_=sb_: e.activation(out=sb_[:, 0:Tt], in_=sb_[:, 0:Tt], func=AF.Sqrt), reads=[sk], writes=[sk])
            S.op("dve", lambda e, sb_=sb_: e.reciprocal(out=sb_[:, 0:Tt], in_=sb_[:, 0:Tt]), reads=[sk], writes=[sk])
            S.op("dve", lambda e, qb=qb, sb_=sb_: e.tensor_tensor(out=qb[:, 0:Tt], in0=qb[:, 0:Tt], in1=sb_[:, 0:Tt], op=ALU.mult),
                 reads=[qk, sk], writes=[qk])
            S.dma("pool", qkv_d[h, which, :, t0:t0 + Tt], qb[:, 0:Tt], reads=[qk], writes=[f"qkv_d{j}"])

    stage1(0)
    stage1(1)
    for it in range(1, len(tiles)):
        conv(it)
        shortconv(it)
        if it + 1 < len(tiles):
            stage1(it + 1)
        finish(it)
    cx.pop()


def phase_b(cx, S_core, qkv_r, gb_r, pv_d, oc_d, st_d):
    nc, S, c = cx.nc, cx.S, cx.c
    ident, ones = c["ident"], c["ones"]
    cx.push()
    NB = 4 * S_core // 128
    NCH = 2 * NB
    rps128 = S_core // 128
    rps64 = S_core // 64
    pv = cx.sb("pvb", [128, NPV])
    S.dma("sp", pv[:], pv_d[:, :], writes=["pv"])
    triBD = cx.sb("triBD", [128, 128])
    maskpos = cx.sb("maskpos", [128, 128])
    strict = cx.sb("strict", [128, 128])
    selL = cx.sb("selL", [128, 128])
    sel63 = cx.sb("sel63", [128, 128])
    negexpa = cx.sb("negexpa", [128, 3])
    S.op("pool", lambda e: e.memset(triBD[:], 1.0), writes=["triBD"])
    S.op("pool", lambda e: e.affine_select(out=triBD[:], in_=triBD[:], pattern=[[1, 128]], compare_op=ALU.is_ge,
                                           fill=0.0, base=0, channel_multiplier=-1), reads=["triBD"], writes=["triBD"])
    S.op("pool", lambda e: e.memset(triBD[0:64, 64:128], 0.0), reads=["triBD"], writes=["triBD"])
    S.op("pool", lambda e: e.memset(maskpos[:], 0.0), writes=["maskpos"])
    S.op("pool", lambda e: e.affine_select(out=maskpos[:], in_=maskpos[:], pattern=[[-1, 128]], compare_op=ALU.is_ge,
                                           fill=1e9, base=0, channel_multiplier=1), reads=["maskpos"], writes=["maskpos"])
    S.op("pool", lambda e: e.memset(maskpos[64:128, 0:64], 1e9), reads=["maskpos"], writes=["maskpos"])
    S.op("pool", lambda e: e.memset(strict[:], 1.0), writes=["strict"])
    S.op("pool", lambda e: e.affine_select(out=strict[:], in_=strict[:], pattern=[[-1, 128]], compare_op=ALU.is_ge,
                                           fill=0.0, base=-1, channel_multiplier=1), reads=["strict"], writes=["strict"])
    S.op("pool", lambda e: e.memset(strict[64:128, 0:64], 0.0), reads=["strict"], writes=["strict"])
    S.op("pool", lambda e: e.memset(selL[:], 1.0), writes=["selL"])
    S.op("pool", lambda e: e.affine_select(out=selL[:], in_=selL[:], pattern=[[-64, 2], [0, 64]], compare_op=ALU.is_equal,
                                           fill=0.0, base=-63, channel_multiplier=1), reads=["selL"], writes=["selL"])
    S.op("pool", lambda e: e.memset(sel63[:], 1.0), writes=["sel63"])
    S.op("pool", lambda e: e.affine_select(out=sel63[:], in_=sel63[:], pattern=[[0, 128]], compare_op=ALU.is_equal,
                                           fill=0.0, base=-63, channel_multiplier=1), reads=["sel63"], writes=["sel63"])
    S.op("act", lambda e: e.activation(out=negexpa[:], in_=pv[:, PV_ALOG128:PV_ALOG128 + 3], func=AF.Exp), reads=["pv"], writes=["nea"])
    S.op("dve", lambda e: e.tensor_scalar(out=negexpa[:], in0=negexpa[:], scalar1=-1.0, scalar2=None, op0=ALU.mult),
         reads=["nea"], writes=["nea"])
    ps, pk = cx.ps, cx.psk

    def load_rows(w, width, rps, nrows, name):
        ntile = (nrows + 127) // 128
        tl = [cx.sb(f"{name}{i}", [128, width]) for i in range(ntile)]
        for seg in range(NSEG):
            r = seg * rps
            S.dma("sp", tl[r // 128][r % 128:r % 128 + rps, :], gb_r[seg, w, :].rearrange("(r t) -> r t", t=width),
                  writes=[f"{name}{r // 128}"])
        return tl, ntile

    def gbeta_rows(tl_b, tl_a, ntile, nrows, width, name, c0):
        tmp = cx.sb(f"{name}_tmp", [128, width])
        tmp2 = cx.sb(f"{name}_tmp2", [128, width])
        for i in range(ntile):
            n_p = min(128, nrows - i * 128)
            kb, ka = f"{name}b{i}", f"{name}a{i}"
            S.op("act", lambda e, i=i: e.activation(out=tl_b[i][0:n_p, :], in_=tl_b[i][0:n_p, :], func=AF.Sigmoid), reads=[kb], writes=[kb])
            S.op("dve", lambda e, i=i: e.tensor_scalar(out=tl_a[i][0:n_p, :], in0=tl_a[i][0:n_p, :], scalar1=pv[0:n_p, PV_DTB128 + c0 + i:PV_DTB128 + c0 + i + 1],
                                                       scalar2=None, op0=ALU.add), reads=[ka, "pv"], writes=[ka])
            S.op("act", lambda e, i=i: e.activation(out=tmp[0:n_p, :], in_=tl_a[i][0:n_p, :], func=AF.Abs),
                 reads=[ka], writes=[f"{name}tmp"])
            S.op("act", lambda e: e.activation(out=tmp[0:n_p, :], in_=tmp[0:n_p, :], func=AF.Exp, scale=-1.0), reads=[f"{name}tmp"], writes=[f"{name}tmp"])
            S.op("act", lambda e: e.activation(out=tmp[0:n_p, :], in_=tmp[0:n_p, :], func=AF.Ln, bias=1.0), reads=[f"{name}tmp"], writes=[f"{name}tmp"])
            S.op("dve", lambda e, i=i: e.tensor_scalar(out=tmp2[0:n_p, :], in0=tl_a[i][0:n_p, :], scalar1=0.0, scalar2=None, op0=ALU.max),
                 reads=[ka], writes=[f"{name}tmp2"])
            S.op("dve", lambda e: e.tensor_tensor(out=tmp[0:n_p, :], in0=tmp[0:n_p, :], in1=tmp2[0:n_p, :], op=ALU.add),
                 reads=[f"{name}tmp", f"{name}tmp2"], writes=[f"{name}tmp"])
            S.op("dve", lambda e, i=i: e.tensor_scalar(out=tl_a[i][0:n_p, :], in0=tmp[0:n_p, :], scalar1=negexpa[0:n_p, c0 + i:c0 + i + 1], scalar2=None, op0=ALU.mult),
                 reads=[f"{name}tmp", "nea"], writes=[ka])

    def transpose_rows(tl, ntile, nrows, width, dst, dkey, key):
        for i in range(ntile):
            n_p = min(128, nrows - i * 128)
            S.op("pe", lambda e, i=i: e.transpose(out=ps[6][0:width, 0:n_p], in_=tl[i][0:n_p, :], identity=ident[0:n_p, 0:n_p]),
                 reads=[f"{key}{i}", "c_ident"], writes=[pk[6]])
            S.op("dve", lambda e, i=i: e.tensor_copy(out=dst[0:width, i * 128:i * 128 + n_p], in_=ps[6][0:width, 0:n_p]),
                 reads=[pk[6]], writes=[dkey])

    b128r, nt128 = load_rows(0, 128, rps128, NB, "r128b")
    a128r, _ = load_rows(1, 128, rps128, NB, "r128a")
    b64r, nt64 = load_rows(0, 64, rps64, NCH, "r64b")
    a64r, _ = load_rows(1, 64, rps64, NCH, "r64a")
    gbeta_rows(b128r, a128r, nt128, NB, 128, "r128", 0)
    gbeta_rows(b64r, a64r, nt64, NCH, 64, "r64", 1)
    beta128 = cx.sb("beta128", [128, NB])
    g128 = cx.sb("g128", [128, NB])
    gc128 = cx.sb("gc128", [128, NB])
    egc128 = cx.sb("egc128", [128, NB])
    nbeta128 = cx.sb("nbeta128", [128, NB])
    beg128 = cx.sb("beg128", [128, NB])
    g64 = cx.sb("g64", [64, NCH])
    gc64 = cx.sb("gc64", [64, NCH])
    kdsc64 = cx.sb("kdsc64", [64, NCH])
    egl = cx.sb("egl", [128, NCH])
    transpose_rows(b128r, nt128, NB, 128, beta128, "beta128", "r128b")
    transpose_rows(a128r, nt128, NB, 128, g128, "g128", "r128a")
    transpose_rows(a64r, nt64, NCH, 64, g64, "g64", "r64a")
    S.op("pe", lambda e: e.matmul(ps[6][:, 0:NB], lhsT=triBD[:], rhs=g128[:, :], start=True, stop=True), reads=["triBD", "g128"], writes=[pk[6]])
    S.op("dve", lambda e: e.tensor_copy(out=gc128[:, :], in_=ps[6][:, 0:NB]), reads=[pk[6]], writes=["gc128"])
    S.op("act", lambda e: e.activation(out=egc128[:, :], in_=gc128[:, :], func=AF.Exp), reads=["gc128"], writes=["egc128"])
    S.op("dve", lambda e: e.tensor_scalar(out=nbeta128[:, :], in0=beta128[:, :], scalar1=-1.0, scalar2=None, op0=ALU.mult),
         reads=["beta128"], writes=["nbeta128"])
    S.op("dve", lambda e: e.tensor_tensor(out=beg128[:, :], in0=beta128[:, :], in1=egc128[:, :], op=ALU.mult),
         reads=["beta128", "egc128"], writes=["beg128"])
    S.op("pe", lambda e: e.matmul(ps[7][0:64, 0:NCH], lhsT=triBD[0:64, 0:64], rhs=g64[:, :], start=True, stop=True),
         reads=["triBD", "g64"], writes=[pk[7]])
    S.op("dve", lambda e: e.tensor_copy(out=gc64[:, :], in_=ps[7][0:64, 0:NCH]), reads=[pk[7]], writes=["gc64"])
    S.op("pe", lambda e: e.matmul(ps[6][0:64, 0:NCH], lhsT=sel63[0:64, 0:64], rhs=gc64[:, :], start=True, stop=True),
         reads=["sel63", "gc64"], writes=[pk[6]])
    S.op("dve", lambda e: e.tensor_tensor(out=kdsc64[:, :], in0=ps[6][0:64, 0:NCH], in1=gc64[:, :], op=ALU.subtract),
         reads=[pk[6], "gc64"], writes=["kdsc64"])
    S.op("act", lambda e: e.activation(out=kdsc64[:, :], in_=kdsc64[:, :], func=AF.Exp), reads=["kdsc64"], writes=["kdsc64"])
    S.op("pe", lambda e: e.matmul(ps[7][:, 0:NCH], lhsT=sel63[0:64, :], rhs=gc64[:, :], start=True, stop=True),
         reads=["sel63", "gc64"], writes=[pk[7]])
    S.op("act", lambda e: e.activation(out=egl[:, :], in_=ps[7][:, 0:NCH], func=AF.Exp), reads=[pk[7]], writes=["egl"])

    NBh = S_core // 128
    GRP = min(4, NBh)
    qkv = [cx.sb(f"qkv{i}", [128, 3, GRP * 128]) for i in range(2)]
    oc = [cx.sb(f"oc{i}", [128, 2, GRP * 128]) for i in range(2)]
    St = [[cx.sb(f"St{h}_{i}", [128, 256]) for i in range(2)] for h in range(4)]
    sidx = [0, 0, 0, 0]
    for h in range(4):
        S.op("pool", lambda e, h=h: e.memset(St[h][0][:, 0:128], 0.0), writes=[f"St{h}_0"])
        S.op("pool", lambda e, h=h: e.tensor_copy(out=St[h][0][:, 128:256], in_=ident[:]), reads=["c_ident", f"St{h}_0"], writes=[f"St{h}_0"])
    W = {}
    for par in range(2):
        for b in range(GRP):
            for nm, shp in [("kbg", [128, 128]), ("vb", [128, 128]), ("kdec", [64, 256]), ("dg", [128, 256]), ("tmp", [128, 128]),
                            ("Dm", [128, 128]), ("Ds", [128, 128]), ("TT", [128, 128]), ("Pf", [128, 128]),
                            ("attn", [128, 128]), ("attnT", [64, 256]), ("qg", [128, 128]), ("u", [64, 2, 256]), ("wT", [128, 128])]:
                W[(nm, b, par)] = cx.sb(f"{nm}{b}_{par}", shp)
            for nm in ("P", "PT", "TTb"):
                W[(nm, b, par)] = cx.sb(f"{nm}{b}_{par}", [128, 128], BF16)
            S.op("pool", lambda e, b=b, par=par: e.memset(W[("u", b, par)][:, :, :], 0.0), writes=[f"u{b}_{par}"])
    vnew = [cx.sb(f"vnew{i}", [64, 256]) for i in range(2)]
    pctr = [0]

    def bank():
        i = pctr[0] % 5
        pctr[0] += 1
        return ps[i], pk[i]

    groups = [(g, h) for g in range(NBh // GRP) for h in range(4)]
    blocks = list(range(GRP))

    def prepass(gi):
        g, h = groups[gi]
        par = gi % 2
        st = g * GRP * 128
        qb = qkv[par]
        qk = f"qkv{par}"
        K = lambda nm, b: f"{nm}{b}_{par}"
        Wp = lambda nm, b: W[(nm, b, par)]
        bk = {}
        stages = []


        def s_p1():
            for b in blocks:
                n = h * NBh + g * GRP + b
                cs = slice(b * 128, (b + 1) * 128)
                pb, pkk = bank()
                S.op("pe", lambda e, pb=pb, cs=cs: e.transpose(out=pb[:, 0:128], in_=qb[:, 1, cs], identity=ident[:]), reads=[qk, "c_ident"], writes=[pkk])
                S.op("pe", lambda e, pb=pb, cs=cs: e.transpose(out=pb[:, 128:256], in_=qb[:, 2, cs], identity=ident[:]), reads=[qk, "c_ident"], writes=[pkk])
                S.op("pe", lambda e, pb=pb, b=b: e.transpose(out=pb[0:64, 256:384], in_=qb[:, 1, b * 128 + 64:b * 128 + 128], identity=ident[:]),
                     reads=[qk, "c_ident"], writes=[pkk])
                S.op("act", lambda e, pb=pb, b=b, n=n: e.activation(out=Wp("kbg", b)[:], in_=pb[:, 0:128], func=AF.Copy, scale=beg128[:, n:n + 1]),
                     reads=[pkk, "beg128"], writes=[K("kbg", b)])
                S.op("dve", lambda e, pb=pb, b=b, n=n: e.tensor_scalar(out=Wp("vb", b)[:], in0=pb[:, 128:256], scalar1=beta128[:, n:n + 1], scalar2=None, op0=ALU.mult),
                     reads=[pkk, "beta128"], writes=[K("vb", b)])
                S.op("dve", lambda e, pb=pb, b=b, n=n: e.tensor_scalar(out=Wp("kdec", b)[:, 0:128], in0=pb[0:64, 0:128], scalar1=kdsc64[:, 2 * n:2 * n + 1], scalar2=None, op0=ALU.mult),
                     reads=[pkk, "kdsc64"], writes=[K("kdec", b)])
                S.op("act", lambda e, pb=pb, b=b, n=n: e.activation(out=Wp("kdec", b)[:, 128:256], in_=pb[0:64, 256:384], func=AF.Copy, scale=kdsc64[:, 2 * n + 1:2 * n + 2]),
                     reads=[pkk, "kdsc64"], writes=[K("kdec", b) + "b"])
                S.op("pool", lambda e, b=b, n=n: e.tensor_scalar(out=Wp("dg", b)[:, 0:128], in0=ident[:], scalar1=gc128[:, n:n + 1], scalar2=None, op0=ALU.mult),
                     reads=["c_ident", "gc128"], writes=[K("dg", b)])
                S.op("pool", lambda e, b=b, n=n: e.tensor_scalar(out=Wp("dg", b)[:, 128:256], in0=ident[:], scalar1=egc128[:, n:n + 1], scalar2=None, op0=ALU.mult),
                     reads=["c_ident", "egc128"], writes=[K("dg", b)])
        stages.append(s_p1)

        def s_p2():
            for b in blocks:
                cs = slice(b * 128, (b + 1) * 128)
                pb, pkk = bank()
                bk[b] = (pb, pkk)
                S.op("pe", lambda e, pb=pb, cs=cs: e.matmul(pb[:, 0:128], lhsT=qb[:, 1, cs], rhs=qb[:, 1, cs], start=True, stop=True), reads=[qk], writes=[pkk])
                S.op("pe", lambda e, pb=pb, cs=cs: e.matmul(pb[:, 128:256], lhsT=qb[:, 0, cs], rhs=qb[:, 1, cs], start=True, stop=True), reads=[qk], writes=[pkk])
                S.op("pe", lambda e, pb=pb, b=b: e.matmul(pb[:, 256:512], lhsT=ones[:], rhs=Wp("dg", b)[:, :], start=True, stop=True),
                     reads=["c_ones", K("dg", b)], writes=[pkk])
        stages.append(s_p2)

        def s_p3():
            for b in blocks:
                n = h * NBh + g * GRP + b
                cs = slice(b * 128, (b + 1) * 128)
                pb, pkk = bk[b]
                S.op("dve", lambda e, pb=pb, b=b, n=n: e.scalar_tensor_tensor(out=Wp("tmp", b)[:], in0=pb[:, 256:384], scalar=gc128[:, n:n + 1],
                                                                              in1=maskpos[:], op0=ALU.subtract, op1=ALU.add),
                     reads=[pkk, "gc128", "maskpos"], writes=[K("tmp", b)])
                S.op("act", lambda e, b=b: e.activation(out=Wp("Dm", b)[:], in_=Wp("tmp", b)[:], func=AF.Exp, scale=-1.0),
                     reads=[K("tmp", b)], writes=[K("Dm", b)])
                S.op("pool", lambda e, b=b: e.tensor_tensor(out=Wp("Ds", b)[:], in0=Wp("Dm", b)[:], in1=strict[:], op=ALU.mult),
                     reads=[K("Dm", b), "strict"], writes=[K("Ds", b)])
                S.op("dve", lambda e, pb=pb, b=b, n=n: e.scalar_tensor_tensor(out=Wp("Pf", b)[:], in0=pb[:, 0:128], scalar=nbeta128[:, n:n + 1],
                                                                              in1=Wp("Ds", b)[:], op0=ALU.mult, op1=ALU.mult),
                     reads=[pkk, "nbeta128", K("Ds", b)], writes=[K("Pf", b)])
                S.op("pool", lambda e, b=b: e.tensor_copy(out=Wp("P", b)[:], in_=Wp("Pf", b)[:]), reads=[K("Pf", b)], writes=[K("P", b)])
                S.op("dve", lambda e, pb=pb, b=b: e.tensor_tensor(out=Wp("attn", b)[:], in0=pb[:, 128:256], in1=Wp("Dm", b)[:], op=ALU.mult),
                     reads=[pkk, K("Dm", b)], writes=[K("attn", b)])
                S.op("dve", lambda e, pb=pb, b=b, cs=cs: e.tensor_tensor(out=Wp("qg", b)[:], in0=qb[:, 0, cs], in1=pb[:, 384:512], op=ALU.mult),
                     reads=[pkk, qk], writes=[K("qg", b)])
        stages.append(s_p3)

        def s_p4():
            for b in blocks:
                pb, pkk = bank()
                S.op("pe", lambda e, pb=pb, b=b: e.transpose(out=pb[:, 0:128], in_=Wp("Pf", b)[:], identity=ident[:]), reads=[K("Pf", b), "c_ident"], writes=[pkk])
                S.op("pe", lambda e, pb=pb, b=b: e.transpose(out=pb[0:64, 128:256], in_=Wp("attn", b)[:, 0:64], identity=ident[:]), reads=[K("attn", b), "c_ident"], writes=[pkk])
                S.op("pe", lambda e, pb=pb, b=b: e.transpose(out=pb[0:64, 256:384], in_=Wp("attn", b)[:, 64:128], identity=ident[:]), reads=[K("attn", b), "c_ident"], writes=[pkk])
                S.op("act", lambda e, pb=pb, b=b: e.activation(out=Wp("PT", b)[:], in_=pb[:, 0:128], func=AF.Copy), reads=[pkk], writes=[K("PT", b)])
                S.op("dve", lambda e, pb=pb, b=b: e.tensor_tensor(out=Wp("TT", b)[:], in0=pb[:, 0:128], in1=ident[:], op=ALU.add),
                     reads=[pkk, "c_ident"], writes=[K("TT", b)])
                S.op("pool", lambda e, b=b: e.tensor_copy(out=Wp("TTb", b)[:], in_=Wp("TT", b)[:]), reads=[K("TT", b)], writes=[K("TTb", b)])
                S.op("act", lambda e, pb=pb, b=b: e.activation(out=Wp("attnT", b)[:, :], in_=pb[0:64, 128:384], func=AF.Copy), reads=[pkk], writes=[K("attnT", b)])
        stages.append(s_p4)

        for lvl in range(1, 6):
            def s_sq(lvl=lvl):
                for b in blocks:
                    pb, pkk = bank()
                    bk[b] = (pb, pkk)
                    S.op("pe", lambda e, pb=pb, b=b: e.matmul(pb[:, 0:128], lhsT=Wp("PT", b)[:], rhs=Wp("P", b)[:], start=True, stop=True),
                         reads=[K("PT", b), K("P", b)], writes=[pkk])
                    if lvl < 5:
                        S.op("pe", lambda e, pb=pb, b=b: e.matmul(pb[:, 128:256], lhsT=Wp("P", b)[:], rhs=Wp("PT", b)[:], start=True, stop=True),
                             reads=[K("PT", b), K("P", b)], writes=[pkk])
                for b in blocks:
                    pb, pkk = bk[b]
                    S.op("act", lambda e, pb=pb, b=b: e.activation(out=Wp("P", b)[:], in_=pb[:, 0:128], func=AF.Copy), reads=[pkk], writes=[K("P", b)])
                    if lvl < 5:
                        S.op("dve", lambda e, pb=pb, b=b: e.tensor_copy(out=Wp("PT", b)[:], in_=pb[:, 128:256]), reads=[pkk], writes=[K("PT", b)])
            stages.append(s_sq)

            def s_tt(lvl=lvl):
                for b in blocks:
                    pb, pkk = bank()
                    bk[b] = (pb, pkk)
                    S.op("pe", lambda e, pb=pb, b=b: e.matmul(pb[:, 0:128], lhsT=Wp("P", b)[:], rhs=Wp("TTb", b)[:], start=True, stop=True),
                         reads=[K("P", b), K("TTb", b)], writes=[pkk])
                for b in blocks:
                    pb, pkk = bk[b]
                    S.op("dve", lambda e, pb=pb, b=b: e.tensor_tensor(out=Wp("TT", b)[:], in0=Wp("TT", b)[:], in1=pb[:, 0:128], op=ALU.add),
                         reads=[pkk, K("TT", b)], writes=[K("TT", b)])
                    if lvl < 5:
                        S.op("pool", lambda e, b=b: e.tensor_copy(out=Wp("TTb", b)[:], in_=Wp("TT", b)[:]), reads=[K("TT", b)], writes=[K("TTb", b)])
            stages.append(s_tt)

        def s_p6():
            for b in blocks:
                pb, pkk = bank()
                S.op("pe", lambda e, pb=pb, b=b: e.matmul(pb[0:64, 0:128], lhsT=Wp("TT", b)[:, 0:64], rhs=Wp("vb", b)[:], start=True, stop=True),
                     reads=[K("TT", b), K("vb", b)], writes=[pkk])
                S.op("pe", lambda e, pb=pb, b=b: e.matmul(pb[0:64, 128:256], lhsT=Wp("TT", b)[:, 64:128], rhs=Wp("vb", b)[:], start=True, stop=True),
                     reads=[K("TT", b), K("vb", b)], writes=[pkk])
                S.op("pe", lambda e, pb=pb, b=b: e.matmul(pb[:, 256:384], lhsT=Wp("kbg", b)[:], rhs=Wp("TT", b)[:], start=True, stop=True),
                     reads=[K("TT", b), K("kbg", b)], writes=[pkk])
                S.op("act", lambda e, pb=pb, b=b: e.activation(out=Wp("u", b)[:, :, 0:128], in_=pb[0:64, 0:256].rearrange("p (c e) -> p c e", e=128), func=AF.Copy),
                     reads=[pkk], writes=[K("u", b)])
                S.op("dve", lambda e, pb=pb, b=b: e.tensor_copy(out=Wp("wT", b)[:], in_=pb[:, 256:384]), reads=[pkk], writes=[K("wT", b)])
        stages.append(s_p6)
        return stages

    def seqsteps(gi):
        g, h = groups[gi]
        par = gi % 2
        st = g * GRP * 128
        ob = oc[par]
        ok = f"oc{par}"
        K = lambda nm, b: f"{nm}{b}_{par}"
        Wp = lambda nm, b: W[(nm, b, par)]
        steps = []
        for b in blocks:
            for ch in range(2):
                def s_chunk(b=b, ch=ch):
                    n = h * NBh + g * GRP + b
                    ci = 2 * n + ch
                    si = sidx[h]
                    Sc, Sn = St[h][si], St[h][1 - si]
                    Sck, Snk = f"St{h}_{si}", f"St{h}_{1 - si}"
                    vb_, vk = vnew[ci % 2], f"vnew{ci % 2}"
                    S.op("pe", lambda e: e.matmul(ps[5][0:64, 0:256], lhsT=Wp("wT", b)[:, ch * 64:(ch + 1) * 64], rhs=Sc[:, :], start=True, stop=True),
                         reads=[K("wT", b), Sck], writes=[pk[5]])
                    S.op("dve", lambda e: e.tensor_tensor(out=vb_[:, :], in0=Wp("u", b)[:, ch, :], in1=ps[5][0:64, 0:256], op=ALU.subtract),
                         reads=[K("u", b), pk[5]], writes=[vk])
                    for part in range(2):
                        S.op("pe", lambda e, part=part: e.matmul(ps[6][:, part * 64:part * 64 + 64], lhsT=Sc[:, part * 128:(part + 1) * 128],
                                                                 rhs=Wp("qg", b)[:, ch * 64:(ch + 1) * 64], start=True, stop=False),
                             reads=[Sck, K("qg", b)], writes=[pk[6]])
                        S.op("pe", lambda e, part=part: e.matmul(ps[6][:, part * 64:part * 64 + 64], lhsT=vb_[:, part * 128:(part + 1) * 128],
                                                                 rhs=Wp("attnT", b)[:, ch * 128 + ch * 64:ch * 128 + ch * 64 + 64],
                                                                 start=False, stop=True),
                             reads=[vk, K("attnT", b)], writes=[pk[6]])
                    S.op("act", lambda e: e.activation(out=ob[:, :, b * 128 + ch * 64:b * 128 + ch * 64 + 64],
                                                       in_=ps[6][:, 0:128].rearrange("p (w c) -> p w c", c=64), func=AF.Copy),
                         reads=[pk[6]], writes=[ok])
                    S.op("pe", lambda e: e.matmul(ps[7][:, 0:256], lhsT=Wp("kdec", b)[:, ch * 128:(ch + 1) * 128], rhs=vb_[:, :], start=True, stop=True),
                         reads=[K("kdec", b), K("kdec", b) + "b", vk], writes=[pk[7]])
                    S.op("dve", lambda e: e.scalar_tensor_tensor(out=Sn[:, :], in0=Sc[:, :], scalar=egl[:, ci:ci + 1], in1=ps[7][:, 0:256],
                                                                 op0=ALU.mult, op1=ALU.add),
                         reads=[Sck, "egl", pk[7]], writes=[Snk])
                    sidx[h] = 1 - si
                steps.append(s_chunk)

        def s_out():
            S.dma("sp", oc_d[:, h * 128:(h + 1) * 128, st:st + GRP * 128].rearrange("w e t -> e w t"), ob[:, :, :], reads=[ok], writes=[f"oc_d{par}"])
        steps.append(s_out)
        return steps

    def qload(gi):
        g, h = groups[gi]
        st = g * GRP * 128
        S.dma("sp", qkv[gi % 2][:, :, :], qkv_r[h, :, :, st:st + GRP * 128].rearrange("w d t -> d w t"), writes=[f"qkv{gi % 2}"])

    qload(0)
    if len(groups) > 1:
        qload(1)
    for f in prepass(0):
        f()
    for gi in range(len(groups)):
        nxt = prepass(gi + 1) if gi + 1 < len(groups) else []
        if nxt:
            if gi + 2 < len(groups):
                nxt.insert(3, lambda gi=gi: qload(gi + 2))
        seq = seqsteps(gi)
        n_n, n_s = len(nxt), len(seq)
        si_ = 0
        for k in range(n_n):
            nxt[k]()
            while si_ < n_s and (si_ + 1) * n_n <= (k + 1) * n_s:
                seq[si_]()
                si_ += 1
        while si_ < n_s:
            seq[si_]()
            si_ += 1
    for h in range(4):
        S.dma("sp", st_d[h, :, :], St[h][sidx[h]][:, :], reads=[f"St{h}_{sidx[h]}"], writes=[f"st_d{h}"])
    cx.pop()


def phase_c(cx, S_core, oc_d, yc_d, zg_d, xin, wout, lnrows, pv_d, ffn, xout, sin, scr=None):
    nc, S, c = cx.nc, cx.S, cx.c
    ident, ones = c["ident"], c["ones"]
    ps, pk = cx.ps, cx.psk
    moe = ffn["moe"]
    G = S_core if moe else min(1024, S_core)
    T = min(512, G)
    nsubG = G // 128
    NT = 2 * S_core // 512 + NEXP - 1
    nexp = NEXP if moe else 1
    dff = DFF_EXP if moe else DFF_DENSE
    nfc = dff // 128
    FG = 4
    fgroups = [(f0, min(FG, nfc - f0)) for f0 in range(0, nfc, FG)]
    cx.push()
    pv = cx.sb("pvc", [128, NPV])
    S.dma("sp", pv[:], pv_d[:, :], writes=["pv"])
    o128 = cx.sb("o128", [128, 128])
    S.op("pool", lambda e: e.memset(o128[:], 1.0 / 128.0), writes=["o128"])
    lnp = cx.sb("lnp", [128, 4, D])
    for i in range(4):
        S.dma("sp", lnp[:, i, :], lnrows[i, :, :], writes=[f"lnp{i}"])
    wo = cx.sb("wo", [128, 8, D], BF16)
    wov = wout.rearrange("(kc p) n -> p kc n", p=128)
    for kc in range(8):
        S.dma("pool", wo[:, kc, :], wov[:, kc, :], writes=[f"wo{kc}"])
    if not moe:
        accb = cx.sb("accb", [128, nsubG, D])
        x1T = cx.sb("x1T", [128, 8, G], BF16)
    if moe:
        rw = cx.sb("rw", [128, 8, NEXP])
        S.dma("sp", rw[:, :, :], ffn["router"].rearrange("(kc p) n -> p kc n", p=128), writes=["rw"])
        selA = cx.sb("selA", [128, nsubG, NEXP])
        m1A = cx.sb("m1A", [128, nsubG, NEXP])
        combA = cx.sb("combA", [128, nsubG, NEXP])
        rankA = cx.sb("rankA", [128, nsubG, NEXP])
        base = cx.sb("base", [128, NEXP])
        UT = cx.sb("UT", [128, 128])
        S.op("pool", lambda e: e.memset(base[:], 0.0), writes=["base"])
        S.op("pool", lambda e: e.memset(UT[:], 1.0), writes=["UT"])
        S.op("pool", lambda e: e.affine_select(out=UT[:], in_=UT[:], pattern=[[1, 128]], compare_op=ALU.is_ge,
                                               fill=0.0, base=-1, channel_multiplier=-1), reads=["UT"], writes=["UT"])
    pctr = [0]

    def bank():
        i = pctr[0] % 8
        pctr[0] += 1
        return ps[i], pk[i]

    def layer_norm_rows(src, skey, dst, dkey, gi_, bi_, small):
        st, mv = small
        for hh in range(2):
            S.op("dve", lambda e, hh=hh: e.bn_stats(out=st[:, hh * 6:(hh + 1) * 6], in_=src[:, hh * 512:(hh + 1) * 512]), reads=[skey], writes=["bnst"])
        S.op("dve", lambda e: e.bn_aggr(out=mv[:, 0:2], in_=st[:, 0:12]), reads=["bnst"], writes=["bnmv"])
        S.op("dve", lambda e: e.tensor_scalar(out=mv[:, 2:3], in0=mv[:, 1:2], scalar1=LN_EPS, scalar2=None, op0=ALU.add), reads=["bnmv"], writes=["bnr"])
        S.op("act", lambda e: e.activation(out=mv[:, 2:3], in_=mv[:, 2:3], func=AF.Sqrt), reads=["bnr"], writes=["bnr"])
        S.op("dve", lambda e: e.reciprocal(out=mv[:, 2:3], in_=mv[:, 2:3]), reads=["bnr"], writes=["bnr"])
        S.op("dve", lambda e: e.tensor_scalar(out=mv[:, 3:4], in0=mv[:, 0:1], scalar1=mv[:, 2:3], scalar2=-1.0, op0=ALU.mult, op1=ALU.mult),
             reads=["bnmv", "bnr"], writes=["bnnb"])
        S.op("act", lambda e: e.activation(out=dst, in_=src, func=AF.Identity, scale=mv[:, 2:3], bias=mv[:, 3:4]),
             reads=[skey, "bnr", "bnnb"], writes=[dkey])
        S.op("pool", lambda e: e.tensor_tensor(out=dst, in0=dst, in1=lnp[:, gi_, :], op=ALU.mult), reads=[dkey, f"lnp{gi_}"], writes=[dkey])
        S.op("dve", lambda e: e.tensor_tensor(out=dst, in0=dst, in1=lnp[:, bi_, :], op=ALU.add), reads=[dkey, f"lnp{bi_}"], writes=[dkey])

    for g0 in range(0, S_core, G):
        cx.push()
        xt = [cx.sb(f"cxt{i}", [128, 4, D]) for i in range(2)]
        oTt = [cx.sb(f"coT{i}", [128, 4, 512]) for i in range(2)]
        zgt = [cx.sb(f"czg{i}", [128, 4, 512]) for i in range(2)]
        cTt = [cx.sb(f"ccT{i}", [128, 4, 512]) for i in range(2)]
        yct = [cx.sb(f"cyc{i}", [128, 4, 512], BF16) for i in range(2)]
        ydn = cx.sb("ydn", [128, 4, 512], BF16)
        sqc = cx.sb("sqc", [128, 512])
        rrs = [cx.sb(f"rr{i}", [128, D]) for i in range(2)]
        x1 = cx.sb("x1", [128, D])
        st = cx.sb("bnst", [128, 12])
        mv = cx.sb("bnmv", [128, 4])
        x1f = cx.sb("x1f", [128, 8, 128]) if moe else None
        lgt = cx.sb("lgt", [128, 8, 8]) if moe else None
        tile_starts = list(range(0, G, T))

        def load_tile(it):
            t0 = g0 + tile_starts[it]
            i2 = it % 2
            nsub = T // 128
            S.dma("sp", xt[i2][:, 0:nsub, :], xin[HALO + t0:HALO + t0 + T, :].rearrange("(s p) d -> p s d", p=128), writes=[f"cxt{i2}"])
            S.dma("sp", oTt[i2][:, :, 0:T], oc_d[0, :, t0:t0 + T].rearrange("(h e) t -> e h t", e=128), writes=[f"coT{i2}"])
            S.dma("sp", cTt[i2][:, :, 0:T], oc_d[1, :, t0:t0 + T].rearrange("(h e) t -> e h t", e=128), writes=[f"ccT{i2}"])
            S.dma("sp", zgt[i2][:, :, 0:T], zg_d[:, t0:t0 + T].rearrange("(h e) t -> e h t", e=128), writes=[f"czg{i2}"])
            S.dma("pool", yct[i2][:, :, 0:T], yc_d[:, t0:t0 + T].rearrange("(h e) t -> e h t", e=128), writes=[f"cyc{i2}"])

        load_tile(0)
        for it, tt0 in enumerate(tile_starts):
            t0 = g0 + tt0
            i2 = it % 2
            nsub = T // 128
            if it + 1 < len(tile_starts):
                load_tile(it + 1)
            for h in range(4):
                pb, pkk = bank()
                S.op("pe", lambda e, pb=pb, h=h: e.matmul(pb[:, 0:T], lhsT=sin[h][:, :], rhs=cTt[i2][:, h, 0:T], start=True, stop=True),
                     reads=[f"sin{h}", f"ccT{i2}"], writes=[pkk])
                S.op("dve", lambda e, pb=pb, h=h: e.tensor_tensor(out=oTt[i2][:, h, 0:T], in0=oTt[i2][:, h, 0:T], in1=pb[:, 0:T], op=ALU.add),
                     reads=[pkk, f"coT{i2}"], writes=[f"coT{i2}"])
                pb, pkk = bank()
                S.op("act", lambda e, h=h: e.activation(out=sqc[:, 0:T], in_=oTt[i2][:, h, 0:T], func=AF.Square), reads=[f"coT{i2}"], writes=["sqc"])
                S.op("pe", lambda e, pb=pb: e.matmul(pb[:, 0:T], lhsT=o128[:], rhs=sqc[:, 0:T], start=True, stop=True), reads=["o128", "sqc"], writes=[pkk])
                S.op("dve", lambda e, pb=pb: e.tensor_scalar(out=sqc[:, 0:T], in0=pb[:, 0:T], scalar1=RMS_EPS, scalar2=None, op0=ALU.add), reads=[pkk], writes=["sqc"])
                S.op("act", lambda e: e.activation(out=sqc[:, 0:T], in_=sqc[:, 0:T], func=AF.Sqrt), reads=["sqc"], writes=["sqc"])
                S.op("dve", lambda e: e.reciprocal(out=sqc[:, 0:T], in_=sqc[:, 0:T]), reads=["sqc"], writes=["sqc"])
                S.op("dve", lambda e, h=h: e.tensor_tensor(out=sqc[:, 0:T], in0=sqc[:, 0:T], in1=oTt[i2][:, h, 0:T], op=ALU.mult), reads=["sqc", f"coT{i2}"], writes=["sqc"])
                S.op("dve", lambda e, h=h: e.scalar_tensor_tensor(out=ydn[:, h, 0:T], in0=sqc[:, 0:T], scalar=pv[:, PV_ONG:PV_ONG + 1],
                                                                  in1=zgt[i2][:, h, 0:T], op0=ALU.mult, op1=ALU.mult),
                     reads=["sqc", "pv", f"czg{i2}"], writes=[f"ydn{h}"])
            def wout_stage(s):
                ts = slice(s * 128, (s + 1) * 128)
                rr = rrs[s % 2]
                for half in range(2):
                    pb, pkk = bank()
                    for kc in range(8):
                        lhs = yct[i2][:, kc, ts] if kc < 4 else ydn[:, kc - 4, ts]
                        lk = f"cyc{i2}" if kc < 4 else f"ydn{kc - 4}"
                        S.op("pe", lambda e, pb=pb, lhs=lhs, kc=kc, half=half: e.matmul(pb[:, :], lhsT=lhs, rhs=wo[:, kc, half * 512:(half + 1) * 512],
                                                                                        start=(kc == 0), stop=(kc == 7)),
                             reads=[lk, f"wo{kc}"], writes=[pkk])
                    S.op("dve", lambda e, pb=pb, s=s, half=half, rr=rr: e.scalar_tensor_tensor(out=rr[:, half * 512:(half + 1) * 512], in0=xt[i2][:, s, half * 512:(half + 1) * 512],
                                                                                               scalar=ALPHA, in1=pb[:, :], op0=ALU.mult, op1=ALU.add),
                         reads=[pkk, f"cxt{i2}"], writes=[f"rr{s % 2}"])

            wout_stage(0)
            for s in range(nsub):
                sg_ = (tt0 // 128) + s
                ts = slice(s * 128, (s + 1) * 128)
                if s + 1 < nsub:
                    wout_stage(s + 1)
                layer_norm_rows(rrs[s % 2][:, :], f"rr{s % 2}", x1[:, :], "x1", 0, 1, (st, mv))
                if moe:
                    S.dma("pool", scr["x1f"][t0 + s * 128:t0 + (s + 1) * 128, :], x1[:, :], reads=["x1"], writes=["x1f_d"])
                else:
                    S.op("act", lambda e, sg_=sg_: e.activation(out=accb[:, sg_, :], in_=x1[:, :], func=AF.Copy, scale=ALPHA), reads=["x1"], writes=[f"accb{sg_}"])
                for kc in range(8):
                    if kc % 4 == 0:
                        pb, pkk = bank()
                    q4 = kc % 4
                    S.op("pe", lambda e, pb=pb, kc=kc, q4=q4: e.transpose(out=pb[:, q4 * 128:(q4 + 1) * 128], in_=x1[:, kc * 128:(kc + 1) * 128], identity=ident[:]),
                         reads=["x1", "c_ident"], writes=[pkk])
                    if q4 == 3:
                        k0 = kc - 3
                        if not moe:
                            S.op("act", lambda e, pb=pb, k0=k0, sg_=sg_: e.activation(out=x1T[:, k0:k0 + 4, sg_ * 128:(sg_ + 1) * 128],
                                                                                     in_=pb[:, :].rearrange("p (k t) -> p k t", t=128), func=AF.Copy),
                                 reads=[pkk], writes=[f"x1T{sg_}"])
                        if moe:
                            S.op("dve", lambda e, pb=pb, k0=k0: e.tensor_copy(out=x1f[:, k0:k0 + 4, :], in_=pb[:, :].rearrange("p (k t) -> p k t", t=128)),
                                 reads=[pkk], writes=[f"x1f{k0}"])
                if moe:
                    pb, pkk = bank()
                    for kc in range(8):
                        S.op("pe", lambda e, pb=pb, kc=kc: e.matmul(pb[:, 0:NEXP], lhsT=x1f[:, kc, :], rhs=rw[:, kc, :], start=(kc == 0), stop=(kc == 7)),
                             reads=[f"x1f{(kc // 4) * 4}", "rw"], writes=[pkk])
                    L = lgt
                    S.op("dve", lambda e, pb=pb: e.tensor_copy(out=L[:, 0, :], in_=pb[:, 0:NEXP]), reads=[pkk], writes=["lgt"])
                    S.op("dve", lambda e: e.tensor_reduce(out=L[:, 7, 0:1], in_=L[:, 0, :], axis=AX.X, op=ALU.max), reads=["lgt"], writes=["lgt"])
                    S.op("dve", lambda e, sg_=sg_: e.tensor_scalar(out=m1A[:, sg_, :], in0=L[:, 0, :], scalar1=L[:, 7, 0:1], scalar2=None, op0=ALU.is_equal),
                         reads=["lgt"], writes=["m1A"])
                    S.op("dve", lambda e, sg_=sg_: e.tensor_scalar(out=L[:, 1, :], in0=m1A[:, sg_, :], scalar1=-1e30, scalar2=None, op0=ALU.mult),
                         reads=["lgt", "m1A"], writes=["lgt"])
                    S.op("dve", lambda e: e.tensor_tensor(out=L[:, 1, :], in0=L[:, 1, :], in1=L[:, 0, :], op=ALU.add), reads=["lgt"], writes=["lgt"])
                    S.op("dve", lambda e: e.tensor_reduce(out=L[:, 7, 1:2], in_=L[:, 1, :], axis=AX.X, op=ALU.max), reads=["lgt"], writes=["lgt"])
                    S.op("dve", lambda e: e.tensor_scalar(out=L[:, 2, :], in0=L[:, 0, :], scalar1=L[:, 7, 1:2], scalar2=None, op0=ALU.is_ge), reads=["lgt"], writes=["lgt"])
                    S.op("dve", lambda e: e.tensor_scalar(out=L[:, 3, :], in0=L[:, 0, :], scalar1=L[:, 7, 0:1], scalar2=None, op0=ALU.subtract), reads=["lgt"], writes=["lgt"])
                    S.op("act", lambda e: e.activation(out=L[:, 3, :], in_=L[:, 3, :], func=AF.Exp), reads=["lgt"], writes=["lgt"])
                    S.op("dve", lambda e: e.tensor_tensor(out=L[:, 3, :], in0=L[:, 3, :], in1=L[:, 2, :], op=ALU.mult), reads=["lgt"], writes=["lgt"])
                    S.op("dve", lambda e: e.tensor_reduce(out=L[:, 7, 2:3], in_=L[:, 3, :], axis=AX.X, op=ALU.add), reads=["lgt"], writes=["lgt"])
                    S.op("dve", lambda e: e.reciprocal(out=L[:, 7, 3:4], in_=L[:, 7, 2:3]), reads=["lgt"], writes=["lgt"])
                    S.op("dve", lambda e, sg_=sg_: e.tensor_scalar(out=combA[:, sg_, :], in0=L[:, 3, :], scalar1=L[:, 7, 3:4], scalar2=None, op0=ALU.mult),
                         reads=["lgt"], writes=["combA"])
                    S.op("dve", lambda e, sg_=sg_: e.tensor_copy(out=selA[:, sg_, :], in_=L[:, 2, :]), reads=["lgt"], writes=["selA"])
                    pb, pkk = bank()
                    S.op("pe", lambda e, pb=pb, sg_=sg_: e.matmul(pb[:, 0:NEXP], lhsT=UT[:], rhs=selA[:, sg_, :], start=True, stop=True),
                         reads=["UT", "selA"], writes=[pkk])
                    S.op("pe", lambda e, pb=pb, sg_=sg_: e.matmul(pb[:, NEXP:2 * NEXP], lhsT=ones[:], rhs=selA[:, sg_, :], start=True, stop=True),
                         reads=["c_ones", "selA"], writes=[pkk])
                    S.op("dve", lambda e, pb=pb, sg_=sg_: e.tensor_tensor(out=rankA[:, sg_, :], in0=pb[:, 0:NEXP], in1=base[:], op=ALU.add),
                         reads=[pkk, "base"], writes=["rankA"])
                    S.op("dve", lambda e, pb=pb: e.tensor_tensor(out=base[:], in0=pb[:, NEXP:2 * NEXP], in1=base[:], op=ALU.add),
                         reads=[pkk, "base"], writes=["base"])
        cx.pop()
        if moe:
            moe_sparse(cx, S_core, NT, ffn, scr, xout, lnp, layer_norm_rows, selA, m1A, combA, rankA, base)
            continue
        cx.push()
        wg = [cx.sb(f"wg{i}", [128, 8, FG * 128], BF16) for i in range(2)]
        wu = [cx.sb(f"wu{i}", [128, 8, FG * 128], BF16) for i in range(2)]
        wd = [cx.sb(f"wd{i}", [128, FG, D], BF16) for i in range(2)]
        hT = cx.sb("hT", [128, FG, G], BF16)
        sgb = [cx.sb(f"sgb{i}", [128, 512]) for i in range(2)]
        yo = [cx.sb(f"yo{i}", [128, D]) for i in range(2)]
        st = cx.sb("bnst2", [128, 12])
        mv = cx.sb("bnmv2", [128, 4])
        units = [(e_, f0, nf) for e_ in range(nexp) for (f0, nf) in fgroups]

        def load_w(ui):
            e_, f0, nf = units[ui]
            i2 = ui % 2
            gsrc = ffn["wg"][e_, :, f0 * 128:(f0 + nf) * 128].rearrange("(kc p) n -> p kc n", p=128)
            usrc = ffn["wu"][e_, :, f0 * 128:(f0 + nf) * 128].rearrange("(kc p) n -> p kc n", p=128)
            dsrc = ffn["wd"][e_, f0 * 128:(f0 + nf) * 128, :].rearrange("(f p) n -> p f n", p=128)
            S.dma("pool", wg[i2][:, :, 0:nf * 128], gsrc, writes=[f"wg{i2}"])
            S.dma("pool", wu[i2][:, :, 0:nf * 128], usrc, writes=[f"wu{i2}"])
            S.dma("pool", wd[i2][:, 0:nf, :], dsrc, writes=[f"wd{i2}"])

        load_w(0)
        cnt = 0
        for ui, (e_, f0, nf) in enumerate(units):
            i2 = ui % 2
            if ui + 1 < len(units):
                load_w(ui + 1)
            for tt0 in range(0, G, T):
                for f in range(nf):
                    pg, pgk = bank()
                    pu, puk = bank()
                    for kc in range(8):
                        S.op("pe", lambda e, pg=pg, kc=kc, f=f: e.matmul(pg[:, 0:T], lhsT=wg[i2][:, kc, f * 128:(f + 1) * 128], rhs=x1T[:, kc, tt0:tt0 + T],
                                                                         start=(kc == 0), stop=(kc == 7)),
                             reads=[f"wg{i2}"] + [f"x1T{(tt0 // 128) + s}" for s in range(T // 128)], writes=[pgk])
                    for kc in range(8):
                        S.op("pe", lambda e, pu=pu, kc=kc, f=f: e.matmul(pu[:, 0:T], lhsT=wu[i2][:, kc, f * 128:(f + 1) * 128], rhs=x1T[:, kc, tt0:tt0 + T],
                                                                         start=(kc == 0), stop=(kc == 7)),
                             reads=[f"wu{i2}"] + [f"x1T{(tt0 // 128) + s}" for s in range(T // 128)], writes=[puk])
                    sb_ = sgb[cnt % 2]
                    sk = f"sgb{cnt % 2}"
                    cnt += 1
                    S.op("act", lambda e, pg=pg, sb_=sb_: e.activation(out=sb_[:, 0:T], in_=pg[:, 0:T], func=AF.Silu), reads=[pgk], writes=[sk])
                    S.op("dve", lambda e, pu=pu, sb_=sb_, f=f: e.tensor_tensor(out=hT[:, f, tt0:tt0 + T], in0=sb_[:, 0:T], in1=pu[:, 0:T], op=ALU.mult),
                         reads=[puk, sk], writes=[f"hT{f}_{tt0}"])
            for sg_ in range(nsubG):
                tt0 = (sg_ * 128 // T) * T
                for half in range(2):
                    pb, pkk = bank()
                    for f in range(nf):
                        S.op("pe", lambda e, pb=pb, f=f, sg_=sg_, half=half: e.matmul(pb[:, :], lhsT=hT[:, f, sg_ * 128:(sg_ + 1) * 128],
                                                                                      rhs=wd[i2][:, f, half * 512:(half + 1) * 512],
                                                                                      start=(f == 0), stop=(f == nf - 1)),
                             reads=[f"hT{f}_{tt0}", f"wd{i2}"], writes=[pkk])
                    if moe:
                        S.op("dve", lambda e, pb=pb, sg_=sg_, half=half, e_=e_: e.scalar_tensor_tensor(
                            out=accb[:, sg_, half * 512:(half + 1) * 512], in0=pb[:, :], scalar=comb[:, sg_, e_:e_ + 1],
                            in1=accb[:, sg_, half * 512:(half + 1) * 512], op0=ALU.mult, op1=ALU.add),
                             reads=[pkk, "comb", f"accb{sg_}"], writes=[f"accb{sg_}"])
                    else:
                        S.op("dve", lambda e, pb=pb, sg_=sg_, half=half: e.tensor_tensor(
                            out=accb[:, sg_, half * 512:(half + 1) * 512], in0=pb[:, :],
                            in1=accb[:, sg_, half * 512:(half + 1) * 512], op=ALU.add),
                             reads=[pkk, f"accb{sg_}"], writes=[f"accb{sg_}"])
        for sg_ in range(nsubG):
            yb, yk = yo[sg_ % 2], f"yo{sg_ % 2}"
            layer_norm_rows(accb[:, sg_, :], f"accb{sg_}", yb[:, :], yk, 2, 3, (st, mv))
            S.dma("sp", xout[g0 + sg_ * 128:g0 + (sg_ + 1) * 128, :], yb[:, :], reads=[yk], writes=[f"xout{sg_ % 2}"])
        cx.pop()
    cx.pop()


def moe_sparse(cx, S_core, NT, ffn, scr, xout, lnp, layer_norm_rows, selA, m1A, combA, rankA, base):
    nc, S, c = cx.nc, cx.S, cx.c
    ident = c["ident"]
    ps, pk = cx.ps, cx.psk
    nsub = S_core // 128
    x1f_d, s2t_d, y_d = scr["x1f"], scr["s2t"], scr["y"]
    cx.push()
    pcn = cx.sb("pcn", [128, NEXP])
    tmp8 = cx.sb("tmp8", [128, NEXP])
    offs = cx.sb("offs", [128, NEXP])
    ends = cx.sb("ends", [128, NEXP])
    S.op("dve", lambda e: e.tensor_scalar(out=pcn[:], in0=base[:], scalar1=0.0, scalar2=None, op0=ALU.is_gt), reads=["base"], writes=["pcn"])
    for k in range(1, 2 * S_core // 512 + 1):
        S.op("dve", lambda e, k=k: e.tensor_scalar(out=tmp8[:], in0=base[:], scalar1=512.0 * k, scalar2=None, op0=ALU.is_gt), reads=["base"], writes=["tmp8"])
        S.op("dve", lambda e: e.tensor_tensor(out=pcn[:], in0=pcn[:], in1=tmp8[:], op=ALU.add), reads=["tmp8", "pcn"], writes=["pcn"])
    S.op("dve", lambda e: e.tensor_scalar(out=pcn[:], in0=pcn[:], scalar1=512.0, scalar2=None, op0=ALU.mult), reads=["pcn"], writes=["pcn"])
    S.op("dve", lambda e: e.memset(offs[:], 0.0), writes=["offs"])
    for e_ in range(1, NEXP):
        S.op("dve", lambda e, e_=e_: e.tensor_tensor(out=offs[:, e_:e_ + 1], in0=offs[:, e_ - 1:e_], in1=pcn[:, e_ - 1:e_], op=ALU.add),
             reads=["offs", "pcn"], writes=["offs"])
    S.op("dve", lambda e: e.tensor_tensor(out=ends[:], in0=offs[:], in1=pcn[:], op=ALU.add), reads=["offs", "pcn"], writes=["ends"])
    eidf = cx.sb("eidf", [128, NT])
    eidi = cx.sb("eidi", [128, NT], I32)
    for i in range(NT):
        S.op("dve", lambda e, i=i: e.tensor_scalar(out=tmp8[:], in0=ends[:], scalar1=512.0 * i, scalar2=None, op0=ALU.is_le), reads=["ends"], writes=["tmp8"])
        S.op("dve", lambda e, i=i: e.tensor_reduce(out=eidf[:, i:i + 1], in_=tmp8[:], axis=AX.X, op=ALU.add), reads=["tmp8"], writes=["eidf"])
    S.op("dve", lambda e: e.tensor_scalar(out=eidf[:], in0=eidf[:], scalar1=float(NEXP - 1), scalar2=None, op0=ALU.min), reads=["eidf"], writes=["eidf"])
    NFG = DFF_EXP // 128 // 4
    gi_ = cx.sb("gi_", [128, NFG], I32)
    gf_ = cx.sb("gf_", [128, NFG])
    tabGf = cx.sb("tabGf", [128, NT, NFG])
    tabDf = cx.sb("tabDf", [128, NT, NFG])
    tabG = cx.sb("tabG", [128, NT, NFG], I32)
    tabD = cx.sb("tabD", [128, NT, NFG], I32)
    e1 = cx.sb("e1", [128, NT])
    S.op("pool", lambda e: e.iota(gi_[:], pattern=[[1, NFG]], base=0, channel_multiplier=0), writes=["gi_"])
    S.op("dve", lambda e: e.tensor_copy(out=gf_[:], in_=gi_[:]), reads=["gi_"], writes=["gf_"])
    S.op("dve", lambda e: e.tensor_scalar(out=e1[:], in0=eidf[:], scalar1=float(D * DFF_EXP // 512), scalar2=None, op0=ALU.mult), reads=["eidf"], writes=["e1"])
    for i in range(NT):
        S.op("dve", lambda e, i=i: e.tensor_scalar(out=tabGf[:, i, :], in0=gf_[:], scalar1=e1[:, i:i + 1], scalar2=512.0, op0=ALU.add, op1=ALU.mult),
             reads=["gf_", "e1"], writes=["tabGf"])
    S.op("dve", lambda e: e.tensor_scalar(out=e1[:], in0=eidf[:], scalar1=float(D * DFF_EXP // 524288), scalar2=None, op0=ALU.mult), reads=["eidf", "tabGf"], writes=["e1"])
    for i in range(NT):
        S.op("dve", lambda e, i=i: e.tensor_scalar(out=tabDf[:, i, :], in0=gf_[:], scalar1=e1[:, i:i + 1], scalar2=524288.0, op0=ALU.add, op1=ALU.mult),
             reads=["gf_", "e1"], writes=["tabDf"])
    S.op("dve", lambda e: e.tensor_copy(out=tabG[:], in_=tabGf[:]), reads=["tabGf"], writes=["tabG"])
    S.op("dve", lambda e: e.tensor_copy(out=tabD[:], in_=tabDf[:]), reads=["tabDf"], writes=["tabD"])
    posf = cx.sb("posf", [128, 2, nsub])
    gts = cx.sb("gts", [128, 2, nsub])
    tmpb = cx.sb("tmpb", [128, nsub, NEXP])
    m2A = cx.sb("m2A", [128, nsub, NEXP])
    for sg in range(nsub):
        S.op("dve", lambda e, sg=sg: e.tensor_tensor(out=rankA[:, sg, :], in0=rankA[:, sg, :], in1=offs[:], op=ALU.add),
             reads=["rankA", "offs"], writes=["rankA"])
    S.op("dve", lambda e: e.tensor_tensor(out=m2A[:], in0=selA[:], in1=m1A[:], op=ALU.subtract), reads=["selA", "m1A"], writes=["m2A"])
    for w, mk, mkey in ((0, m1A, "m1A"), (1, m2A, "m2A")):
        S.op("dve", lambda e, mk=mk: e.tensor_tensor(out=tmpb[:], in0=mk[:], in1=rankA[:], op=ALU.mult), reads=[mkey, "rankA"], writes=["tmpb"])
        S.op("dve", lambda e, w=w: e.tensor_reduce(out=posf[:, w, :], in_=tmpb[:], axis=AX.X, op=ALU.add), reads=["tmpb"], writes=["posf"])
        S.op("dve", lambda e, mk=mk: e.tensor_tensor(out=tmpb[:], in0=mk[:], in1=combA[:], op=ALU.mult), reads=[mkey, "combA"], writes=["tmpb"])
        S.op("dve", lambda e, w=w: e.tensor_reduce(out=gts[:, w, :], in_=tmpb[:], axis=AX.X, op=ALU.add), reads=["tmpb"], writes=["gts"])
    posi = cx.sb("posi", [128, 2, nsub], I32)
    tokid = cx.sb("tokid", [128, nsub], I32)
    zer = cx.sb("zer", [128, NT * 4], I32)
    idx_all = cx.sb("idx_all", [128, NT * 4], I32)
    S.op("dve", lambda e: e.tensor_scalar(out=posf[:], in0=posf[:], scalar1=float(NT * 512 - 1), scalar2=0.0, op0=ALU.min, op1=ALU.max),
         reads=["posf"], writes=["posf"])
    S.op("dve", lambda e: e.tensor_copy(out=posi[:], in_=posf[:]), reads=["posf"], writes=["posi"])
    S.op("pool", lambda e: e.iota(tokid[:], pattern=[[128, nsub]], base=0, channel_multiplier=1), writes=["tokid"])
    S.op("pool", lambda e: e.memset(zer[:], 0), writes=["zer"])
    S.dma("sp", s2t_d.rearrange("(p j) o -> p (j o)", p=128), zer[:, :], reads=["zer"], writes=["s2t_d"])
    for w in range(2):
        for sg in range(nsub):
            S.ind_dma(s2t_d[:, :], tokid[:, sg:sg + 1], posi[:, w, sg:sg + 1], False, reads=["tokid", "posi", "s2t_d"], writes=[f"s2t_{w}_{sg}"])
    S.dma("sp", idx_all[:, :], s2t_d.rearrange("(j p) o -> p (j o)", p=128),
          reads=["s2t_d"] + [f"s2t_{w}_{sg}" for w in range(2) for sg in range(nsub)], writes=["idx_all"], allow_slow_non_contiguous=True)

    cx.push()
    FG = 4
    nfc = DFF_EXP // 128
    fgroups = [(f0, min(FG, nfc - f0)) for f0 in range(0, nfc, FG)]
    wg = [cx.sb(f"swg{i}", [128, 8, FG * 128], BF16) for i in range(2)]
    wu = [cx.sb(f"swu{i}", [128, 8, FG * 128], BF16) for i in range(2)]
    wd = [cx.sb(f"swd{i}", [128, FG, D], BF16) for i in range(2)]
    hT = cx.sb("shT", [128, FG, 512], BF16)
    sgb = [cx.sb(f"ssgb{i}", [128, 512]) for i in range(2)]
    xg = [cx.sb(f"xg{i}", [128, D]) for i in range(2)]
    xgT = [cx.sb(f"xgT{i}", [128, 8, 512], BF16) for i in range(2)]
    yacc = [cx.sb(f"yacc{i}", [128, 4, D]) for i in range(2)]
    pctr = [0]

    def bank():
        i = pctr[0] % 8
        pctr[0] += 1
        return ps[i], pk[i]

    regG = nc.gpsimd.alloc_register("offg_reg")
    regD = nc.gpsimd.alloc_register("offd_reg")
    units = [(i, f0, nf) for i in range(NT) for (f0, nf) in fgroups]
    RH = type(regG)

    def dyn_dma(dst, tens, val, ap, key):
        src = bass.AP(tensor=tens.tensor, offset=val, ap=ap)
        S.dma("pool", dst, src, writes=[key])
        js = nc.instruction_to_json(S.last_inst.ins)
        return set(re.findall(r'"reg_ap_offset": "([^"]+)"', js))

    def load_w(ui):
        i, f0, nf = units[ui]
        i2 = ui % 2
        g = f0 // FG
        S._deps("pool", ["tabG", "tabD"], [])
        nc.gpsimd.reg_load(regG, tabG[0:1, i, g:g + 1])
        nc.gpsimd.reg_load(regD, tabD[0:1, i, g:g + 1])
        vG = nc.gpsimd.snap(regG)
        vD = nc.gpsimd.snap(regD)
        tmps = set()
        tmps |= dyn_dma(wg[i2][:, :, 0:nf * 128], ffn["wg"], vG, [[DFF_EXP, 128], [128 * DFF_EXP, 8], [1, nf * 128]], f"swg{i2}")
        tmps |= dyn_dma(wu[i2][:, :, 0:nf * 128], ffn["wu"], vG, [[DFF_EXP, 128], [128 * DFF_EXP, 8], [1, nf * 128]], f"swu{i2}")
        tmps |= dyn_dma(wd[i2][:, 0:nf, :], ffn["wd"], vD, [[D, 128], [128 * D, nf], [1, D]], f"swd{i2}")
        for t in tmps:
            nc.gpsimd.free_register(RH(name=t, engine=regG.engine))
        nc.gpsimd.free_register(vG.val)
        nc.gpsimd.free_register(vD.val)

    def gather_tile(i):
        t2 = i % 2
        for s_ in range(4):
            j = i * 4 + s_
            xb, xk = xg[j % 2], f"xg{j % 2}"
            S.ind_dma(xb[:, :], x1f_d[:, :], idx_all[:, j:j + 1], True, reads=["idx_all"], writes=[xk])
            for kc in range(8):
                if kc % 4 == 0:
                    pb, pkk = bank()
                q4 = kc % 4
                S.op("pe", lambda e, pb=pb, kc=kc, q4=q4, xb=xb: e.transpose(out=pb[:, q4 * 128:(q4 + 1) * 128], in_=xb[:, kc * 128:(kc + 1) * 128], identity=ident[:]),
                     reads=[xk, "c_ident"], writes=[pkk])
                if q4 == 3:
                    k0 = kc - 3
                    eng = "act" if k0 == 0 else "dve"
                    if eng == "act":
                        S.op("act", lambda e, pb=pb, k0=k0, s_=s_: e.activation(out=xgT[t2][:, k0:k0 + 4, s_ * 128:(s_ + 1) * 128],
                                                                                in_=pb[:, :].rearrange("p (k t) -> p k t", t=128), func=AF.Copy),
                             reads=[pkk], writes=[f"xgT{t2}_{s_}a"])
                    else:
                        S.op("dve", lambda e, pb=pb, k0=k0, s_=s_: e.tensor_copy(out=xgT[t2][:, k0:k0 + 4, s_ * 128:(s_ + 1) * 128],
                                                                                 in_=pb[:, :].rearrange("p (k t) -> p k t", t=128)),
                             reads=[pkk], writes=[f"xgT{t2}_{s_}d"])

    load_w(0)
    gather_tile(0)
    cnt = 0
    for ui, (i, f0, nf) in enumerate(units):
        i2 = ui % 2
        t2 = i % 2
        if ui + 1 < len(units):
            load_w(ui + 1)
        if f0 == 0 and i + 1 < NT:
            gather_tile(i + 1)
        xkeys = [f"xgT{t2}_{s_}{a}" for s_ in range(4) for a in "ad"]
        for f in range(nf):
            pg, pgk = bank()
            pu, puk = bank()
            for kc in range(8):
                S.op("pe", lambda e, pg=pg, kc=kc, f=f: e.matmul(pg[:, :], lhsT=wg[i2][:, kc, f * 128:(f + 1) * 128], rhs=xgT[t2][:, kc, :],
                                                                 start=(kc == 0), stop=(kc == 7)),
                     reads=[f"swg{i2}"] + xkeys, writes=[pgk])
            for kc in range(8):
                S.op("pe", lambda e, pu=pu, kc=kc, f=f: e.matmul(pu[:, :], lhsT=wu[i2][:, kc, f * 128:(f + 1) * 128], rhs=xgT[t2][:, kc, :],
                                                                 start=(kc == 0), stop=(kc == 7)),
                     reads=[f"swu{i2}"] + xkeys, writes=[puk])
            sb_ = sgb[cnt % 2]
            sk = f"ssgb{cnt % 2}"
            cnt += 1
            S.op("act", lambda e, pg=pg, sb_=sb_: e.activation(out=sb_[:, :], in_=pg[:, :], func=AF.Silu), reads=[pgk], writes=[sk])
            S.op("dve", lambda e, pu=pu, sb_=sb_, f=f: e.tensor_tensor(out=hT[:, f, :], in0=sb_[:, :], in1=pu[:, :], op=ALU.mult),
                 reads=[puk, sk], writes=[f"shT{f}"])
        for s_ in range(4):
            for half in range(2):
                pb, pkk = bank()
                for f in range(nf):
                    S.op("pe", lambda e, pb=pb, f=f, s_=s_, half=half: e.matmul(pb[:, :], lhsT=hT[:, f, s_ * 128:(s_ + 1) * 128],
                                                                                rhs=wd[i2][:, f, half * 512:(half + 1) * 512],
                                                                                start=(f == 0), stop=(f == nf - 1)),
                         reads=[f"shT{f}", f"swd{i2}"], writes=[pkk])
                yk = f"yacc{t2}_{s_}"
                if f0 == 0:
                    S.op("act", lambda e, pb=pb, s_=s_, half=half: e.activation(out=yacc[t2][:, s_, half * 512:(half + 1) * 512], in_=pb[:, :], func=AF.Copy),
                         reads=[pkk], writes=[yk])
                else:
                    S.op("dve", lambda e, pb=pb, s_=s_, half=half: e.tensor_tensor(out=yacc[t2][:, s_, half * 512:(half + 1) * 512], in0=pb[:, :],
                                                                                   in1=yacc[t2][:, s_, half * 512:(half + 1) * 512], op=ALU.add),
                         reads=[pkk, yk], writes=[yk])
        if f0 + nf == nfc:
            S.dma("sp", y_d[i * 512:(i + 1) * 512, :].rearrange("(s p) d -> p s d", p=128), yacc[t2][:, :, :],
                  reads=[f"yacc{t2}_{s_}" for s_ in range(4)], writes=[f"y_d{t2}"])
    cx.pop()
    ya = [cx.sb(f"ya{i}", [128, D]) for i in range(2)]
    yb = [cx.sb(f"yb{i}", [128, D]) for i in range(2)]
    x1b = [cx.sb(f"x1b{i}", [128, D]) for i in range(2)]
    yo = [cx.sb(f"syo{i}", [128, D]) for i in range(2)]
    st = cx.sb("bnst3", [128, 12])
    mv = cx.sb("bnmv3", [128, 4])
    def comb_load(sg):
        i2 = sg % 2
        S.dma("sp", x1b[i2][:, :], x1f_d[sg * 128:(sg + 1) * 128, :], writes=[f"x1b{i2}"])
        S.ind_dma(ya[i2][:, :], y_d[:, :], posi[:, 0, sg:sg + 1], True, reads=["posi"], writes=[f"ya{i2}"])
        S.ind_dma(yb[i2][:, :], y_d[:, :], posi[:, 1, sg:sg + 1], True, reads=["posi"], writes=[f"yb{i2}"])

    comb_load(0)
    for sg in range(nsub):
        i2 = sg % 2
        if sg + 1 < nsub:
            comb_load(sg + 1)
        S.op("act", lambda e: e.activation(out=x1b[i2][:, :], in_=x1b[i2][:, :], func=AF.Copy, scale=ALPHA), reads=[f"x1b{i2}"], writes=[f"x1b{i2}"])
        S.op("dve", lambda e, sg=sg: e.scalar_tensor_tensor(out=x1b[i2][:, :], in0=ya[i2][:, :], scalar=gts[:, 0, sg:sg + 1], in1=x1b[i2][:, :],
                                                            op0=ALU.mult, op1=ALU.add), reads=[f"ya{i2}", "gts", f"x1b{i2}"], writes=[f"x1b{i2}"])
        S.op("dve", lambda e, sg=sg: e.scalar_tensor_tensor(out=x1b[i2][:, :], in0=yb[i2][:, :], scalar=gts[:, 1, sg:sg + 1], in1=x1b[i2][:, :],
                                                            op0=ALU.mult, op1=ALU.add), reads=[f"yb{i2}", "gts", f"x1b{i2}"], writes=[f"x1b{i2}"])
        layer_norm_rows(x1b[i2][:, :], f"x1b{i2}", yo[i2][:, :], f"syo{i2}", 2, 3, (st, mv))
        S.dma("sp", xout[sg * 128:(sg + 1) * 128, :], yo[i2][:, :], reads=[f"syo{i2}"], writes=[f"xout{i2}"])
    cx.pop()


GROUPS = [[0, 1, 2, 3], [4, 5, 6, 7]]


def phase_x(cx, st_d, stall_d, pv_d, sin):
    nc, S, c = cx.nc, cx.S, cx.c
    ident = c["ident"]
    ps, pk = cx.ps, cx.psk
    cx.push()
    pv = cx.sb("pvx", [128, NPV])
    S.dma("sp", pv[:], pv_d[:, :], writes=["pv"])
    S.coll("AllGather", st_d[:, :], stall_d[:, :], GROUPS, writes=["stall"])
    Gt = cx.sb("Gt", [128, 4, 4, 256])
    for sg in range(3):
        S.dma("sp", Gt[:, sg, :, :], stall_d[sg, :].rearrange("(h d c) -> d h c", h=4, d=128), reads=["stall"], writes=[f"Gt{sg}"])
    pmt = [cx.sb(f"pmt{i}", [128, 128]) for i in range(2)]
    t2 = cx.sb("t2", [128, 128])
    t3 = cx.sb("t3", [128, 128])
    for h in range(4):
        for i, sg in enumerate((1, 2)):
            S.op("pe", lambda e, sg=sg, i=i: e.transpose(out=ps[i][:, 0:128], in_=Gt[:, sg, h, 128:256], identity=ident[:]),
                 reads=[f"Gt{sg}", "c_ident"], writes=[pk[i]])
            S.op("dve", lambda e, i=i: e.tensor_copy(out=pmt[i][:], in_=ps[i][:, 0:128]), reads=[pk[i]], writes=[f"pmt{i}"])
        S.op("pe", lambda e: e.matmul(ps[2][:, 0:128], lhsT=pmt[0][:], rhs=Gt[:, 0, h, 0:128], start=True, stop=True),
             reads=["pmt0", "Gt0"], writes=[pk[2]])
        S.op("dve", lambda e: e.tensor_tensor(out=t2[:], in0=Gt[:, 1, h, 0:128], in1=ps[2][:, 0:128], op=ALU.add), reads=[pk[2], "Gt1"], writes=["t2"])
        S.op("pe", lambda e: e.matmul(ps[3][:, 0:128], lhsT=pmt[1][:], rhs=t2[:], start=True, stop=True), reads=["pmt1", "t2"], writes=[pk[3]])
        S.op("dve", lambda e: e.tensor_tensor(out=t3[:], in0=Gt[:, 2, h, 0:128], in1=ps[3][:, 0:128], op=ALU.add), reads=[pk[3], "Gt2"], writes=["t3"])
        S.op("dve", lambda e: e.tensor_scalar(out=sin[h][:, :], in0=Gt[:, 0, h, 0:128], scalar1=pv[:, PV_MSEG + 1:PV_MSEG + 2], scalar2=None, op0=ALU.mult),
             reads=["Gt0", "pv"], writes=[f"sin{h}"])
        S.op("dve", lambda e: e.scalar_tensor_tensor(out=sin[h][:, :], in0=t2[:], scalar=pv[:, PV_MSEG + 2:PV_MSEG + 3], in1=sin[h][:, :],
                                                     op0=ALU.mult, op1=ALU.add), reads=["t2", "pv", f"sin{h}"], writes=[f"sin{h}"])
        S.op("dve", lambda e: e.scalar_tensor_tensor(out=sin[h][:, :], in0=t3[:], scalar=pv[:, PV_MSEG + 3:PV_MSEG + 4], in1=sin[h][:, :],
                                                     op0=ALU.mult, op1=ALU.add), reads=["t3", "pv", f"sin{h}"], writes=[f"sin{h}"])
    cx.pop()


def phase_h(cx, S_core, x1in, hall_d, pv_d):
    S = cx.S
    cx.push()
    pv = cx.sb("pvh", [128, NPV])
    S.dma("sp", pv[:], pv_d[:, :], writes=["pv"])
    S.coll("AllGather", x1in[S_core:S_core + HALO, :], hall_d[:, :], GROUPS, writes=["hall"])
    ht = [cx.sb(f"ht{i}", [128, D]) for i in range(2)]
    hacc = cx.sb("hacc", [128, D])
    for r in range(4):
        S.dma("sp", ht[r % 2][:, :], hall_d[r * 128:(r + 1) * 128, :], reads=["hall"], writes=[f"ht{r % 2}"])
        if r == 0:
            S.op("dve", lambda e: e.tensor_scalar(out=hacc[:, :], in0=ht[0][:, :], scalar1=pv[:, PV_MPRED:PV_MPRED + 1], scalar2=None, op0=ALU.mult),
                 reads=["ht0", "pv"], writes=["hacc"])
        else:
            S.op("dve", lambda e, r=r: e.scalar_tensor_tensor(out=hacc[:, :], in0=ht[r % 2][:, :], scalar=pv[:, PV_MPRED + r:PV_MPRED + r + 1],
                                                              in1=hacc[:, :], op0=ALU.mult, op1=ALU.add),
                 reads=[f"ht{r % 2}", "pv", "hacc"], writes=["hacc"])
    S.dma("sp", x1in[0:HALO, :], hacc[:, :], reads=["hacc"], writes=["x1halo"])
    cx.pop()


def build_fused(S_core, depth=2):
    nc = bass.Bass("TRN2", target_bir_lowering=False)
    cx = Ctx(nc)
    cx.push()
    cx.consts()
    di = lambda n, s: nc.dram_tensor(n, s, F32, kind="ExternalInput").ap()
    dn = lambda n, s: nc.dram_tensor(n, s, F32).ap()
    xin0 = di("xin0", [HALO + S_core, D])
    win = di("win", [depth, D, IN_COLS])
    wout = di("wout", [depth, D, D])
    pv = di("pv", [depth, 128, NPV])
    lnrows = di("lnrows", [depth, 4, 128, D])
    dense = {"moe": False, "wg": di("dwg", [1, D, DFF_DENSE]), "wu": di("dwu", [1, D, DFF_DENSE]), "wd": di("dwd", [1, DFF_DENSE, D])}
    moe = {"moe": True, "router": di("router", [D, NEXP]), "wg": di("mwg", [NEXP, D, DFF_EXP]),
           "wu": di("mwu", [NEXP, D, DFF_EXP]), "wd": di("mwd", [NEXP, DFF_EXP, D])}
    xout = nc.dram_tensor("xout", [S_core, D], F32, kind="ExternalOutput").ap()
    yc = dn("yc_i", [512, S_core])
    zg = dn("zg_i", [512, S_core])
    qkv = dn("qkv_i", [4, 3, 128, S_core])
    gb = dn("gb_i", [4, 2, S_core])
    oc = dn("oc_i", [2, 512, S_core])
    st = dn("st_i", [1, 4 * 128 * 256])
    stall = dn("stall_i", [4, 4 * 128 * 256])
    x1in = dn("x1in_i", [HALO + S_core, D])
    hall = dn("hall_i", [4 * HALO, D])
    stv = st[0, :].rearrange("(h d c) -> h d c", h=4, d=128)
    NTs = 2 * S_core // 512 + NEXP - 1
    scr = {"x1f": dn("x1f_i", [S_core, D]), "s2t": nc.dram_tensor("s2t_i", [NTs * 512, 1], I32).ap(), "y": dn("y_i", [NTs * 512, D])}
    for l in range(depth):
        cx.push()
        sin = [cx.sb(f"sin{h}", [128, 128]) for h in range(4)]
        xin = xin0 if l == 0 else x1in
        phase_a(cx, S_core, xin, win[l], pv[l], yc, zg, qkv, gb)
        phase_b(cx, S_core, qkv, gb, pv[l], oc, stv)
        phase_x(cx, st, stall, pv[l], sin)
        last = (l == depth - 1)
        dst = xout if last else x1in[HALO:HALO + S_core, :]
        phase_c(cx, S_core, oc, yc, zg, xin, wout[l], lnrows[l], pv[l], moe if l % 2 == 1 else dense, dst, sin, scr)
        if not last:
            phase_h(cx, S_core, x1in, hall, pv[l])
        cx.pop()
    cx.pop()
    return nc


_CACHE = {}


def _pvec(inp, l, seg, S_core):
    f = np.float32
    pv = np.zeros((128, NPV), f)
    dw = np.asarray(inp["conv_dw_w"][l], f)
    pv[:, PV_DWW:PV_DWW + 124] = dw.reshape(31, 4, 128).transpose(2, 1, 0).reshape(128, 124)
    pv[:, PV_DWB:PV_DWB + 4] = np.asarray(inp["conv_dw_b"][l], f).reshape(4, 128).T
    pv[:, PV_CLG:PV_CLG + 4] = np.asarray(inp["conv_ln_g"][l], f).reshape(4, 128).T
    pv[:, PV_CLB:PV_CLB + 4] = np.asarray(inp["conv_ln_b"][l], f).reshape(4, 128).T
    sc = np.asarray(inp["short_conv_w"][l], f)
    pv[:, PV_SCW:PV_SCW + 48] = sc.reshape(4, 12, 128).transpose(2, 1, 0).reshape(128, 48)
    pv[:, PV_ONG] = np.asarray(inp["out_norm_g"][l], f)
    alog = np.asarray(inp["a_log"][l], f)
    dtb = np.asarray(inp["dt_bias"][l], f)
    p = np.arange(128)
    h128 = np.minimum(p // (S_core // 128), 3)
    pv[:, PV_ALOG128] = alog[h128]
    pv[:, PV_DTB128] = dtb[h128]
    for i in range(2):
        h64 = np.minimum((i * 128 + p) // (S_core // 64), 3)
        pv[:, PV_ALOG64 + i] = alog[h64]
        pv[:, PV_DTB64 + i] = dtb[h64]
    pv[:, PV_MSEG + seg] = 1.0
    if seg >= 1:
        pv[:, PV_MPRED + seg - 1] = 1.0
    return pv


def kernel(**inp):
    x = np.asarray(inp["x"], np.float32)
    B, S_tot, _ = x.shape
    S_core = S_tot // NSEG
    cores = list(range(NCORES))
    depth = inp["w_in"].shape[0]
    if ("f", S_core) not in _CACHE:
        _CACHE[("f", S_core)] = build_fused(S_core, depth)
    prog = _CACHE[("f", S_core)]
    ca = lambda a: np.ascontiguousarray(np.asarray(a, dtype=np.float32))
    lnrows = ca(np.stack([np.stack([np.broadcast_to(np.asarray(inp[k][l], np.float32), (128, D))
                                    for k in ("ln_mix_g", "ln_mix_b", "ln_ffn_g", "ln_ffn_b")], 0) for l in range(depth)], 0))
    shared = {"win": ca(inp["w_in"]), "wout": ca(inp["w_out"]), "lnrows": lnrows,
              "dwg": ca(inp["ffn_w_gate"][0:1]), "dwu": ca(inp["ffn_w_up"][0:1]), "dwd": ca(inp["ffn_w_down"][0:1]),
              "router": ca(inp["router_w"][0]), "mwg": ca(inp["moe_w_gate"][0]), "mwu": ca(inp["moe_w_up"][0]),
              "mwd": ca(inp["moe_w_down"][0])}
    in_maps = []
    for cidx in cores:
        b, sg = divmod(cidx, NSEG)
        buf = np.zeros((HALO + S_core, D), np.float32)
        lo = sg * S_core - HALO
        if lo >= 0:
            buf[:] = x[b, lo:lo + HALO + S_core]
        else:
            buf[HALO:] = x[b, 0:S_core]
        d = {"xin0": buf, "pv": ca(np.stack([_pvec(inp, l, sg, S_core) for l in range(depth)], 0))}
        d.update(shared)
        in_maps.append(d)
    res = run_bass_kernel_spmd(prog, in_maps, core_ids=cores).results
    out = np.empty_like(x)
    for cidx in cores:
        b, sg = divmod(cidx, NSEG)
        out[b, sg * S_core:(sg + 1) * S_core] = res[cidx]["xout"]
    return out
```
